# Optimizing a Trainium2 kernel written in Bass

```python
import jax, jax.numpy as jnp
from jax import lax
import numpy as np

D_MODEL = 4096
BATCH = 1
SEQ = 8192
DEPTH = 1

MIX_WIDTH = D_MODEL
ATTN_WIDTH = MIX_WIDTH // 2
POOL_WIDTH = MIX_WIDTH - ATTN_WIDTH
HEAD_DIM = 128
N_HEADS = ATTN_WIDTH // HEAD_DIM
N_KV_HEADS = 4
GROUP = N_HEADS // N_KV_HEADS
KV_WIDTH = N_KV_HEADS * HEAD_DIM
IN_WIDTH = POOL_WIDTH + ATTN_WIDTH + 2 * KV_WIDTH
POOL_WINDOWS = (2, 4, 8, 16)
N_POOL_GROUPS = len(POOL_WINDOWS)
POOL_GROUP_WIDTH = POOL_WIDTH // N_POOL_GROUPS
GRID_W = 64
ROPE_THETA = 10000.0
ROPE_AXIS_DIM = HEAD_DIM // 2
Q_BLOCK = 128
N_EXPERTS = 32
TOP_K = 4
D_FF = D_MODEL // 4
SWIGLU_ALPHA = 1.702
SWIGLU_LIMIT = 7.0
MOE_BLOCK = 128
N_MOD = 6
EPS = 1e-6

kernel_name = "hybrid_pool_axialgqa_moe_block"


def _rmsnorm(x, g):
    xf = x.astype(jnp.float32)
    y = xf * lax.rsqrt(jnp.mean(xf * xf, axis=-1, keepdims=True) + EPS)
    return (y * g.astype(jnp.float32)).astype(x.dtype)


def _axial_rope_tables(seq):
    n_rows = seq // GRID_W
    row = jnp.repeat(jnp.arange(n_rows, dtype=jnp.float32), GRID_W)
    col = jnp.tile(jnp.arange(GRID_W, dtype=jnp.float32), n_rows)
    inv_freq = ROPE_THETA ** (-jnp.arange(0, ROPE_AXIS_DIM, 2, dtype=jnp.float32) / ROPE_AXIS_DIM)
    ang = jnp.stack([row, col], axis=-1)[:, :, None] * inv_freq
    return jnp.cos(ang), jnp.sin(ang)


def _apply_axial_rope(x, cos, sin):
    b, s, h, _ = x.shape
    xr = x.astype(jnp.float32).reshape(b, s, h, 2, 2, ROPE_AXIS_DIM // 2)
    x1, x2 = xr[..., 0, :], xr[..., 1, :]
    cs = cos[None, :, None]
    sn = sin[None, :, None]
    out = jnp.stack([x1 * cs - x2 * sn, x1 * sn + x2 * cs], axis=-2)
    return out.reshape(b, s, h, HEAD_DIM).astype(x.dtype)


def _attention(q, k, v, q_norm_g, k_norm_g, cos, sin):
    b, s, _ = q.shape
    q = _apply_axial_rope(_rmsnorm(q.reshape(b, s, N_HEADS, HEAD_DIM), q_norm_g), cos, sin)
    k = _apply_axial_rope(_rmsnorm(k.reshape(b, s, N_KV_HEADS, HEAD_DIM), k_norm_g), cos, sin)
    v = v.reshape(b, s, N_KV_HEADS, HEAD_DIM)
    nb = s // Q_BLOCK
    qb = q.reshape(b, nb, Q_BLOCK, N_KV_HEADS, GROUP, HEAD_DIM).transpose(1, 0, 3, 4, 2, 5)
    kt = k.transpose(0, 2, 1, 3)
    vt = v.transpose(0, 2, 1, 3)
    scale = HEAD_DIM ** -0.5

    def attend_block(q_blk):
        sc = jnp.einsum('bkgqd,bksd->bkgqs', q_blk, kt, preferred_element_type=jnp.float32) * scale
        p = jax.nn.softmax(sc, axis=-1)
        return jnp.einsum('bkgqs,bksd->bkgqd', p.astype(vt.dtype), vt)

    o = lax.map(attend_block, qb)
    return o.transpose(1, 0, 4, 2, 3, 5).reshape(b, s, ATTN_WIDTH)


def _multiscale_pool(p, w_pool, pool_scale):
    b, s, _ = p.shape
    pf = p.astype(jnp.float32)
    cs = jnp.concatenate([jnp.zeros((b, 1, POOL_WIDTH), jnp.float32), jnp.cumsum(pf, axis=1)], axis=1)
    t = jnp.arange(s)
    outs = []
    for gi, w in enumerate(POOL_WINDOWS):
        lo = jnp.clip(t - w // 2, 0, s - 1)
        hi = jnp.clip(t + w // 2 - 1, 0, s - 1)
        sl = slice(gi * POOL_GROUP_WIDTH, (gi + 1) * POOL_GROUP_WIDTH)
        csg = cs[:, :, sl]
        win_sum = csg[:, hi + 1] - csg[:, lo]
        cnt = (hi - lo + 1).astype(jnp.float32)[None, :, None]
        outs.append(win_sum / cnt - pf[:, :, sl])
    d = jnp.stack(outs, axis=2).astype(p.dtype)
    y = jnp.einsum('bsgc,gcd->bsgd', d, w_pool)
    return y.reshape(b, s, POOL_WIDTH) * pool_scale


def _moe(h, w_router, b_router, w1, b1, w2, b2):
    b, s, d = h.shape
    n = b * s
    xt = h.reshape(n, d)
    logits = (xt @ w_router + b_router).astype(jnp.float32)
    top_vals, top_idx = lax.top_k(logits, TOP_K)
    gates = jax.nn.softmax(top_vals, axis=-1)
    nk = n * TOP_K
    e_flat = top_idx.reshape(-1)
    tok_flat = jnp.arange(nk, dtype=jnp.int32) // TOP_K
    g_flat = gates.reshape(-1)
    order = jnp.argsort(e_flat, stable=True)
    e_sorted = e_flat[order]
    tok_sorted = tok_flat[order]
    g_sorted = g_flat[order]
    sizes = jnp.bincount(e_flat, length=N_EXPERTS)
    padded = ((sizes + MOE_BLOCK - 1) // MOE_BLOCK) * MOE_BLOCK
    start = jnp.cumsum(sizes) - sizes
    pend = jnp.cumsum(padded)
    pstart = pend - padded
    dest = pstart[e_sorted] + (jnp.arange(nk) - start[e_sorted])
    n_blocks = nk // MOE_BLOCK + N_EXPERTS
    n_rows = n_blocks * MOE_BLOCK
    row_tok = jnp.zeros((n_rows,), jnp.int32).at[dest].set(tok_sorted.astype(jnp.int32))
    row_gate = jnp.zeros((n_rows,), jnp.float32).at[dest].set(g_sorted)
    blk_start = jnp.arange(n_blocks) * MOE_BLOCK
    block_expert = jnp.clip(jnp.searchsorted(pend, blk_start, side='right'), 0, N_EXPERTS - 1)

    def step(out, blk):
        tok, gte, e = blk
        xb = xt[tok]
        hb = xb @ w1[e] + b1[e]
        glu = jnp.minimum(hb[:, :D_FF], SWIGLU_LIMIT)
        lin = jnp.clip(hb[:, D_FF:], -SWIGLU_LIMIT, SWIGLU_LIMIT)
        act = glu * jax.nn.sigmoid(SWIGLU_ALPHA * glu) * (lin + 1)
        yb = act @ w2[e] + b2[e]
        out = out.at[tok].add(yb.astype(jnp.float32) * gte[:, None])
        return out, None

    out, _ = lax.scan(step, jnp.zeros((n, d), jnp.float32),
                      (row_tok.reshape(n_blocks, MOE_BLOCK), row_gate.reshape(n_blocks, MOE_BLOCK), block_expert))
    return out.reshape(b, s, d).astype(h.dtype)


def setup_inputs(seed: int = 0) -> dict:
    key = jax.random.key(seed)
    ks = jax.random.split(key, 18)

    def nrm(k, shape, scale):
        return jax.random.normal(k, shape, jnp.float32) * scale

    return {
        "x": nrm(ks[0], (BATCH, SEQ, D_MODEL), 1.0),
        "c": nrm(ks[1], (BATCH, D_MODEL), 1.0),
        "w_mod": nrm(ks[2], (DEPTH, D_MODEL, N_MOD * D_MODEL), 0.5 * D_MODEL ** -0.5),
        "b_mod": nrm(ks[3], (DEPTH, N_MOD * D_MODEL), 0.02),
        "norm1_g": 1.0 + nrm(ks[4], (DEPTH, D_MODEL), 0.05),
        "w_in": nrm(ks[5], (DEPTH, D_MODEL, IN_WIDTH), D_MODEL ** -0.5),
        "q_norm_g": 1.0 + nrm(ks[6], (DEPTH, HEAD_DIM), 0.05),
        "k_norm_g": 1.0 + nrm(ks[7], (DEPTH, HEAD_DIM), 0.05),
        "w_pool": nrm(ks[8], (DEPTH, N_POOL_GROUPS, POOL_GROUP_WIDTH, POOL_GROUP_WIDTH), POOL_GROUP_WIDTH ** -0.5),
        "pool_scale": 1.0 + nrm(ks[9], (DEPTH, POOL_WIDTH), 0.1),
        "w_out": nrm(ks[10], (DEPTH, MIX_WIDTH, D_MODEL), MIX_WIDTH ** -0.5),
        "norm2_g": 1.0 + nrm(ks[11], (DEPTH, D_MODEL), 0.05),
        "w_router": nrm(ks[12], (DEPTH, D_MODEL, N_EXPERTS), D_MODEL ** -0.5),
        "b_router": nrm(ks[13], (DEPTH, N_EXPERTS), 0.01),
        "w1": nrm(ks[14], (DEPTH, N_EXPERTS, D_MODEL, 2 * D_FF), D_MODEL ** -0.5),
        "b1": nrm(ks[15], (DEPTH, N_EXPERTS, 2 * D_FF), 0.01),
        "w2": nrm(ks[16], (DEPTH, N_EXPERTS, D_FF, D_MODEL), D_FF ** -0.5),
        "b2": nrm(ks[17], (DEPTH, N_EXPERTS, D_MODEL), 0.01),
    }


def reference(x, c, w_mod, b_mod, norm1_g, w_in, q_norm_g, k_norm_g, w_pool, pool_scale,
              w_out, norm2_g, w_router, b_router, w1, b1, w2, b2):
    b, s, d = x.shape
    cos, sin = _axial_rope_tables(s)
    c_act = jax.nn.silu(c)
    for l in range(DEPTH):
        mod = c_act @ w_mod[l] + b_mod[l]
        sh1, sc1, g1, sh2, sc2, g2 = [m[:, None, :] for m in jnp.split(mod, N_MOD, axis=-1)]
        h = _rmsnorm(x, norm1_g[l]) * (1 + sc1) + sh1
        proj = h @ w_in[l]
        pool_in, q, k, v = jnp.split(
            proj, [POOL_WIDTH, POOL_WIDTH + ATTN_WIDTH, POOL_WIDTH + ATTN_WIDTH + KV_WIDTH], axis=-1)
        attn_out = _attention(q, k, v, q_norm_g[l], k_norm_g[l], cos, sin)
        pool_out = _multiscale_pool(pool_in, w_pool[l], pool_scale[l])
        mix = jnp.concatenate([attn_out, pool_out], axis=-1)
        x = x + g1 * (mix @ w_out[l])
        h2 = _rmsnorm(x, norm2_g[l]) * (1 + sc2) + sh2
        x = x + g2 * _moe(h2, w_router[l], b_router[l], w1[l], b1[l], w2[l], b2[l])
    return x
```

```python
import os
import numpy as np
from contextlib import ExitStack
import ml_dtypes
import concourse.bass as bass
import concourse.mybir as mybir
from concourse.bass_utils import run_bass_kernel_spmd

F32 = mybir.dt.float32
BF16 = mybir.dt.bfloat16
ALU = mybir.AluOpType
AF = mybir.ActivationFunctionType
AX = mybir.AxisListType

NC = 8
SKIP = set(filter(None, os.environ.get("K_SKIP", "").split(",")))
S = 8192
D = 4096
T = S // NC
HALO = 8
NG = S // 512
NOWN = T // 512
KC = D // 128
EPS = 1e-6
NE = 32
DFF = 1024
SW_ALPHA = 1.702
SW_LIMIT = 7.0


def _tables(core):
    tok = (core * T + np.arange(S)) % S
    row = (tok // 64).astype(np.float32)
    col = (tok % 64).astype(np.float32)
    inv_freq = (10000.0 ** (-np.arange(0, 64, 2, dtype=np.float32) / 64.0)).astype(np.float32)
    ang_r = row[None, :] * inv_freq[:, None]
    ang_c = col[None, :] * inv_freq[:, None]
    C = np.concatenate([np.cos(ang_r), np.cos(ang_r), np.cos(ang_c), np.cos(ang_c)], 0).astype(np.float32)
    Sn = np.concatenate([np.sin(ang_r), np.sin(ang_r), np.sin(ang_c), np.sin(ang_c)], 0).astype(np.float32)
    t_ext = core * T - HALO + np.arange(T + 2 * HALO)
    valid = ((t_ext >= 0) & (t_ext < S)).astype(np.float32)
    pmask = np.broadcast_to(valid[None, :], (128, T + 2 * HALO)).copy()
    t_own = core * T + np.arange(T)
    inv = np.zeros((4, T), np.float32)
    for gi, w in enumerate((2, 4, 8, 16)):
        lo = np.clip(t_own - w // 2, 0, S - 1)
        hi = np.clip(t_own + w // 2 - 1, 0, S - 1)
        inv[gi] = 1.0 / (hi - lo + 1).astype(np.float32)
    invcnt = np.broadcast_to(inv[:, None, :], (4, 128, T)).copy()
    return C, Sn, pmask, invcnt


def _consts():
    rot = np.zeros((128, 128), np.float32)
    for m in range(128):
        if (m % 64) < 32:
            rot[m + 32, m] = -1.0
        else:
            rot[m - 32, m] = 1.0
    ident = np.eye(128, dtype=np.float32)
    esel = np.zeros((32, 32, 128), np.float32)
    for e in range(32):
        esel[e, e, :] = 1.0
    return rot.astype(ml_dtypes.bfloat16), ident.astype(ml_dtypes.bfloat16), ident, esel


_G = {"st0": None, "pool": [], "resid": {}}


class Cnt:
    def __init__(self, nc, st, name):
        self.s = _G["st0"].enter_context(nc.semaphore(name))
        self.n = 0


def clear_sems(nc, sems=None):
    return


def hi16(ap):
    v = ap.bitcast(BF16)
    return v[:, 1::2]


class Sched:
    ENG = ("sync", "scalar", "vector", "tensor", "gpsimd")

    def __init__(self, nc, st, name):
        self.nc = nc; self.st = st; self.name = name
        self.steps = []
        self.sems = {}
        self.counts = {}

    def _sem(self, key):
        if key not in self.sems:
            h = _G["pool"].pop(0)
            self.sems[key] = h
            self.counts[key] = _G["resid"][h.num]
        return self.sems[key]

    def add(self, owner, fn, deps=(), dma_key=None):
        key = f"d_{dma_key}" if dma_key is not None else f"e_{owner}"
        self._sem(key)
        self.counts[key] += 16 if dma_key is not None else 1
        self.steps.append((owner, fn, tuple(d for d in deps if d is not None), key, self.counts[key]))
        return len(self.steps) - 1

    def run(self, final_waits=()):
        nc = self.nc
        steps = self.steps
        trunc = os.environ.get("K_TRUNC_" + self.name)
        if trunc is not None:
            n = int(trunc)
            steps = steps[:n]
            self.steps = steps
            final_waits = [i for i in range(max(0, n - 12), n)]
        with nc.Block() as blk:
            for owner in self.ENG:
                mine = [i for i, s in enumerate(steps) if s[0] == owner]
                if not mine and not (owner == "gpsimd" and final_waits):
                    continue
                def body(e, owner=owner, mine=mine):
                    waited = {}
                    for i in mine:
                        _, fn, deps, key, cnt = steps[i]
                        for d in deps:
                            dk, dc = steps[d][3], steps[d][4]
                            if waited.get(dk, 0) < dc:
                                e.wait_ge(self.sems[dk], dc)
                                waited[dk] = dc
                        ins = fn(e)
                        ins.then_inc(self.sems[key], 16 if key.startswith("d_") else 1)
                    if owner == "gpsimd":
                        for d in final_waits:
                            e.wait_ge(self.sems[steps[d][3]], steps[d][4])
                getattr(blk, owner)(body)
        for key, h in self.sems.items():
            _G["resid"][h.num] = self.counts[key]
            _G["pool"].append(h)


def build(upto=99, debug_outs=False):
    nc = bass.Bass("TRN2", target_bir_lowering=False)
    declared = []
    nc._declared_inputs = declared
    def di(name, shape, dt=F32, need=0):
        if upto < need:
            return None
        declared.append(name)
        return nc.dram_tensor(name, list(shape), dt, kind="ExternalInput").ap()
    x = di("x", [S, D])
    c_in = di("c", [1, D])
    w_mod = di("w_mod", [D, 6 * D])
    b_mod = di("b_mod", [1, 6 * D])
    norm1_g = di("norm1_g", [1, D])
    w_in = di("w_in", [D, 5120])
    q_norm_g = di("q_norm_g", [1, 128])
    k_norm_g = di("k_norm_g", [1, 128])
    w_pool = di("w_pool", [4, 512, 512], need=3)
    pool_scale = di("pool_scale", [1, 2048], need=3)
    w_out = di("w_out", [D, D], need=4)
    norm2_g = di("norm2_g", [1, D])
    w_router = di("w_router", [D, NE], need=4)
    b_router = di("b_router", [1, NE], need=4)
    NEd = int(os.environ.get("K_NEXP", NE))
    w1 = di("w1", [NEd, D, 2 * DFF], need=5)
    b1 = di("b1", [NE, 2 * DFF], need=5)
    w2 = di("w2", [NEd, DFF, D], need=5)
    b2 = di("b2", [NE, D], need=4)
    ropeC = di("ropeC", [128, S], need=2)
    ropeS = di("ropeS", [128, S], need=2)
    pmask = di("pmask", [128, T + 2 * HALO], need=3)
    invcnt = di("invcnt", [4, 128, T], need=3)
    rot_in = di("rotm", [128, 128], BF16)
    identb_in = di("identb", [128, 128], BF16)
    identf_in = di("identf", [128, 128])
    onesb_in = di("onesb", [128, 128], BF16)
    esel_in = di("esel", [32, 32 * 128], need=4)

    out = nc.dram_tensor("out", [T, D], F32, kind="ExternalOutput").ap()

    kind_dbg = "ExternalOutput" if debug_outs else "Internal"
    mod_d = nc.dram_tensor("mod_d", [1, 6 * D], F32, kind=kind_dbg).ap()
    qraw_d = nc.dram_tensor("qraw_d", [16, 128, T], F32, kind=kind_dbg).ap()
    kraw_d = nc.dram_tensor("kraw_d", [4, 128, S], F32, kind=kind_dbg).ap()
    v_d = nc.dram_tensor("v_d", [4, 128, S // 128, 128], BF16, kind=kind_dbg).ap()
    pi_d = nc.dram_tensor("pi_d", [16, 128, T + 2 * HALO], F32, kind=kind_dbg).ap()
    qT_d = nc.dram_tensor("qT_d", [16, 128, T], BF16).ap()
    kT_d = nc.dram_tensor("kT_d", [4, 128, S], BF16).ap()
    mixT_d = nc.dram_tensor("mixT_d", [32, 128, T], BF16, kind=kind_dbg).ap()
    h2T_d = nc.dram_tensor("h2T_d", [32, 128, T], BF16, kind=kind_dbg).ap()
    lgs_d = nc.dram_tensor("lgs_d", [1, 16], F32).ap()
    moe_d = nc.dram_tensor("moe_d", [T, D], F32, kind=kind_dbg).ap()
    gates_dbg = nc.dram_tensor("gates_dbg", [32, T], F32, kind=kind_dbg).ap()

    st0 = ExitStack()
    _G["st0"] = st0
    _G["pool"] = [st0.enter_context(nc.semaphore(f"pool{i}")) for i in range(52)]
    _G["resid"] = {h.num: 0 for h in _G["pool"]}
    sb = lambda name, shape, dt=F32: st0.enter_context(nc.sbuf_tensor(name, list(shape), dt))
    G1 = sb("G1", [128, 32]); SH1 = sb("SH1", [128, 32])
    G2 = sb("G2", [128, 32]); SH2 = sb("SH2", [128, 32])
    identb = sb("identb_s", [128, 128], BF16)
    identf = sb("identf_s", [128, 128])
    rotm = sb("rotm_s", [128, 128], BF16)
    onesb = sb("onesb_s", [128, 128], BF16)
    qng = sb("qng", [128, 1]); kng = sb("kng", [128, 1])
    gatesT = sb("gatesT", [32, T])

    cT = sb("cT", [128, KC]); cact = sb("cact", [128, KC])
    HM = 3 * D
    for half in range(2):
        with ExitStack() as st:
            wst = [st.enter_context(nc.sbuf_tensor(f"wmod{half}_{i}", [128, KC, 256], F32)) for i in range(2)]
            mo = st.enter_context(nc.sbuf_tensor(f"mo{half}", [1, HM], F32))
            bm = st.enter_context(nc.sbuf_tensor(f"bm{half}", [1, HM], F32))
            ps = [st.enter_context(nc.psum_tensor(f"pm{half}_{i}", [128, 512], F32)) for i in range(2)]
            ld = Cnt(nc, st, f"p0_ld{half}"); wf = Cnt(nc, st, f"p0_wf{half}")
            ca = Cnt(nc, st, f"p0_ca{half}"); pd = Cnt(nc, st, f"p0_pd{half}"); pfree = Cnt(nc, st, f"p0_pfree{half}")
            stc = Cnt(nc, st, f"p0_st{half}")
            NT = HM // 256
            nld = 8 if half == 0 else 1
            with nc.Block() as blk:
                @blk.sync
                def _(e):
                    if half == 0:
                        with nc.allow_non_contiguous_dma(reason="small param transposes"):
                            e.dma_start(out=cT[:], in_=c_in.rearrange("o (k p) -> p (o k)", p=128)).then_inc(ld.s, 16)
                            e.dma_start(out=qng[:], in_=q_norm_g.rearrange("o p -> p o")).then_inc(ld.s, 16)
                            e.dma_start(out=kng[:], in_=k_norm_g.rearrange("o p -> p o")).then_inc(ld.s, 16)
                        e.dma_start(out=identb[:], in_=identb_in[:, :]).then_inc(ld.s, 16)
                        e.dma_start(out=identf[:], in_=identf_in[:, :]).then_inc(ld.s, 16)
                        e.dma_start(out=rotm[:], in_=rot_in[:, :]).then_inc(ld.s, 16)
                        e.dma_start(out=onesb[:], in_=onesb_in[:, :]).then_inc(ld.s, 16)
                    e.dma_start(out=bm[:], in_=b_mod[:, half * HM:(half + 1) * HM]).then_inc(ld.s, 16)
                    for i in range(NT):
                        if i >= 2:
                            e.wait_ge(pfree.s, i - 1)
                        c0 = half * HM + i * 256
                        e.dma_start(out=wst[i % 2][:],
                                    in_=w_mod[:, c0:c0 + 256].rearrange("(k p) n -> p k n", p=128)
                                    ).then_inc(wf.s, 16)

                @blk.scalar
                def _(e):
                    e.wait_ge(ld.s, 16 * nld)
                    if half == 0:
                        e.activation(out=cact[:], in_=cT[:], func=AF.Silu).then_inc(ca.s, 1)

                @blk.tensor
                def _(e):
                    if half == 0:
                        e.wait_ge(ca.s, 1)
                    for i in range(NT):
                        e.wait_ge(wf.s, 16 * (i + 1))
                        if i >= 2:
                            e.wait_ge(pfree.s, i - 1)
                        for k in range(KC):
                            mm = e.matmul(ps[i % 2][0:1, 0:256], lhsT=cact[:, k:k + 1], rhs=wst[i % 2][:, k, :],
                                          start=(k == 0), stop=(k == KC - 1))
                        mm.then_inc(pd.s, 1)

                @blk.vector
                def _(e):
                    e.wait_ge(ld.s, 16 * nld)
                    for i in range(NT):
                        e.wait_ge(pd.s, i + 1)
                        e.tensor_tensor(out=mo[0:1, i * 256:(i + 1) * 256], in0=ps[i % 2][0:1, 0:256],
                                        in1=bm[0:1, i * 256:(i + 1) * 256], op=ALU.add).then_inc(pfree.s, 1)

                @blk.gpsimd
                def _(e):
                    e.wait_ge(pfree.s, NT)
                    e.dma_start(out=mod_d[:, half * HM:(half + 1) * HM], in_=mo[:]).then_inc(stc.s, 16)
                    e.wait_ge(stc.s, 16)
            clear_sems(nc)

    with ExitStack() as st:
        modF = st.enter_context(nc.sbuf_tensor("modF", [128, 192], F32))
        n1g = st.enter_context(nc.sbuf_tensor("n1g", [128, 32], F32))
        n2g = st.enter_context(nc.sbuf_tensor("n2g", [128, 32], F32))
        ld = Cnt(nc, st, "p0b_ld"); dv = Cnt(nc, st, "p0b_dv")
        with nc.Block() as blk:
            @blk.sync
            def _(e):
                with nc.allow_non_contiguous_dma(reason="small param transposes"):
                    e.dma_start(out=modF[:], in_=mod_d.rearrange("o (j p) -> p (o j)", p=128)).then_inc(ld.s, 16)
                    e.dma_start(out=n1g[:], in_=norm1_g.rearrange("o (j p) -> p (o j)", p=128)).then_inc(ld.s, 16)
                    e.dma_start(out=n2g[:], in_=norm2_g.rearrange("o (j p) -> p (o j)", p=128)).then_inc(ld.s, 16)

            @blk.vector
            def _(e):
                e.wait_ge(ld.s, 48)
                e.scalar_tensor_tensor(out=G1[:], in0=modF[:, 32:64], scalar=1.0, in1=n1g[:],
                                       op0=ALU.add, op1=ALU.mult)
                e.scalar_tensor_tensor(out=G2[:], in0=modF[:, 128:160], scalar=1.0, in1=n2g[:],
                                       op0=ALU.add, op1=ALU.mult)
                e.tensor_copy(out=SH1[:], in_=modF[:, 0:32])
                e.tensor_copy(out=SH2[:], in_=modF[:, 96:128]).then_inc(dv.s, 1)
                e.wait_ge(dv.s, 1)
        clear_sems(nc)

    if upto <= 0:
        _finish_dummy(nc, out)
        return nc

    if "1" not in SKIP:
        def blocks_for(g):
            if g < NOWN:
                return list(range(20))
            if g == NOWN or g == NG - 1:
                return list(range(8)) + [16, 17, 18, 19]
            return [16, 17, 18, 19]

        with ExitStack() as st:
            xb = [st.enter_context(nc.sbuf_tensor(f"xb{i}", [128, D], F32)) for i in range(2)]
            xn = [st.enter_context(nc.sbuf_tensor(f"xn{i}", [128, D], BF16)) for i in range(2)]
            hT = [st.enter_context(nc.sbuf_tensor(f"hT{i}", [128, KC, 512], BF16)) for i in range(2)]
            wb = [st.enter_context(nc.sbuf_tensor(f"wb{i}", [128, KC, 256], F32)) for i in range(2)]
            ob = [st.enter_context(nc.sbuf_tensor(f"ob{i}", [128, 512], F32)) for i in range(4)]
            NTILE = S // 128
            ss = st.enter_context(nc.sbuf_tensor("ss", [128, NTILE], F32))
            rs = st.enter_context(nc.sbuf_tensor("rs", [128, NTILE], F32))
            rs2 = st.enter_context(nc.sbuf_tensor("rs2", [128, NTILE], F32))
            sq = st.enter_context(nc.sbuf_tensor("sq", [128, NTILE], F32))
            sq_done = Cnt(nc, st, "sq_done")
            junk = st.enter_context(nc.sbuf_tensor("junk", [128, D], BF16))
            tp = [st.enter_context(nc.psum_tensor(f"tp{i}", [128, 4, 128], BF16)) for i in range(2)]
            acc = [st.enter_context(nc.psum_tensor(f"acc{i}", [128, 512], F32)) for i in range(2)]
            z_done = Cnt(nc, st, "z_done")
            x_full = Cnt(nc, st, "x_full")
            ss_done = Cnt(nc, st, "ss_done")
            rs_done = Cnt(nc, st, "rs_done")
            xn_done = Cnt(nc, st, "xn_done")
            tp_done = Cnt(nc, st, "tp_done")
            tp_free = Cnt(nc, st, "tp_free")
            w_full = Cnt(nc, st, "w_full")
            acc_done = Cnt(nc, st, "acc_done")
            ev_done = Cnt(nc, st, "ev_done")
            o_free = Cnt(nc, st, "o_free")

            sched = []
            blk_end_chunk = []
            grp_end_chunk = []
            for g in range(NG):
                for b in blocks_for(g):
                    if b >= 18:
                        for sub in range(4):
                            sched.append((g, b, 'V', sub))
                    else:
                        for m in range(2):
                            sched.append((g, b, 'F', m))
                    blk_end_chunk.append(len(sched))
                grp_end_chunk.append(len(sched))

            with nc.Block() as blk:
                @blk.sync
                def _(e):
                    bi = 0
                    def load_x(g):
                        for sub in range(4):
                            i = g * 4 + sub
                            if i >= 2:
                                e.wait_ge(xn_done.s, i - 1)
                            e.dma_start(out=xb[i % 2][:], in_=x[i * 128:(i + 1) * 128, :]).then_inc(x_full.s, 16)
                    def load_w(g):
                        nonlocal bi
                        for b in blocks_for(g):
                            if bi >= 2:
                                e.wait_ge(acc_done.s, blk_end_chunk[bi - 2])
                            e.dma_start(out=wb[bi % 2][:],
                                        in_=w_in[:, b * 256:(b + 1) * 256].rearrange("(k p) n -> p k n", p=128)
                                        ).then_inc(w_full.s, 16)
                            bi += 1
                    load_x(0)
                    for g in range(1, NG):
                        load_x(g)
                        load_w(g - 1)
                    load_w(NG - 1)

                @blk.scalar
                def _(e):
                    nb = 0
                    e.wait_ge(z_done.s, 1)
                    for i in range(NTILE):
                        g = i // 4; sub = i % 4
                        e.wait_ge(x_full.s, 16 * (i + 1))
                        e.activation(out=junk[:], in_=xb[i % 2][:], func=AF.Square,
                                     accum_out=ss[:, i:i + 1]).then_inc(ss_done.s, 1)
                        e.wait_ge(rs_done.s, 2 * i + 1)
                        e.activation(out=sq[:, i:i + 1], in_=rs[:, i:i + 1], func=AF.Sqrt).then_inc(sq_done.s, 1)
                        if sub == 0 and g >= 2:
                            e.wait_ge(acc_done.s, grp_end_chunk[g - 2])
                        for bt in range(8):
                            e.wait_ge(tp_done.s, nb + 1)
                            for j in range(4):
                                kc = bt * 4 + j
                                ins = e.activation(out=hT[g % 2][:, kc, sub * 128:(sub + 1) * 128],
                                                   in_=tp[nb % 2][:, j, :], func=AF.Identity,
                                                   bias=SH1[:, kc:kc + 1], scale=G1[:, kc:kc + 1])
                            ins.then_inc(tp_free.s, 1)
                            nb += 1

                @blk.vector
                def _(e):
                    e.memset(ss[:], 0.0).then_inc(z_done.s, 1)
                    ci = 0
                    def evac_chunks(upto_ci):
                        nonlocal ci
                        while ci < upto_ci:
                            g, b, kind, sidx = sched[ci]
                            e.wait_ge(acc_done.s, ci + 1)
                            if ci >= 4:
                                e.wait_ge(o_free.s, 16 * (ci - 3))
                            if kind == 'V':
                                ins = e.tensor_copy(out=ob[ci % 4][:, 0:128].bitcast(BF16),
                                                    in_=acc[ci % 2][:, 0:256])
                            else:
                                ins = e.tensor_copy(out=ob[ci % 4][:], in_=acc[ci % 2][:])
                            ins.then_inc(ev_done.s, 1)
                            ci += 1
                    for i in range(NTILE):
                        g = i // 4
                        e.wait_ge(ss_done.s, i + 1)
                        col = slice(i, i + 1)
                        e.tensor_scalar(out=rs[:, col], in0=ss[:, col], scalar1=1.0 / D, scalar2=EPS,
                                        op0=ALU.mult, op1=ALU.add).then_inc(rs_done.s, 1)
                        e.wait_ge(sq_done.s, i + 1)
                        e.reciprocal(out=rs2[:, col], in_=sq[:, col]).then_inc(rs_done.s, 1)
                        e.wait_ge(rs_done.s, 2 * i + 2)
                        if i >= 2:
                            e.wait_ge(tp_done.s, 8 * (i - 1))
                        e.tensor_scalar(out=xn[i % 2][:], in0=xb[i % 2][:], scalar1=rs2[:, col], scalar2=None,
                                        op0=ALU.mult).then_inc(xn_done.s, 1)
                        if i % 4 == 3 and g >= 1:
                            evac_chunks(grp_end_chunk[g - 1])
                    evac_chunks(len(sched))

                @blk.tensor
                def _(e):
                    nb = 0
                    ci = 0
                    bi = 0
                    def proj_group(g):
                        nonlocal ci, bi
                        e.wait_ge(tp_free.s, 32 * (g + 1))
                        for b in blocks_for(g):
                            e.wait_ge(w_full.s, 16 * (bi + 1))
                            w = wb[bi % 2]
                            if b >= 18:
                                for sub in range(4):
                                    if ci >= 2:
                                        e.wait_ge(ev_done.s, ci - 1)
                                    for k in range(KC):
                                        mm = e.matmul(acc[ci % 2][:, 0:256],
                                                      lhsT=hT[g % 2][:, k, sub * 128:(sub + 1) * 128],
                                                      rhs=hi16(w[:, k, :]), start=(k == 0), stop=(k == KC - 1))
                                    mm.then_inc(acc_done.s, 1)
                                    ci += 1
                            else:
                                for m in range(2):
                                    if ci >= 2:
                                        e.wait_ge(ev_done.s, ci - 1)
                                    for k in range(KC):
                                        mm = e.matmul(acc[ci % 2][:, :],
                                                      lhsT=hi16(w[:, k, m * 128:(m + 1) * 128]),
                                                      rhs=hT[g % 2][:, k, :], start=(k == 0), stop=(k == KC - 1))
                                    mm.then_inc(acc_done.s, 1)
                                    ci += 1
                            bi += 1

                    for i in range(NTILE):
                        g = i // 4
                        e.wait_ge(xn_done.s, i + 1)
                        for bt in range(8):
                            if nb >= 2:
                                e.wait_ge(tp_free.s, nb - 1)
                            for j in range(4):
                                kc = bt * 4 + j
                                tr = e.transpose(tp[nb % 2][:, j, :], xn[i % 2][:, kc * 128:(kc + 1) * 128], identb[:])
                            tr.then_inc(tp_done.s, 1)
                            nb += 1
                        if i % 4 == 3 and g >= 1:
                            proj_group(g - 1)
                    proj_group(NG - 1)

                @blk.gpsimd
                def _(e):
                    with nc.allow_non_contiguous_dma(reason="scratch layouts"):
                        for ci, (g, b, kind, sidx) in enumerate(sched):
                            e.wait_ge(ev_done.s, ci + 1)
                            if kind == 'V':
                                half = b - 18
                                chunk = g * 4 + sidx
                                src = ob[ci % 4][:, 0:128].bitcast(BF16)
                                e.dma_start(out=v_d[half * 2:half * 2 + 2, :, chunk, :].rearrange("h p d -> p h d"),
                                            in_=src.rearrange("p (h d) -> p h d", h=2)).then_inc(o_free.s, 16)
                            else:
                                f = b * 2 + sidx
                                if f < 16:
                                    if g < NOWN:
                                        e.dma_start(out=pi_d[f, :, HALO + g * 512:HALO + (g + 1) * 512],
                                                    in_=ob[ci % 4][:]).then_inc(o_free.s, 16)
                                    elif g == NOWN:
                                        e.dma_start(out=pi_d[f, :, HALO + T:HALO + T + HALO],
                                                    in_=ob[ci % 4][:, 0:HALO]).then_inc(o_free.s, 16)
                                    else:
                                        e.dma_start(out=pi_d[f, :, 0:HALO],
                                                    in_=ob[ci % 4][:, 512 - HALO:512]).then_inc(o_free.s, 16)
                                elif f < 32:
                                    e.dma_start(out=qraw_d[f - 16, :, g * 512:(g + 1) * 512],
                                                in_=ob[ci % 4][:]).then_inc(o_free.s, 16)
                                else:
                                    e.dma_start(out=kraw_d[f - 32, :, g * 512:(g + 1) * 512],
                                                in_=ob[ci % 4][:]).then_inc(o_free.s, 16)
                        e.wait_ge(o_free.s, 16 * len(sched))
            clear_sems(nc)

    if upto <= 1:
        _finish_dummy(nc, out)
        return nc

    if "1b" not in SKIP:
        items = []
        for g in range(NG):
            for kv in range(4):
                items.append(('k', kv, g))
            if g < NOWN:
                for h in range(16):
                    items.append(('q', h, g))
        NI = len(items)
        grp_first = {}
        for n, (kind, idx, g) in enumerate(items):
            grp_first.setdefault(g, n)
        with ExitStack() as st:
            ib = [st.enter_context(nc.sbuf_tensor(f"ib{i}", [128, 512], F32)) for i in range(2)]
            cs = [st.enter_context(nc.sbuf_tensor(f"cs{i}", [128, 2, 512], F32)) for i in range(2)]
            sqv = [st.enter_context(nc.sbuf_tensor(f"sqv{i}", [128, 512], BF16)) for i in range(2)]
            yg = [st.enter_context(nc.sbuf_tensor(f"yg{i}", [128, 512], BF16)) for i in range(2)]
            t0 = [st.enter_context(nc.sbuf_tensor(f"t0_{i}", [128, 512], F32)) for i in range(2)]
            t1 = [st.enter_context(nc.sbuf_tensor(f"t1_{i}", [128, 512], F32)) for i in range(2)]
            rstd = [st.enter_context(nc.sbuf_tensor(f"rstd{i}", [128, 512], F32)) for i in range(2)]
            u1 = [st.enter_context(nc.sbuf_tensor(f"u1_{i}", [128, 512], F32)) for i in range(2)]
            u2 = [st.enter_context(nc.sbuf_tensor(f"u2_{i}", [128, 512], F32)) for i in range(2)]
            u3 = [st.enter_context(nc.sbuf_tensor(f"u3_{i}", [128, 512], F32)) for i in range(2)]
            outb = [st.enter_context(nc.sbuf_tensor(f"outb{i}", [128, 512], BF16)) for i in range(2)]
            p1 = [st.enter_context(nc.psum_tensor(f"p1_{i}", [128, 512], F32)) for i in range(2)]
            p2 = [st.enter_context(nc.psum_tensor(f"p2_{i}", [128, 512], F32)) for i in range(2)]
            in_full = Cnt(nc, st, "b_in_full"); cs_full = Cnt(nc, st, "b_cs_full")
            a_done = Cnt(nc, st, "b_a_done")
            a3_done = Cnt(nc, st, "b_a3_done")
            pe_done = Cnt(nc, st, "b_pe_done")
            d_done = Cnt(nc, st, "b_d_done")
            st_done = Cnt(nc, st, "b_st_done")
            with nc.Block() as blk:
                @blk.sync
                def _(e):
                    for n, (kind, idx, g) in enumerate(items):
                        if grp_first[g] == n:
                            if g >= 2:
                                e.wait_ge(d_done.s, 6 * grp_first[g - 1])
                            e.dma_start(out=cs[g % 2][:, 0, :], in_=ropeC[:, g * 512:(g + 1) * 512]).then_inc(cs_full.s, 16)
                            e.dma_start(out=cs[g % 2][:, 1, :], in_=ropeS[:, g * 512:(g + 1) * 512]).then_inc(cs_full.s, 16)
                        if n >= 2:
                            e.wait_ge(a_done.s, 2 * (n - 1))
                        src = kraw_d[idx, :, g * 512:(g + 1) * 512] if kind == 'k' else qraw_d[idx, :, g * 512:(g + 1) * 512]
                        e.dma_start(out=ib[n % 2][:], in_=src).then_inc(in_full.s, 16)

                @blk.scalar
                def _(e):
                    for n, (kind, idx, g) in enumerate(items):
                        b = n % 2
                        e.wait_ge(in_full.s, 16 * (n + 1))
                        if n >= 2:
                            e.wait_ge(pe_done.s, n - 1)
                        e.activation(out=sqv[b][:], in_=ib[b][:], func=AF.Square).then_inc(a_done.s, 1)
                        ng = kng if kind == 'k' else qng
                        e.activation(out=yg[b][:], in_=ib[b][:], func=AF.Copy, scale=ng[:, 0:1]).then_inc(a_done.s, 1)
                        e.wait_ge(d_done.s, 6 * n + 1)
                        e.activation(out=t1[b][:], in_=t0[b][:], func=AF.Sqrt).then_inc(a3_done.s, 1)

                @blk.tensor
                def _(e):
                    for n, (kind, idx, g) in enumerate(items):
                        b = n % 2
                        e.wait_ge(a_done.s, 2 * (n + 1))
                        if n >= 2:
                            e.wait_ge(d_done.s, 6 * (n - 2) + 4)
                        e.matmul(p1[b][:], lhsT=onesb[:], rhs=sqv[b][:], start=True, stop=True)
                        e.matmul(p2[b][:], lhsT=rotm[:], rhs=yg[b][:], start=True, stop=True).then_inc(pe_done.s, 1)

                @blk.vector
                def _(e):
                    for n, (kind, idx, g) in enumerate(items):
                        b = n % 2
                        e.wait_ge(pe_done.s, n + 1)
                        e.wait_ge(cs_full.s, 32 * (g + 1))
                        if n >= 2:
                            e.wait_ge(st_done.s, 16 * (n - 1))
                        e.tensor_scalar(out=t0[b][:], in0=p1[b][:], scalar1=1.0 / 128, scalar2=EPS,
                                        op0=ALU.mult, op1=ALU.add).then_inc(d_done.s, 1)
                        e.tensor_tensor(out=u1[b][:], in0=yg[b][:], in1=cs[g % 2][:, 0, :], op=ALU.mult
                                        ).then_inc(d_done.s, 1)
                        e.tensor_tensor(out=u2[b][:], in0=p2[b][:], in1=cs[g % 2][:, 1, :], op=ALU.mult
                                        ).then_inc(d_done.s, 1)
                        e.wait_ge(d_done.s, 6 * n + 3)
                        e.tensor_tensor(out=u3[b][:], in0=u1[b][:], in1=u2[b][:], op=ALU.add
                                        ).then_inc(d_done.s, 1)
                        e.wait_ge(a3_done.s, n + 1)
                        e.reciprocal(out=rstd[b][:], in_=t1[b][:]).then_inc(d_done.s, 1)
                        e.wait_ge(d_done.s, 6 * n + 5)
                        e.tensor_tensor(out=outb[b][:], in0=u3[b][:], in1=rstd[b][:], op=ALU.mult
                                        ).then_inc(d_done.s, 1)

                @blk.gpsimd
                def _(e):
                    for n, (kind, idx, g) in enumerate(items):
                        e.wait_ge(d_done.s, 6 * (n + 1))
                        dst = kT_d[idx, :, g * 512:(g + 1) * 512] if kind == 'k' else qT_d[idx, :, g * 512:(g + 1) * 512]
                        e.dma_start(out=dst, in_=outb[n % 2][:]).then_inc(st_done.s, 16)
                    e.wait_ge(st_done.s, 16 * NI)
            clear_sems(nc)

    if "2" not in SKIP:
        aitems = [(kv, qt, hg) for kv in range(4) for qt in range(NOWN) for hg in range(4)]
        NA = len(aitems)
        NSC = S // 128
        SCALE = 128 ** -0.5
        with ExitStack() as st:
            kb = [st.enter_context(nc.sbuf_tensor(f"kb{i}", [128, S], BF16)) for i in range(2)]
            vb = [st.enter_context(nc.sbuf_tensor(f"vb{i}", [128, NSC, 128], BF16)) for i in range(2)]
            qb = [st.enter_context(nc.sbuf_tensor(f"qb{i}", [128, 512], BF16)) for i in range(2)]
            pb = [st.enter_context(nc.sbuf_tensor(f"pb{i}", [128, 512], BF16)) for i in range(2)]
            rden = [st.enter_context(nc.sbuf_tensor(f"rden{i}", [128, 512], F32)) for i in range(2)]
            aob = [st.enter_context(nc.sbuf_tensor(f"aob{i}", [128, 512], BF16)) for i in range(2)]
            sp = [st.enter_context(nc.psum_tensor(f"sp{i}", [128, 512], F32)) for i in range(2)]
            oacc = [st.enter_context(nc.psum_tensor(f"oacc{i}", [128, 512], F32)) for i in range(2)]
            dacc = [st.enter_context(nc.psum_tensor(f"dacc{i}", [128, 512], F32)) for i in range(2)]
            kv_full = Cnt(nc, st, "kv_full"); q_full = Cnt(nc, st, "q_full")
            s_done = Cnt(nc, st, "s_done"); p_done = Cnt(nc, st, "p_done"); od_done = Cnt(nc, st, "od_done")
            fin = Cnt(nc, st, "a_fin"); ast = Cnt(nc, st, "a_st")
            with nc.Block() as blk:
                @blk.sync
                def _(e):
                    with nc.allow_non_contiguous_dma(reason="kv tiles"):
                        for n, (kv, qt, hg) in enumerate(aitems):
                            if qt == 0 and hg == 0:
                                if kv >= 2:
                                    e.wait_ge(od_done.s, NSC * (NOWN * 4) * (kv - 1))
                                e.dma_start(out=kb[kv % 2][:], in_=kT_d[kv, :, :]).then_inc(kv_full.s, 16)
                                e.dma_start(out=vb[kv % 2][:], in_=v_d[kv, :, :, :]).then_inc(kv_full.s, 16)
                            if n >= 2:
                                e.wait_ge(s_done.s, NSC * (n - 1))
                            h = kv * 4 + hg
                            e.dma_start(out=qb[n % 2][:], in_=qT_d[h, :, qt * 512:(qt + 1) * 512]).then_inc(q_full.s, 16)

                @blk.tensor
                def _(e):
                    def s_mm(n, sc):
                        kv = aitems[n][0]
                        c = n * NSC + sc
                        if sc == 0:
                            e.wait_ge(q_full.s, 16 * (n + 1))
                            if aitems[n][1] == 0 and aitems[n][2] == 0:
                                e.wait_ge(kv_full.s, 32 * (kv + 1))
                        if c >= 2:
                            e.wait_ge(p_done.s, c - 1)
                        e.matmul(sp[c % 2][:], lhsT=kb[kv % 2][:, sc * 128:(sc + 1) * 128], rhs=qb[n % 2][:],
                                 start=True, stop=True).then_inc(s_done.s, 1)
                    def od_mm(n, sc):
                        kv = aitems[n][0]
                        c = n * NSC + sc
                        e.wait_ge(p_done.s, c + 1)
                        if sc == 0 and n >= 2:
                            e.wait_ge(fin.s, 2 * (n - 1))
                        e.matmul(oacc[n % 2][:], lhsT=vb[kv % 2][:, sc, :], rhs=pb[c % 2][:],
                                 start=(sc == 0), stop=(sc == NSC - 1))
                        e.matmul(dacc[n % 2][:], lhsT=onesb[:], rhs=pb[c % 2][:],
                                 start=(sc == 0), stop=(sc == NSC - 1)).then_inc(od_done.s, 1)
                    total = NA * NSC
                    for c in range(total + 1):
                        if c < total:
                            s_mm(c // NSC, c % NSC)
                        if c >= 1:
                            od_mm((c - 1) // NSC, (c - 1) % NSC)

                @blk.scalar
                def _(e):
                    for c in range(NA * NSC):
                        e.wait_ge(s_done.s, c + 1)
                        if c >= 2:
                            e.wait_ge(od_done.s, c - 1)
                        e.activation(out=pb[c % 2][:], in_=sp[c % 2][:], func=AF.Exp, scale=SCALE).then_inc(p_done.s, 1)

                @blk.vector
                def _(e):
                    for n in range(NA):
                        e.wait_ge(od_done.s, NSC * (n + 1))
                        e.reciprocal(out=rden[n % 2][:], in_=dacc[n % 2][:]).then_inc(fin.s, 1)
                        e.wait_ge(fin.s, 2 * n + 1)
                        if n >= 2:
                            e.wait_ge(ast.s, 16 * (n - 1))
                        e.tensor_tensor(out=aob[n % 2][:], in0=oacc[n % 2][:], in1=rden[n % 2][:], op=ALU.mult
                                        ).then_inc(fin.s, 1)

                @blk.gpsimd
                def _(e):
                    for n, (kv, qt, hg) in enumerate(aitems):
                        e.wait_ge(fin.s, 2 * (n + 1))
                        h = kv * 4 + hg
                        e.dma_start(out=mixT_d[h, :, qt * 512:(qt + 1) * 512], in_=aob[n % 2][:]).then_inc(ast.s, 16)
                    e.wait_ge(ast.s, 16 * NA)
            clear_sems(nc)

    if upto <= 2:
        _finish_dummy(nc, out)
        return nc

    if "3" not in SKIP:
        L = T + 2 * HALO
        with ExitStack() as st3:
            pmk = st3.enter_context(nc.sbuf_tensor("pmk", [128, L], F32))
            ps_sb = st3.enter_context(nc.sbuf_tensor("ps_sb", [128, 16], F32))
            pin = [st3.enter_context(nc.sbuf_tensor(f"pin{i}", [128, L], F32)) for i in range(2)]
            pmb = st3.enter_context(nc.sbuf_tensor("pmb", [128, L], F32))
            aw = [st3.enter_context(nc.sbuf_tensor(f"aw{i}", [128, L], F32)) for i in range(4)]
            ptmp = st3.enter_context(nc.sbuf_tensor("ptmp", [128, T], F32))
            invc = st3.enter_context(nc.sbuf_tensor("invc", [128, T], F32))
            dT = st3.enter_context(nc.sbuf_tensor("dT", [128, 4, T], BF16))
            wp = st3.enter_context(nc.sbuf_tensor("wp", [128, 4, 512], F32))
            pob = [st3.enter_context(nc.sbuf_tensor(f"pob{i}", [128, 512], BF16)) for i in range(2)]
            pacc = [st3.enter_context(nc.psum_tensor(f"pacc{i}", [128, 512], F32)) for i in range(2)]
            with ExitStack() as st:
                sc = Sched(nc, st, "p3pre")
                with nc.allow_non_contiguous_dma(reason="small"):
                    a = sc.add("gpsimd", lambda e: e.dma_start(out=pmk[:], in_=pmask[:, :]), dma_key="pmk")
                    b = sc.add("gpsimd", lambda e: e.dma_start(out=ps_sb[:], in_=pool_scale.rearrange("o (j p) -> p (o j)", p=128)),
                               dma_key="ps")
                    sc.run(final_waits=[a, b])
            for gi in range(4):
                wlog = gi + 1
                with ExitStack() as st:
                    sc = Sched(nc, st, f"p3a{gi}")
                    s_inv = sc.add("sync", lambda e: e.dma_start(out=invc[:], in_=invcnt[gi, :, :]), dma_key="invc")
                    last_pin = [None, None]
                    last = None
                    for j in range(4):
                        cc = gi * 4 + j
                        s_ld = sc.add("sync", lambda e, cc=cc, j=j: e.dma_start(out=pin[j % 2][:], in_=pi_d[cc, :, :]),
                                      deps=[last_pin[j % 2]], dma_key=f"pin{j % 2}")
                        s = sc.add("vector", lambda e, j=j: e.tensor_tensor(out=pmb[:], in0=pin[j % 2][:], in1=pmk[:], op=ALU.mult),
                                   deps=[s_ld, last])
                        last_pin[j % 2] = s
                        src = pmb
                        lo, hi = 0, L
                        for lv in range(wlog):
                            sh = 1 if lv == 0 else 2 ** (lv - 1)
                            dst = aw[lv]
                            if lv == 0:
                                nlo, nhi = lo + 1, hi
                                s = sc.add("vector", lambda e, dst=dst, src=src, nlo=nlo, nhi=nhi:
                                           e.tensor_tensor(out=dst[:, nlo:nhi], in0=src[:, nlo - 1:nhi - 1], in1=src[:, nlo:nhi], op=ALU.add),
                                           deps=[s])
                            else:
                                nlo, nhi = lo + sh, hi - sh
                                s = sc.add("vector", lambda e, dst=dst, src=src, nlo=nlo, nhi=nhi, sh=sh:
                                           e.tensor_tensor(out=dst[:, nlo:nhi], in0=src[:, nlo - sh:nhi - sh], in1=src[:, nlo + sh:nhi + sh], op=ALU.add),
                                           deps=[s])
                            src = dst; lo, hi = nlo, nhi
                        assert lo <= HALO and hi >= HALO + T
                        s = sc.add("vector", lambda e, src=src: e.tensor_tensor(out=ptmp[:], in0=src[:, HALO:HALO + T], in1=invc[:], op=ALU.mult),
                                   deps=[s, s_inv])
                        s = sc.add("vector", lambda e, j=j: e.tensor_tensor(out=dT[:, j, :], in0=ptmp[:], in1=pmb[:, HALO:HALO + T], op=ALU.subtract),
                                   deps=[s])
                        last = s
                    sc.run()
                with ExitStack() as st:
                    sc = Sched(nc, st, f"p3b{gi}")
                    s_w = sc.add("sync", lambda e: e.dma_start(out=wp[:], in_=w_pool[gi, :, :].rearrange("(j p) n -> p j n", p=128)),
                                 dma_key="wp")
                    evs = []; sts = []
                    cidx = 0
                    for jo in range(4):
                        for tt in range(NOWN):
                            c = cidx; cidx += 1
                            def mmf(e, jo=jo, tt=tt, c=c):
                                for ji in range(4):
                                    mm = e.matmul(pacc[c % 2][:], lhsT=hi16(wp[:, ji, jo * 128:(jo + 1) * 128]),
                                                  rhs=dT[:, ji, tt * 512:(tt + 1) * 512], start=(ji == 0), stop=(ji == 3))
                                return mm
                            s_mm = sc.add("tensor", mmf, deps=[s_w, evs[c - 2] if c >= 2 else None])
                            col = gi * 4 + jo
                            s_ev = sc.add("vector", lambda e, c=c, col=col: e.tensor_scalar(out=pob[c % 2][:], in0=pacc[c % 2][:],
                                          scalar1=ps_sb[:, col:col + 1], scalar2=None, op0=ALU.mult),
                                          deps=[s_mm, sts[c - 2] if c >= 2 else None])
                            evs.append(s_ev)
                            s_st = sc.add("gpsimd", lambda e, c=c, col=col, tt=tt: e.dma_start(
                                          out=mixT_d[16 + col, :, tt * 512:(tt + 1) * 512], in_=pob[c % 2][:]),
                                          deps=[s_ev], dma_key=f"pob{c % 2}")
                            sts.append(s_st)
                    sc.run(final_waits=sts[-2:])

    if upto <= 3:
        _finish_dummy(nc, out)
        return nc

    if "4a" not in SKIP:
        with ExitStack() as st:
            g1bc = st.enter_context(nc.sbuf_tensor("g1bc", [128, D], F32))
            mixb = [st.enter_context(nc.sbuf_tensor(f"mixb{i}", [128, KC, 512], BF16)) for i in range(2)]
            wob = [st.enter_context(nc.sbuf_tensor(f"wob{i}", [128, KC, 256], F32)) for i in range(2)]
            xp = [st.enter_context(nc.sbuf_tensor(f"xp{i}", [128, 256], F32)) for i in range(4)]
            tq = [st.enter_context(nc.sbuf_tensor(f"tq{i}", [128, 256], F32)) for i in range(2)]
            om = [st.enter_context(nc.sbuf_tensor(f"om{i}", [128, 256], F32)) for i in range(4)]
            oacc4 = [st.enter_context(nc.psum_tensor(f"oacc4_{i}", [128, 256], F32)) for i in range(2)]
            sc = Sched(nc, st, "p4a")
            s_g1 = sc.add("sync", lambda e: e.dma_start(out=g1bc[:], in_=mod_d[0:1, 2 * D:3 * D].partition_broadcast(128)),
                          dma_key="g1bc")
            mm_steps = []; t_steps = []; xm_steps = []; st_steps = []
            ci = 0
            last_mm_of_tg = {}
            last_mm_of_blk = {}
            for tg in range(NOWN):
                s_mix = sc.add("sync", lambda e, tg=tg: e.dma_start(out=mixb[tg % 2][:],
                               in_=mixT_d[:, :, tg * 512:(tg + 1) * 512].rearrange("m p t -> p m t")),
                               deps=[last_mm_of_tg.get(tg - 2)], dma_key=f"mixb{tg % 2}")
                for nb in range(16):
                    k = tg * 16 + nb
                    s_wo = sc.add("sync", lambda e, nb=nb, k=k: e.dma_start(out=wob[k % 2][:],
                                  in_=w_out[:, nb * 256:(nb + 1) * 256].rearrange("(m p) n -> p m n", p=128)),
                                  deps=[last_mm_of_blk.get(k - 2)], dma_key=f"wob{k % 2}")
                    for sub in range(4):
                        r0 = tg * 512 + sub * 128
                        s_x = sc.add("sync", lambda e, r0=r0, nb=nb, ci=ci: e.dma_start(out=xp[ci % 4][:],
                                     in_=x[r0:r0 + 128, nb * 256:(nb + 1) * 256]),
                                     deps=[xm_steps[ci - 4] if ci >= 4 else None], dma_key=f"xp{ci % 4}")
                        def mmf(e, tg=tg, k=k, sub=sub, ci=ci):
                            for m in range(KC):
                                mm = e.matmul(oacc4[ci % 2][:], lhsT=mixb[tg % 2][:, m, sub * 128:(sub + 1) * 128],
                                              rhs=hi16(wob[k % 2][:, m, :]), start=(m == 0), stop=(m == KC - 1))
                            return mm
                        s_mm = sc.add("tensor", mmf, deps=[s_mix, s_wo, t_steps[ci - 2] if ci >= 2 else None])
                        s_t = sc.add("vector", lambda e, ci=ci, nb=nb: e.tensor_tensor(out=tq[ci % 2][:], in0=oacc4[ci % 2][:],
                                     in1=g1bc[:, nb * 256:(nb + 1) * 256], op=ALU.mult),
                                     deps=[s_mm, s_g1, xm_steps[ci - 2] if ci >= 2 else None])
                        s_xm = sc.add("vector", lambda e, ci=ci: e.tensor_tensor(out=om[ci % 4][:], in0=tq[ci % 2][:],
                                      in1=xp[ci % 4][:], op=ALU.add),
                                      deps=[s_t, s_x, st_steps[ci - 4] if ci >= 4 else None])
                        s_st = sc.add("gpsimd", lambda e, ci=ci, r0=r0, nb=nb: e.dma_start(
                                      out=out[r0:r0 + 128, nb * 256:(nb + 1) * 256], in_=om[ci % 4][:]),
                                      deps=[s_xm], dma_key=f"om{ci % 4}")
                        mm_steps.append(s_mm); t_steps.append(s_t); xm_steps.append(s_xm); st_steps.append(s_st)
                        last_mm_of_tg[tg] = s_mm; last_mm_of_blk[k] = s_mm
                        ci += 1
            with nc.allow_non_contiguous_dma(reason="p4a tiles"):
                sc.run(final_waits=st_steps[-4:])

    if upto <= 4 and not os.environ.get("K_P4B"):
        return nc

    NT4 = T // 128
    with ExitStack() as st:
        xs = [st.enter_context(nc.sbuf_tensor(f"xs{i}", [128, D], F32)) for i in range(2)]
        junk2 = st.enter_context(nc.sbuf_tensor("junk2", [128, D], BF16))
        xn2 = [st.enter_context(nc.sbuf_tensor(f"xn2_{i}", [128, D], BF16)) for i in range(2)]
        h2t = [st.enter_context(nc.sbuf_tensor(f"h2t{i}", [128, KC, 128], BF16)) for i in range(2)]
        wr = st.enter_context(nc.sbuf_tensor("wr", [128, KC, NE], F32))
        brt = st.enter_context(nc.sbuf_tensor("brt", [128, NE], F32))
        b2s = st.enter_context(nc.sbuf_tensor("b2s", [NE, D], F32))
        g2bc = st.enter_context(nc.sbuf_tensor("g2bc", [128, D], F32))
        ss2 = st.enter_context(nc.sbuf_tensor("ss2", [128, NT4], F32))
        rsA = st.enter_context(nc.sbuf_tensor("rsA", [128, NT4], F32))
        rsB = st.enter_context(nc.sbuf_tensor("rsB", [128, NT4], F32))
        rsC = st.enter_context(nc.sbuf_tensor("rsC", [128, NT4], F32))
        lgs = st.enter_context(nc.sbuf_tensor("lgs", [128, NE], F32))
        mx8 = st.enter_context(nc.sbuf_tensor("mx8", [128, 8], F32))
        nmx = st.enter_context(nc.sbuf_tensor("nmx", [128, 1], F32))
        msk = st.enter_context(nc.sbuf_tensor("msk", [128, NE], F32))
        exv = st.enter_context(nc.sbuf_tensor("exv", [128, NE], F32))
        exm = st.enter_context(nc.sbuf_tensor("exm", [128, NE], F32))
        ssum = st.enter_context(nc.sbuf_tensor("ssum", [128, 1], F32))
        rsum = st.enter_context(nc.sbuf_tensor("rsum", [128, 1], F32))
        gts = st.enter_context(nc.sbuf_tensor("gts", [128, NE], F32))
        tb = [st.enter_context(nc.sbuf_tensor(f"tb{i}", [128, 512], F32)) for i in range(2)]
        tp2 = [st.enter_context(nc.psum_tensor(f"tp2_{i}", [128, 4, 128], BF16)) for i in range(2)]
        lgp = st.enter_context(nc.psum_tensor("lgp", [128, NE], F32))
        gtp = st.enter_context(nc.psum_tensor("gtp", [NE, 128], F32))
        bp = [st.enter_context(nc.psum_tensor(f"bp{i}", [128, 512], F32)) for i in range(2)]
        sc = Sched(nc, st, "p4b")
        s_wr = sc.add("sync", lambda e: e.dma_start(out=wr[:], in_=w_router.rearrange("(k p) n -> p k n", p=128)), dma_key="wr")
        s_br = sc.add("sync", lambda e: e.dma_start(out=brt[:], in_=b_router[0:1, :].partition_broadcast(128)), dma_key="brt")
        s_b2 = sc.add("sync", lambda e: e.dma_start(out=b2s[:], in_=b2[:, :]), dma_key="b2s")
        s_g2 = sc.add("sync", lambda e: e.dma_start(out=g2bc[:], in_=mod_d[0:1, 5 * D:6 * D].partition_broadcast(128)), dma_key="g2bc")
        s_z = sc.add("vector", lambda e: e.memset(ss2[:], 0.0))
        store = {}; xn_s = {}; last_tr = {}; h2_free = {}; lg_s = None; gcp_s = None
        u2_hist = []
        nbt = 0; evt_hist = []
        nq = 0
        for i in range(NT4):
            b = i % 2
            r0 = i * 128
            s_ld = sc.add("sync", lambda e, b=b, r0=r0: e.dma_start(out=xs[b][:], in_=out[r0:r0 + 128, :]),
                          deps=[store.get(i - 2)], dma_key=f"xs{b}")
            s_sq = sc.add("scalar", lambda e, b=b, i=i: e.activation(out=junk2[:], in_=xs[b][:], func=AF.Square,
                          accum_out=ss2[:, i:i + 1]), deps=[s_ld, s_z])
            s_r1 = sc.add("vector", lambda e, i=i: e.tensor_scalar(out=rsA[:, i:i + 1], in0=ss2[:, i:i + 1], scalar1=1.0 / D,
                          scalar2=EPS, op0=ALU.mult, op1=ALU.add), deps=[s_sq])
            s_r2 = sc.add("scalar", lambda e, i=i: e.activation(out=rsB[:, i:i + 1], in_=rsA[:, i:i + 1], func=AF.Sqrt), deps=[s_r1])
            s_r3 = sc.add("vector", lambda e, i=i: e.reciprocal(out=rsC[:, i:i + 1], in_=rsB[:, i:i + 1]), deps=[s_r2])
            s_xn = sc.add("vector", lambda e, b=b, i=i: e.tensor_scalar(out=xn2[b][:], in0=xs[b][:], scalar1=rsC[:, i:i + 1],
                          scalar2=None, op0=ALU.mult), deps=[s_r3, last_tr.get(i - 2)])
            xn_s[i] = s_xn
            s_evt = None
            for bt in range(8):
                def trf(e, b=b, bt=bt, nbt=nbt):
                    for j in range(4):
                        kc = bt * 4 + j
                        tr = e.transpose(tp2[nbt % 2][:, j, :], xn2[b][:, kc * 128:(kc + 1) * 128], identb[:])
                    return tr
                s_tr = sc.add("tensor", trf, deps=[s_xn, evt_hist[nbt - 2] if nbt >= 2 else None])
                def evf(e, b=b, bt=bt, nbt=nbt):
                    for j in range(4):
                        kc = bt * 4 + j
                        ins = e.activation(out=h2t[b][:, kc, :], in_=tp2[nbt % 2][:, j, :], func=AF.Identity,
                                           bias=SH2[:, kc:kc + 1], scale=G2[:, kc:kc + 1])
                    return ins
                s_evt = sc.add("scalar", evf, deps=[s_tr] + (list(h2_free.get(i - 2, ())) if bt == 0 else []))
                evt_hist.append(s_evt)
                nbt += 1
            last_tr[i] = s_tr
            s_sth = sc.add("gpsimd", lambda e, b=b, r0=r0: e.dma_start(
                           out=h2T_d[:, :, r0:r0 + 128].rearrange("k p t -> p k t"), in_=h2t[b][:]),
                           deps=[s_evt], dma_key=f"h2t{b}")
            def rtf(e, b=b):
                for kc in range(KC):
                    mm = e.matmul(lgp[:], lhsT=h2t[b][:, kc, :], rhs=hi16(wr[:, kc, :]), start=(kc == 0), stop=(kc == KC - 1))
                return mm
            s_rt = sc.add("tensor", rtf, deps=[s_evt, s_wr, lg_s])
            s_lg = sc.add("vector", lambda e: e.tensor_tensor(out=lgs[:], in0=lgp[:], in1=brt[:], op=ALU.add), deps=[s_rt, s_br, gcp_s])
            lg_s = s_lg
            s_mx = sc.add("vector", lambda e: e.max(out=mx8[:], in_=lgs[:]), deps=[s_lg])
            s_nm = sc.add("vector", lambda e: e.tensor_scalar(out=nmx[:], in0=mx8[:, 0:1], scalar1=-1.0, scalar2=None, op0=ALU.mult), deps=[s_mx])
            s_mk = sc.add("vector", lambda e: e.tensor_scalar(out=msk[:], in0=lgs[:], scalar1=mx8[:, 3:4], scalar2=None, op0=ALU.is_ge), deps=[s_mx])
            s_ex = sc.add("scalar", lambda e: e.activation(out=exv[:], in_=lgs[:], func=AF.Exp, bias=nmx[:, 0:1], scale=1.0), deps=[s_nm])
            s_em = sc.add("vector", lambda e: e.tensor_tensor(out=exm[:], in0=exv[:], in1=msk[:], op=ALU.mult), deps=[s_ex, s_mk])
            s_sm = sc.add("vector", lambda e: e.reduce_sum(out=ssum[:], in_=exm[:], axis=AX.X), deps=[s_em])
            s_rc = sc.add("vector", lambda e: e.reciprocal(out=rsum[:], in_=ssum[:]), deps=[s_sm])
            s_gt = sc.add("vector", lambda e: e.tensor_scalar(out=gts[:], in0=exm[:], scalar1=rsum[:, 0:1], scalar2=None, op0=ALU.mult), deps=[s_rc])
            s_gtt = sc.add("tensor", lambda e: e.transpose(gtp[:], gts[:], identf[:]), deps=[s_gt, gcp_s])
            s_gcp = sc.add("vector", lambda e, r0=r0: e.tensor_copy(out=gatesT[:, r0:r0 + 128], in_=gtp[:]), deps=[s_gtt])
            gcp_s = s_gcp
            s_u2 = None
            for n8 in range(8):
                q = nq; nq += 1
                s_bm = sc.add("tensor", lambda e, q=q, r0=r0, n8=n8: e.matmul(bp[q % 2][:], lhsT=gatesT[:, r0:r0 + 128],
                              rhs=b2s[:, n8 * 512:(n8 + 1) * 512], start=True, stop=True),
                              deps=[s_gcp, s_b2, u2_hist[q - 2] if q >= 2 else None])
                s_u1 = sc.add("vector", lambda e, q=q, n8=n8: e.tensor_tensor(out=tb[q % 2][:], in0=bp[q % 2][:],
                              in1=g2bc[:, n8 * 512:(n8 + 1) * 512], op=ALU.mult), deps=[s_bm, s_g2])
                s_u2 = sc.add("vector", lambda e, q=q, n8=n8, b=b: e.tensor_tensor(out=xs[b][:, n8 * 512:(n8 + 1) * 512],
                              in0=tb[q % 2][:], in1=xs[b][:, n8 * 512:(n8 + 1) * 512], op=ALU.add), deps=[s_u1, s_xn, s_sq])
                u2_hist.append(s_u2)
            s_st = sc.add("gpsimd", lambda e, b=b, r0=r0: e.dma_start(out=out[r0:r0 + 128, :], in_=xs[b][:]),
                          deps=[s_u2], dma_key=f"xo{b}")
            store[i] = s_st
            h2_free[i] = (s_sth, s_rt)
            sth_last = s_sth
        s_gd = sc.add("gpsimd", lambda e: e.dma_start(out=gates_dbg[:, :], in_=gatesT[:]), deps=[gcp_s], dma_key="gdbg")
        with nc.allow_non_contiguous_dma(reason="p4b tiles"):
            sc.run(final_waits=[store[NT4 - 1], store[NT4 - 2], h2_free[NT4 - 1][0], h2_free[NT4 - 2][0], s_gd])

    if upto <= 4:
        return nc

    class Ring:
        def __init__(self, tiles):
            self.tiles = tiles; self.i = -1; self.users = [[] for _ in tiles]
        def acquire(self):
            self.i += 1
            k = self.i % len(self.tiles)
            deps = self.users[k]; self.users[k] = []
            return k, self.tiles[k], deps
        def use(self, k, step):
            self.users[k].append(step)

    TCH = 1024
    NCH = T // TCH
    NEXP = int(os.environ.get("K_NEXP", NE))
    with ExitStack() as st:
        sbt = lambda name, shape, dt=F32: st.enter_context(nc.sbuf_tensor(name, list(shape), dt))
        h2c = sbt("h2c", [128, KC, TCH], BF16)
        w1r = Ring([sbt(f"w1r{i}", [128, KC, 128]) for i in range(4)])
        w2r = Ring([sbt(f"w2r{i}", [128, 8, 256]) for i in range(2)])
        actT = sbt("actT", [128, 8, TCH], BF16)
        gbc = Ring([sbt(f"gbc{i}", [128, 512]) for i in range(4)])
        tglu = Ring([sbt(f"tglu{i}", [128, 512]) for i in range(2)])
        tsg = Ring([sbt(f"tsg{i}", [128, 512]) for i in range(2)])
        tlin = Ring([sbt(f"tlin{i}", [128, 512]) for i in range(2)])
        prev = Ring([sbt(f"prev{i}", [128, 256]) for i in range(4)])
        obuf = Ring([sbt(f"obuf{i}", [128, 256]) for i in range(4)])
        eselr = Ring([sbt(f"eselr{i}", [NE, 128]) for i in range(2)])
        b1r = Ring([sbt(f"b1r{i}", [128, 16]) for i in range(2)])
        pg = Ring([st.enter_context(nc.psum_tensor(f"pg{i}", [128, 512], F32)) for i in range(2)])
        pl = Ring([st.enter_context(nc.psum_tensor(f"pl{i}", [128, 512], F32)) for i in range(2)])
        gbp = Ring([st.enter_context(nc.psum_tensor("gbp0", [128, 512], F32))])
        py = Ring([st.enter_context(nc.psum_tensor(f"py{i}", [128, 256], F32)) for i in range(2)])
        sc = Sched(nc, st, "p5")
        w1_pref = {}

        def load_w1(ex, j):
            kwg, wg, dg = w1r.acquire()
            s_wg = sc.add("sync", lambda e, wg=wg, ex=ex, j=j: e.dma_start(out=wg[:],
                          in_=w1[ex, :, j * 128:(j + 1) * 128].rearrange("(k p) n -> p k n", p=128)),
                          deps=dg, dma_key=f"w1r{kwg}")
            w1r.use(kwg, s_wg)
            kwl, wl, dl = w1r.acquire()
            s_wl = sc.add("sync", lambda e, wl=wl, ex=ex, j=j: e.dma_start(out=wl[:],
                          in_=w1[ex, :, DFF + j * 128:DFF + (j + 1) * 128].rearrange("(k p) n -> p k n", p=128)),
                          deps=dl, dma_key=f"w1r{kwl}")
            w1r.use(kwl, s_wl)
            return kwg, wg, s_wg, kwl, wl, s_wl

        last_store = {}
        h2_users = []
        act_readers = []
        stores = []
        for c in range(NCH):
            t0 = c * TCH
            s_h2 = sc.add("sync", lambda e, t0=t0: e.dma_start(out=h2c[:], in_=h2T_d[:, :, t0:t0 + TCH].rearrange("k p t -> p k t")),
                          deps=h2_users[-1:], dma_key="h2c")
            for ex in range(NEXP):
                ke, et, ed = eselr.acquire()
                s_es = sc.add("sync", lambda e, et=et, ex=ex: e.dma_start(out=et[:], in_=esel_in[:, ex * 128:(ex + 1) * 128]),
                              deps=ed, dma_key=f"esel{ke}")
                eselr.use(ke, s_es)
                kb1, b1t, b1d = b1r.acquire()
                with nc.allow_non_contiguous_dma(reason="b1"):
                    pass
                s_b1 = sc.add("sync", lambda e, b1t=b1t, ex=ex: e.dma_start(out=b1t[:],
                              in_=b1[ex:ex + 1, :].rearrange("o (h p) -> p (o h)", p=128)), deps=b1d, dma_key=f"b1r{kb1}")
                b1r.use(kb1, s_b1)
                gb = []
                for tt in range(TCH // 512):
                    kp, pt, pd = gbp.acquire()
                    s_gm = sc.add("tensor", lambda e, pt=pt, et=et, t0=t0, tt=tt: e.matmul(pt[:], lhsT=et[:],
                                  rhs=gatesT[:, t0 + tt * 512:t0 + (tt + 1) * 512], start=True, stop=True), deps=[s_es] + pd)
                    eselr.use(ke, s_gm)
                    kg, gt_, gd = gbc.acquire()
                    s_gc = sc.add("vector", lambda e, gt_=gt_, pt=pt: e.tensor_copy(out=gt_[:], in_=pt[:]), deps=[s_gm] + gd)
                    gbp.use(kp, s_gc); gbc.use(kg, s_gc)
                    gb.append((kg, gt_, s_gc))
                first_act_deps = list(act_readers); act_readers = []
                for j in range(8):
                    if j == 0 and (c, ex) in w1_pref:
                        kwg, wg, s_wg, kwl, wl, s_wl = w1_pref.pop((c, ex))
                    else:
                        kwg, wg, s_wg, kwl, wl, s_wl = load_w1(ex, j)
                    for tt in range(TCH // 512):
                        cs_ = slice(tt * 512, (tt + 1) * 512)
                        kpg, pgt, pgd = pg.acquire()
                        def mmg(e, pgt=pgt, wg=wg, cs_=cs_):
                            for k in range(KC):
                                mm = e.matmul(pgt[:], lhsT=hi16(wg[:, k, :]), rhs=h2c[:, k, cs_], start=(k == 0), stop=(k == KC - 1))
                            return mm
                        s_mg = sc.add("tensor", mmg, deps=[s_wg, s_h2] + pgd)
                        w1r.use(kwg, s_mg)
                        kpl, plt, pld = pl.acquire()
                        def mml(e, plt=plt, wl=wl, cs_=cs_):
                            for k in range(KC):
                                mm = e.matmul(plt[:], lhsT=hi16(wl[:, k, :]), rhs=h2c[:, k, cs_], start=(k == 0), stop=(k == KC - 1))
                            return mm
                        s_ml = sc.add("tensor", mml, deps=[s_wl, s_h2] + pld)
                        w1r.use(kwl, s_ml)
                        h2_users.append(s_ml)
                        k1, g_t, d1 = tglu.acquire()
                        s_glu = sc.add("vector", lambda e, g_t=g_t, pgt=pgt, b1t=b1t, j=j: e.tensor_scalar(out=g_t[:], in0=pgt[:],
                                       scalar1=b1t[:, j:j + 1], scalar2=SW_LIMIT, op0=ALU.add, op1=ALU.min), deps=[s_mg, s_b1] + d1)
                        pg.use(kpg, s_glu); b1r.use(kb1, s_glu)
                        k2, s_t, d2 = tsg.acquire()
                        s_sg = sc.add("scalar", lambda e, s_t=s_t, g_t=g_t: e.activation(out=s_t[:], in_=g_t[:], func=AF.Sigmoid,
                                      scale=SW_ALPHA), deps=[s_glu] + d2)
                        k3, l_t, d3 = tlin.acquire()
                        s_l1 = sc.add("vector", lambda e, l_t=l_t, plt=plt, b1t=b1t, j=j: e.tensor_scalar(out=l_t[:], in0=plt[:],
                                      scalar1=b1t[:, 8 + j:9 + j], scalar2=SW_LIMIT, op0=ALU.add, op1=ALU.min), deps=[s_ml, s_b1] + d3)
                        pl.use(kpl, s_l1); b1r.use(kb1, s_l1)
                        s_l2 = sc.add("vector", lambda e, l_t=l_t: e.tensor_scalar(out=l_t[:], in0=l_t[:], scalar1=-SW_LIMIT,
                                      scalar2=1.0, op0=ALU.max, op1=ALU.add), deps=[s_l1])
                        s_m1 = sc.add("vector", lambda e, g_t=g_t, s_t=s_t: e.tensor_tensor(out=g_t[:], in0=g_t[:], in1=s_t[:],
                                      op=ALU.mult), deps=[s_sg])
                        tsg.use(k2, s_m1)
                        s_m2 = sc.add("vector", lambda e, g_t=g_t, l_t=l_t: e.tensor_tensor(out=g_t[:], in0=g_t[:], in1=l_t[:],
                                      op=ALU.mult), deps=[s_m1, s_l2])
                        tlin.use(k3, s_m2)
                        kg, gt_, s_gc = gb[tt]
                        s_act = sc.add("vector", lambda e, g_t=g_t, gt_=gt_, j=j, cs_=cs_: e.tensor_tensor(out=actT[:, j, cs_],
                                       in0=g_t[:], in1=gt_[:], op=ALU.mult), deps=[s_m2, s_gc] + first_act_deps)
                        first_act_deps = []
                        tglu.use(k1, s_act); gbc.use(kg, s_act)
                        last_act = s_act
                if ex + 1 < NEXP:
                    w1_pref[(c, ex + 1)] = load_w1(ex + 1, 0)
                elif c + 1 < NCH:
                    w1_pref[(c + 1, 0)] = load_w1(0, 0)
                for n16 in range(16):
                    kw2, w2t, d2w = w2r.acquire()
                    s_w2 = sc.add("sync", lambda e, w2t=w2t, ex=ex, n16=n16: e.dma_start(out=w2t[:],
                                  in_=w2[ex, :, n16 * 256:(n16 + 1) * 256].rearrange("(j p) n -> p j n", p=128)),
                                  deps=d2w, dma_key=f"w2r{kw2}")
                    w2r.use(kw2, s_w2)
                    for ts in range(TCH // 128):
                        r0 = t0 + ts * 128
                        region = (r0, n16)
                        first = (ex == 0)
                        if not first:
                            kpv, pv, pvd = prev.acquire()
                            s_pv = sc.add("scalar", lambda e, pv=pv, r0=r0, n16=n16: e.dma_start(out=pv[:],
                                          in_=moe_d[r0:r0 + 128, n16 * 256:(n16 + 1) * 256]),
                                          deps=pvd + [last_store[region]], dma_key=f"prev{kpv}")
                        kpy, pyt, pyd = py.acquire()
                        def mmy(e, pyt=pyt, w2t=w2t, ts=ts):
                            for j in range(8):
                                mm = e.matmul(pyt[:], lhsT=actT[:, j, ts * 128:(ts + 1) * 128], rhs=hi16(w2t[:, j, :]),
                                              start=(j == 0), stop=(j == 7))
                            return mm
                        s_my = sc.add("tensor", mmy, deps=[s_w2, last_act] + pyd)
                        w2r.use(kw2, s_my)
                        act_readers.append(s_my)
                        if len(act_readers) > 2:
                            act_readers = act_readers[-2:]
                        ko, ot, od = obuf.acquire()
                        if first:
                            s_o = sc.add("vector", lambda e, ot=ot, pyt=pyt: e.tensor_copy(out=ot[:], in_=pyt[:]), deps=[s_my] + od)
                        else:
                            s_o = sc.add("vector", lambda e, ot=ot, pyt=pyt, pv=pv: e.tensor_tensor(out=ot[:], in0=pyt[:], in1=pv[:],
                                         op=ALU.add), deps=[s_my, s_pv] + od)
                            prev.use(kpv, s_o)
                        py.use(kpy, s_o)
                        s_st = sc.add("gpsimd", lambda e, ot=ot, r0=r0, n16=n16: e.dma_start(
                                      out=moe_d[r0:r0 + 128, n16 * 256:(n16 + 1) * 256], in_=ot[:]), deps=[s_o], dma_key=f"obuf{ko}")
                        obuf.use(ko, s_st)
                        last_store[region] = s_st
                        stores.append(s_st)
        with nc.allow_non_contiguous_dma(reason="p5 tiles"):
            sc.run(final_waits=stores[-4:])

    with ExitStack() as st:
        g2b = st.enter_context(nc.sbuf_tensor("g2b6", [128, D], F32))
        oi = Ring([st.enter_context(nc.sbuf_tensor(f"oi{i}", [128, D], F32)) for i in range(2)])
        ma = Ring([st.enter_context(nc.sbuf_tensor(f"ma{i}", [128, D], F32)) for i in range(2)])
        sc = Sched(nc, st, "p6")
        s_g2 = sc.add("sync", lambda e: e.dma_start(out=g2b[:], in_=mod_d[0:1, 5 * D:6 * D].partition_broadcast(128)), dma_key="g2b")
        fin = []
        for i in range(T // 128):
            r0 = i * 128
            ko, ot, od = oi.acquire()
            s_lo = sc.add("sync", lambda e, ot=ot, r0=r0: e.dma_start(out=ot[:], in_=out[r0:r0 + 128, :]), deps=od, dma_key=f"oi{ko}")
            km, mt, md = ma.acquire()
            s_lm = sc.add("sync", lambda e, mt=mt, r0=r0: e.dma_start(out=mt[:], in_=moe_d[r0:r0 + 128, :]), deps=md, dma_key=f"ma{km}")
            s_a = sc.add("vector", lambda e, mt=mt: e.tensor_tensor(out=mt[:], in0=mt[:], in1=g2b[:], op=ALU.mult), deps=[s_lm, s_g2])
            s_b = sc.add("vector", lambda e, mt=mt, ot=ot: e.tensor_tensor(out=ot[:], in0=ot[:], in1=mt[:], op=ALU.add), deps=[s_a, s_lo])
            ma.use(km, s_b)
            s_st = sc.add("gpsimd", lambda e, ot=ot, r0=r0: e.dma_start(out=out[r0:r0 + 128, :], in_=ot[:]), deps=[s_b], dma_key=f"oo{ko}")
            oi.use(ko, s_st)
            fin.append(s_st)
        with nc.allow_non_contiguous_dma(reason="p6"):
            sc.run(final_waits=fin[-2:])
    return nc


def _finish_dummy(nc, out):
    with ExitStack() as st:
        z = st.enter_context(nc.sbuf_tensor("zz", [128, D], F32))
        s = Cnt(nc, st, "zz_s"); s2 = Cnt(nc, st, "zz_s2")
        with nc.Block() as blk:
            @blk.vector
            def _(e):
                e.memset(z[:], 0.0).then_inc(s.s, 1)

            @blk.gpsimd
            def _(e):
                e.wait_ge(s.s, 1)
                for i in range(T // 128):
                    e.dma_start(out=out[i * 128:(i + 1) * 128, :], in_=z[:]).then_inc(s2.s, 16)
                e.wait_ge(s2.s, 16 * (T // 128))
        clear_sems(nc)


def make_in_maps(inputs, cores, names=None):
    rot, identb, identf, esel = _consts()
    x = np.asarray(inputs["x"])[0]
    shapes = {"c": (1, D), "b_mod": (1, 6 * D), "norm1_g": (1, D), "q_norm_g": (1, 128), "k_norm_g": (1, 128),
              "pool_scale": (1, 2048), "norm2_g": (1, D), "b_router": (1, NE)}
    shared = {"rotm": rot, "identb": identb, "identf": identf, "onesb": np.ones((128, 128), ml_dtypes.bfloat16), "esel": esel.reshape(32, 32 * 128)}
    for k, v in inputs.items():
        if k == "x" or (names is not None and k not in names):
            continue
        a = np.asarray(v)
        shared[k] = a.reshape(shapes[k]) if k in shapes else np.ascontiguousarray(a[0])
        if k in ("w1", "w2") and os.environ.get("K_NEXP"):
            shared[k] = np.ascontiguousarray(shared[k][:int(os.environ["K_NEXP"])])
    maps = []
    for core in cores:
        C, Sn, pm, ic = _tables(core)
        m = dict(shared)
        m["x"] = np.ascontiguousarray(np.roll(x, -core * T, axis=0))
        m["ropeC"] = C; m["ropeS"] = Sn; m["pmask"] = pm; m["invcnt"] = ic
        if names is not None:
            m = {k: v for k, v in m.items() if k in names}
        maps.append(m)
    return maps


def kernel(**inputs):
    nc = build()
    maps = make_in_maps(inputs, list(range(NC)))
    res = run_bass_kernel_spmd(nc, maps, core_ids=list(range(NC)))
    outs = [res.results[i]["out"] for i in range(NC)]
    return np.concatenate(outs, axis=0).reshape(1, S, D).astype(np.float32)
```

```python
import os
import numpy as np
from contextlib import ExitStack
import ml_dtypes
import concourse.bass as bass
import concourse.mybir as mybir
from concourse.bass_utils import run_bass_kernel_spmd

F32 = mybir.dt.float32
BF16 = mybir.dt.bfloat16
ALU = mybir.AluOpType
AF = mybir.ActivationFunctionType
AX = mybir.AxisListType

NC = 8
SKIP = set(filter(None, os.environ.get("K_SKIP", "").split(",")))
S = 8192
D = 4096
T = S // NC
HALO = 8
NG = S // 512
NOWN = T // 512
KC = D // 128
EPS = 1e-6
NE = 32
DFF = 1024
SW_ALPHA = 1.702
SW_LIMIT = 7.0


def _tables(core):
    tok = (core * T + np.arange(S)) % S
    row = (tok // 64).astype(np.float32)
    col = (tok % 64).astype(np.float32)
    inv_freq = (10000.0 ** (-np.arange(0, 64, 2, dtype=np.float32) / 64.0)).astype(np.float32)
    ang_r = row[None, :] * inv_freq[:, None]
    ang_c = col[None, :] * inv_freq[:, None]
    C = np.concatenate([np.cos(ang_r), np.cos(ang_r), np.cos(ang_c), np.cos(ang_c)], 0).astype(np.float32)
    Sn = np.concatenate([np.sin(ang_r), np.sin(ang_r), np.sin(ang_c), np.sin(ang_c)], 0).astype(np.float32)
    t_ext = core * T - HALO + np.arange(T + 2 * HALO)
    valid = ((t_ext >= 0) & (t_ext < S)).astype(np.float32)
    pmask = np.broadcast_to(valid[None, :], (128, T + 2 * HALO)).copy()
    t_own = core * T + np.arange(T)
    inv = np.zeros((4, T), np.float32)
    for gi, w in enumerate((2, 4, 8, 16)):
        lo = np.clip(t_own - w // 2, 0, S - 1)
        hi = np.clip(t_own + w // 2 - 1, 0, S - 1)
        inv[gi] = 1.0 / (hi - lo + 1).astype(np.float32)
    invcnt = np.broadcast_to(inv[:, None, :], (4, 128, T)).copy()
    return C, Sn, pmask, invcnt


def _consts():
    rot = np.zeros((128, 128), np.float32)
    for m in range(128):
        if (m % 64) < 32:
            rot[m + 32, m] = -1.0
        else:
            rot[m - 32, m] = 1.0
    ident = np.eye(128, dtype=np.float32)
    esel = np.zeros((32, 32, 128), np.float32)
    for e in range(32):
        esel[e, e, :] = 1.0
    return rot.astype(ml_dtypes.bfloat16), ident.astype(ml_dtypes.bfloat16), ident, esel


_G = {"st0": None, "pool": [], "resid": {}}


class Cnt:
    def __init__(self, nc, st, name):
        self.s = _G["st0"].enter_context(nc.semaphore(name))
        self.n = 0


def clear_sems(nc, sems=None):
    return


def hi16(ap):
    v = ap.bitcast(BF16)
    return v[:, 1::2]


class Sched:
    ENG = ("sync", "scalar", "vector", "tensor", "gpsimd")

    def __init__(self, nc, st, name):
        self.nc = nc; self.st = st; self.name = name
        self.steps = []
        self.sems = {}
        self.counts = {}

    def _sem(self, key):
        if key not in self.sems:
            h = _G["pool"].pop(0)
            self.sems[key] = h
            self.counts[key] = _G["resid"][h.num]
        return self.sems[key]

    def add(self, owner, fn, deps=(), dma_key=None):
        key = f"d_{dma_key}" if dma_key is not None else f"e_{owner}"
        self._sem(key)
        self.counts[key] += 16 if dma_key is not None else 1
        self.steps.append((owner, fn, tuple(d for d in deps if d is not None), key, self.counts[key]))
        return len(self.steps) - 1

    def run(self, final_waits=()):
        nc = self.nc
        steps = self.steps
        trunc = os.environ.get("K_TRUNC_" + self.name)
        if trunc is not None:
            n = int(trunc)
            steps = steps[:n]
            self.steps = steps
            final_waits = [i for i in range(max(0, n - 12), n)]
        with nc.Block() as blk:
            for owner in self.ENG:
                mine = [i for i, s in enumerate(steps) if s[0] == owner]
                if not mine and not (owner == "gpsimd" and final_waits):
                    continue
                def body(e, owner=owner, mine=mine):
                    waited = {}
                    for i in mine:
                        _, fn, deps, key, cnt = steps[i]
                        for d in deps:
                            dk, dc = steps[d][3], steps[d][4]
                            if waited.get(dk, 0) < dc:
                                e.wait_ge(self.sems[dk], dc)
                                waited[dk] = dc
                        ins = fn(e)
                        ins.then_inc(self.sems[key], 16 if key.startswith("d_") else 1)
                    if owner == "gpsimd":
                        for d in final_waits:
                            e.wait_ge(self.sems[steps[d][3]], steps[d][4])
                getattr(blk, owner)(body)
        for key, h in self.sems.items():
            _G["resid"][h.num] = self.counts[key]
            _G["pool"].append(h)


def build(upto=99, debug_outs=False):
    nc = bass.Bass("TRN2", target_bir_lowering=False)
    declared = []
    nc._declared_inputs = declared
    def di(name, shape, dt=F32, need=0):
        if upto < need:
            return None
        declared.append(name)
        return nc.dram_tensor(name, list(shape), dt, kind="ExternalInput").ap()
    x = di("x", [S, D])
    c_in = di("c", [1, D])
    w_mod = di("w_mod", [D, 6 * D])
    b_mod = di("b_mod", [1, 6 * D])
    norm1_g = di("norm1_g", [1, D])
    w_in = di("w_in", [D, 5120])
    q_norm_g = di("q_norm_g", [1, 128])
    k_norm_g = di("k_norm_g", [1, 128])
    w_pool = di("w_pool", [4, 512, 512], need=3)
    pool_scale = di("pool_scale", [1, 2048], need=3)
    w_out = di("w_out", [D, D], need=4)
    norm2_g = di("norm2_g", [1, D])
    w_router = di("w_router", [D, NE], need=4)
    b_router = di("b_router", [1, NE], need=4)
    NEd = int(os.environ.get("K_NEXP", NE))
    w1 = di("w1", [NEd, D, 2 * DFF], need=5)
    b1 = di("b1", [NE, 2 * DFF], need=5)
    w2 = di("w2", [NEd, DFF, D], need=5)
    b2 = di("b2", [NE, D], need=4)
    ropeC = di("ropeC", [128, S], need=2)
    ropeS = di("ropeS", [128, S], need=2)
    pmask = di("pmask", [128, T + 2 * HALO], need=3)
    invcnt = di("invcnt", [4, 128, T], need=3)
    rot_in = di("rotm", [128, 128], BF16)
    identb_in = di("identb", [128, 128], BF16)
    identf_in = di("identf", [128, 128])
    onesb_in = di("onesb", [128, 128], BF16)
    esel_in = di("esel", [32, 32 * 128], need=4)

    out = nc.dram_tensor("out", [T, D], F32, kind="ExternalOutput").ap()

    kind_dbg = "ExternalOutput" if debug_outs else "Internal"
    mod_d = nc.dram_tensor("mod_d", [1, 6 * D], F32, kind=kind_dbg).ap()
    qraw_d = nc.dram_tensor("qraw_d", [16, 128, T], F32, kind=kind_dbg).ap()
    kraw_d = nc.dram_tensor("kraw_d", [4, 128, S], F32, kind=kind_dbg).ap()
    v_d = nc.dram_tensor("v_d", [4, 128, S // 128, 128], BF16, kind=kind_dbg).ap()
    pi_d = nc.dram_tensor("pi_d", [16, 128, T + 2 * HALO], F32, kind=kind_dbg).ap()
    qT_d = nc.dram_tensor("qT_d", [16, 128, T], BF16).ap()
    kT_d = nc.dram_tensor("kT_d", [4, 128, S], BF16).ap()
    mixT_d = nc.dram_tensor("mixT_d", [32, 128, T], BF16, kind=kind_dbg).ap()
    h2T_d = nc.dram_tensor("h2T_d", [32, 128, T], BF16, kind=kind_dbg).ap()
    lgs_d = nc.dram_tensor("lgs_d", [1, 16], F32).ap()
    moe_d = nc.dram_tensor("moe_d", [T, D], F32, kind=kind_dbg).ap()
    gates_dbg = nc.dram_tensor("gates_dbg", [32, T], F32, kind=kind_dbg).ap()

    st0 = ExitStack()
    _G["st0"] = st0
    _G["pool"] = [st0.enter_context(nc.semaphore(f"pool{i}")) for i in range(52)]
    _G["resid"] = {h.num: 0 for h in _G["pool"]}
    sb = lambda name, shape, dt=F32: st0.enter_context(nc.sbuf_tensor(name, list(shape), dt))
    G1 = sb("G1", [128, 32]); SH1 = sb("SH1", [128, 32])
    G2 = sb("G2", [128, 32]); SH2 = sb("SH2", [128, 32])
    identb = sb("identb_s", [128, 128], BF16)
    identf = sb("identf_s", [128, 128])
    rotm = sb("rotm_s", [128, 128], BF16)
    onesb = sb("onesb_s", [128, 128], BF16)
    qng = sb("qng", [128, 1]); kng = sb("kng", [128, 1])
    gatesT = sb("gatesT", [32, T])

    cT = sb("cT", [128, KC]); cact = sb("cact", [128, KC])
    HM = 3 * D
    WM = 384
    for half in range(2):
        with ExitStack() as st:
            wst = [st.enter_context(nc.sbuf_tensor(f"wmod{half}_{i}", [128, KC, WM], F32)) for i in range(2)]
            mo = st.enter_context(nc.sbuf_tensor(f"mo{half}", [1, HM], F32))
            bm = st.enter_context(nc.sbuf_tensor(f"bm{half}", [1, HM], F32))
            ps = [st.enter_context(nc.psum_tensor(f"pm{half}_{i}", [128, 512], F32)) for i in range(2)]
            ld = Cnt(nc, st, f"p0_ld{half}"); wf = Cnt(nc, st, f"p0_wf{half}")
            ca = Cnt(nc, st, f"p0_ca{half}"); pd = Cnt(nc, st, f"p0_pd{half}"); pfree = Cnt(nc, st, f"p0_pfree{half}")
            stc = Cnt(nc, st, f"p0_st{half}")
            NT = HM // WM
            nld = 8 if half == 0 else 1
            with nc.Block() as blk:
                @blk.sync
                def _(e):
                    if half == 0:
                        with nc.allow_non_contiguous_dma(reason="small param transposes"):
                            e.dma_start(out=cT[:], in_=c_in.rearrange("o (k p) -> p (o k)", p=128)).then_inc(ld.s, 16)
                            e.dma_start(out=qng[:], in_=q_norm_g.rearrange("o p -> p o")).then_inc(ld.s, 16)
                            e.dma_start(out=kng[:], in_=k_norm_g.rearrange("o p -> p o")).then_inc(ld.s, 16)
                        e.dma_start(out=identb[:], in_=identb_in[:, :]).then_inc(ld.s, 16)
                        e.dma_start(out=identf[:], in_=identf_in[:, :]).then_inc(ld.s, 16)
                        e.dma_start(out=rotm[:], in_=rot_in[:, :]).then_inc(ld.s, 16)
                        e.dma_start(out=onesb[:], in_=onesb_in[:, :]).then_inc(ld.s, 16)
                    e.dma_start(out=bm[:], in_=b_mod[:, half * HM:(half + 1) * HM]).then_inc(ld.s, 16)
                    for i in range(NT):
                        if i >= 2:
                            e.wait_ge(pfree.s, i - 1)
                        c0 = half * HM + i * WM
                        e.dma_start(out=wst[i % 2][:],
                                    in_=w_mod[:, c0:c0 + WM].rearrange("(k p) n -> p k n", p=128)
                                    ).then_inc(wf.s, 16)

                @blk.scalar
                def _(e):
                    e.wait_ge(ld.s, 16 * nld)
                    if half == 0:
                        e.activation(out=cact[:], in_=cT[:], func=AF.Silu).then_inc(ca.s, 1)

                @blk.tensor
                def _(e):
                    if half == 0:
                        e.wait_ge(ca.s, 1)
                    for i in range(NT):
                        e.wait_ge(wf.s, 16 * (i + 1))
                        if i >= 2:
                            e.wait_ge(pfree.s, i - 1)
                        for k in range(KC):
                            mm = e.matmul(ps[i % 2][0:1, 0:WM], lhsT=cact[:, k:k + 1], rhs=wst[i % 2][:, k, :],
                                          start=(k == 0), stop=(k == KC - 1))
                        mm.then_inc(pd.s, 1)

                @blk.vector
                def _(e):
                    e.wait_ge(ld.s, 16 * nld)
                    for i in range(NT):
                        e.wait_ge(pd.s, i + 1)
                        e.tensor_tensor(out=mo[0:1, i * WM:(i + 1) * WM], in0=ps[i % 2][0:1, 0:WM],
                                        in1=bm[0:1, i * WM:(i + 1) * WM], op=ALU.add).then_inc(pfree.s, 1)

                @blk.gpsimd
                def _(e):
                    e.wait_ge(pfree.s, NT)
                    e.dma_start(out=mod_d[:, half * HM:(half + 1) * HM], in_=mo[:]).then_inc(stc.s, 16)
                    e.wait_ge(stc.s, 16)
            clear_sems(nc)

    with ExitStack() as st:
        modF = st.enter_context(nc.sbuf_tensor("modF", [128, 192], F32))
        n1g = st.enter_context(nc.sbuf_tensor("n1g", [128, 32], F32))
        n2g = st.enter_context(nc.sbuf_tensor("n2g", [128, 32], F32))
        ld = Cnt(nc, st, "p0b_ld"); dv = Cnt(nc, st, "p0b_dv")
        with nc.Block() as blk:
            @blk.sync
            def _(e):
                with nc.allow_non_contiguous_dma(reason="small param transposes"):
                    e.dma_start(out=modF[:], in_=mod_d.rearrange("o (j p) -> p (o j)", p=128)).then_inc(ld.s, 16)
                    e.dma_start(out=n1g[:], in_=norm1_g.rearrange("o (j p) -> p (o j)", p=128)).then_inc(ld.s, 16)
                    e.dma_start(out=n2g[:], in_=norm2_g.rearrange("o (j p) -> p (o j)", p=128)).then_inc(ld.s, 16)

            @blk.vector
            def _(e):
                e.wait_ge(ld.s, 48)
                e.scalar_tensor_tensor(out=G1[:], in0=modF[:, 32:64], scalar=1.0, in1=n1g[:],
                                       op0=ALU.add, op1=ALU.mult)
                e.scalar_tensor_tensor(out=G2[:], in0=modF[:, 128:160], scalar=1.0, in1=n2g[:],
                                       op0=ALU.add, op1=ALU.mult)
                e.tensor_copy(out=SH1[:], in_=modF[:, 0:32])
                e.tensor_copy(out=SH2[:], in_=modF[:, 96:128]).then_inc(dv.s, 1)
                e.wait_ge(dv.s, 1)
        clear_sems(nc)

    if upto <= 0:
        _finish_dummy(nc, out)
        return nc

    if "1" not in SKIP:
        def blocks_for(g):
            if g < NOWN:
                return list(range(20))
            if g == NOWN or g == NG - 1:
                return list(range(8)) + [16, 17, 18, 19]
            return [16, 17, 18, 19]

        with ExitStack() as st:
            xb = [st.enter_context(nc.sbuf_tensor(f"xb{i}", [128, D], F32)) for i in range(2)]
            xn = [st.enter_context(nc.sbuf_tensor(f"xn{i}", [128, D], BF16)) for i in range(2)]
            hT = [st.enter_context(nc.sbuf_tensor(f"hT{i}", [128, KC, 512], BF16)) for i in range(2)]
            wb = [st.enter_context(nc.sbuf_tensor(f"wb{i}", [128, KC, 256], F32)) for i in range(2)]
            ob = [st.enter_context(nc.sbuf_tensor(f"ob{i}", [128, 512], F32)) for i in range(4)]
            NTILE = S // 128
            ss = st.enter_context(nc.sbuf_tensor("ss", [128, NTILE], F32))
            rs = st.enter_context(nc.sbuf_tensor("rs", [128, NTILE], F32))
            rs2 = st.enter_context(nc.sbuf_tensor("rs2", [128, NTILE], F32))
            sq = st.enter_context(nc.sbuf_tensor("sq", [128, NTILE], F32))
            sq_done = Cnt(nc, st, "sq_done")
            junk = st.enter_context(nc.sbuf_tensor("junk", [128, D], BF16))
            tp = [st.enter_context(nc.psum_tensor(f"tp{i}", [128, 4, 128], BF16)) for i in range(2)]
            acc = [st.enter_context(nc.psum_tensor(f"acc{i}", [128, 512], F32)) for i in range(2)]
            z_done = Cnt(nc, st, "z_done")
            x_full = Cnt(nc, st, "x_full")
            ss_done = Cnt(nc, st, "ss_done")
            rs_done = Cnt(nc, st, "rs_done")
            xn_done = Cnt(nc, st, "xn_done")
            tp_done = Cnt(nc, st, "tp_done")
            tp_free = Cnt(nc, st, "tp_free")
            w_full = Cnt(nc, st, "w_full")
            acc_done = Cnt(nc, st, "acc_done")
            ev_done = Cnt(nc, st, "ev_done")
            o_free = Cnt(nc, st, "o_free")

            sched = []
            blk_end_chunk = []
            grp_end_chunk = []
            for g in range(NG):
                for b in blocks_for(g):
                    if b >= 18:
                        for sub in range(4):
                            sched.append((g, b, 'V', sub))
                    else:
                        for m in range(2):
                            sched.append((g, b, 'F', m))
                    blk_end_chunk.append(len(sched))
                grp_end_chunk.append(len(sched))

            with nc.Block() as blk:
                @blk.sync
                def _(e):
                    bi = 0
                    def load_x(g):
                        for sub in range(4):
                            i = g * 4 + sub
                            if i >= 2:
                                e.wait_ge(xn_done.s, i - 1)
                            e.dma_start(out=xb[i % 2][:], in_=x[i * 128:(i + 1) * 128, :]).then_inc(x_full.s, 16)
                    def load_w(g):
                        nonlocal bi
                        for b in blocks_for(g):
                            if bi >= 2:
                                e.wait_ge(acc_done.s, blk_end_chunk[bi - 2])
                            e.dma_start(out=wb[bi % 2][:],
                                        in_=w_in[:, b * 256:(b + 1) * 256].rearrange("(k p) n -> p k n", p=128)
                                        ).then_inc(w_full.s, 16)
                            bi += 1
                    load_x(0)
                    for g in range(1, NG):
                        load_x(g)
                        load_w(g - 1)
                    load_w(NG - 1)

                @blk.scalar
                def _(e):
                    nb = 0
                    e.wait_ge(z_done.s, 1)
                    for i in range(NTILE):
                        g = i // 4; sub = i % 4
                        e.wait_ge(x_full.s, 16 * (i + 1))
                        e.activation(out=junk[:], in_=xb[i % 2][:], func=AF.Square,
                                     accum_out=ss[:, i:i + 1]).then_inc(ss_done.s, 1)
                        e.wait_ge(rs_done.s, 2 * i + 1)
                        e.activation(out=sq[:, i:i + 1], in_=rs[:, i:i + 1], func=AF.Sqrt).then_inc(sq_done.s, 1)
                        if sub == 0 and g >= 2:
                            e.wait_ge(acc_done.s, grp_end_chunk[g - 2])
                        for bt in range(8):
                            e.wait_ge(tp_done.s, nb + 1)
                            for j in range(4):
                                kc = bt * 4 + j
                                ins = e.activation(out=hT[g % 2][:, kc, sub * 128:(sub + 1) * 128],
                                                   in_=tp[nb % 2][:, j, :], func=AF.Identity,
                                                   bias=SH1[:, kc:kc + 1], scale=G1[:, kc:kc + 1])
                            ins.then_inc(tp_free.s, 1)
                            nb += 1

                @blk.vector
                def _(e):
                    e.memset(ss[:], 0.0).then_inc(z_done.s, 1)
                    ci = 0
                    def evac_chunks(upto_ci):
                        nonlocal ci
                        while ci < upto_ci:
                            g, b, kind, sidx = sched[ci]
                            e.wait_ge(acc_done.s, ci + 1)
                            if ci >= 4:
                                e.wait_ge(o_free.s, 16 * (ci - 3))
                            if kind == 'V':
                                ins = e.tensor_copy(out=ob[ci % 4][:, 0:128].bitcast(BF16),
                                                    in_=acc[ci % 2][:, 0:256])
                            else:
                                ins = e.tensor_copy(out=ob[ci % 4][:], in_=acc[ci % 2][:])
                            ins.then_inc(ev_done.s, 1)
                            ci += 1
                    for i in range(NTILE):
                        g = i // 4
                        e.wait_ge(ss_done.s, i + 1)
                        col = slice(i, i + 1)
                        e.tensor_scalar(out=rs[:, col], in0=ss[:, col], scalar1=1.0 / D, scalar2=EPS,
                                        op0=ALU.mult, op1=ALU.add).then_inc(rs_done.s, 1)
                        e.wait_ge(sq_done.s, i + 1)
                        e.reciprocal(out=rs2[:, col], in_=sq[:, col]).then_inc(rs_done.s, 1)
                        e.wait_ge(rs_done.s, 2 * i + 2)
                        if i >= 2:
                            e.wait_ge(tp_done.s, 8 * (i - 1))
                        e.tensor_scalar(out=xn[i % 2][:], in0=xb[i % 2][:], scalar1=rs2[:, col], scalar2=None,
                                        op0=ALU.mult).then_inc(xn_done.s, 1)
                        if i % 4 == 3 and g >= 1:
                            evac_chunks(grp_end_chunk[g - 1])
                    evac_chunks(len(sched))

                @blk.tensor
                def _(e):
                    nb = 0
                    ci = 0
                    bi = 0
                    def proj_group(g):
                        nonlocal ci, bi
                        e.wait_ge(tp_free.s, 32 * (g + 1))
                        for b in blocks_for(g):
                            e.wait_ge(w_full.s, 16 * (bi + 1))
                            w = wb[bi % 2]
                            if b >= 18:
                                for sub in range(4):
                                    if ci >= 2:
                                        e.wait_ge(ev_done.s, ci - 1)
                                    for k in range(KC):
                                        mm = e.matmul(acc[ci % 2][:, 0:256],
                                                      lhsT=hT[g % 2][:, k, sub * 128:(sub + 1) * 128],
                                                      rhs=hi16(w[:, k, :]), start=(k == 0), stop=(k == KC - 1))
                                    mm.then_inc(acc_done.s, 1)
                                    ci += 1
                            else:
                                for m in range(2):
                                    if ci >= 2:
                                        e.wait_ge(ev_done.s, ci - 1)
                                    for k in range(KC):
                                        mm = e.matmul(acc[ci % 2][:, :],
                                                      lhsT=hi16(w[:, k, m * 128:(m + 1) * 128]),
                                                      rhs=hT[g % 2][:, k, :], start=(k == 0), stop=(k == KC - 1))
                                    mm.then_inc(acc_done.s, 1)
                                    ci += 1
                            bi += 1

                    for i in range(NTILE):
                        g = i // 4
                        e.wait_ge(xn_done.s, i + 1)
                        for bt in range(8):
                            if nb >= 2:
                                e.wait_ge(tp_free.s, nb - 1)
                            for j in range(4):
                                kc = bt * 4 + j
                                tr = e.transpose(tp[nb % 2][:, j, :], xn[i % 2][:, kc * 128:(kc + 1) * 128], identb[:])
                            tr.then_inc(tp_done.s, 1)
                            nb += 1
                        if i % 4 == 3 and g >= 1:
                            proj_group(g - 1)
                    proj_group(NG - 1)

                @blk.gpsimd
                def _(e):
                    with nc.allow_non_contiguous_dma(reason="scratch layouts"):
                        for ci, (g, b, kind, sidx) in enumerate(sched):
                            e.wait_ge(ev_done.s, ci + 1)
                            if kind == 'V':
                                half = b - 18
                                chunk = g * 4 + sidx
                                src = ob[ci % 4][:, 0:128].bitcast(BF16)
                                e.dma_start(out=v_d[half * 2:half * 2 + 2, :, chunk, :].rearrange("h p d -> p h d"),
                                            in_=src.rearrange("p (h d) -> p h d", h=2)).then_inc(o_free.s, 16)
                            else:
                                f = b * 2 + sidx
                                if f < 16:
                                    if g < NOWN:
                                        e.dma_start(out=pi_d[f, :, HALO + g * 512:HALO + (g + 1) * 512],
                                                    in_=ob[ci % 4][:]).then_inc(o_free.s, 16)
                                    elif g == NOWN:
                                        e.dma_start(out=pi_d[f, :, HALO + T:HALO + T + HALO],
                                                    in_=ob[ci % 4][:, 0:HALO]).then_inc(o_free.s, 16)
                                    else:
                                        e.dma_start(out=pi_d[f, :, 0:HALO],
                                                    in_=ob[ci % 4][:, 512 - HALO:512]).then_inc(o_free.s, 16)
                                elif f < 32:
                                    e.dma_start(out=qraw_d[f - 16, :, g * 512:(g + 1) * 512],
                                                in_=ob[ci % 4][:]).then_inc(o_free.s, 16)
                                else:
                                    e.dma_start(out=kraw_d[f - 32, :, g * 512:(g + 1) * 512],
                                                in_=ob[ci % 4][:]).then_inc(o_free.s, 16)
                        e.wait_ge(o_free.s, 16 * len(sched))
            clear_sems(nc)

    if upto <= 1:
        _finish_dummy(nc, out)
        return nc

    if "1b" not in SKIP:
        items = []
        for g in range(NG):
            for kv in range(4):
                items.append(('k', kv, g))
            if g < NOWN:
                for h in range(16):
                    items.append(('q', h, g))
        NI = len(items)
        grp_first = {}
        for n, (kind, idx, g) in enumerate(items):
            grp_first.setdefault(g, n)
        with ExitStack() as st:
            ib = [st.enter_context(nc.sbuf_tensor(f"ib{i}", [128, 512], F32)) for i in range(2)]
            cs = [st.enter_context(nc.sbuf_tensor(f"cs{i}", [128, 2, 512], F32)) for i in range(2)]
            sqv = [st.enter_context(nc.sbuf_tensor(f"sqv{i}", [128, 512], BF16)) for i in range(2)]
            yg = [st.enter_context(nc.sbuf_tensor(f"yg{i}", [128, 512], BF16)) for i in range(2)]
            t0 = [st.enter_context(nc.sbuf_tensor(f"t0_{i}", [128, 512], F32)) for i in range(2)]
            t1 = [st.enter_context(nc.sbuf_tensor(f"t1_{i}", [128, 512], F32)) for i in range(2)]
            rstd = [st.enter_context(nc.sbuf_tensor(f"rstd{i}", [128, 512], F32)) for i in range(2)]
            u1 = [st.enter_context(nc.sbuf_tensor(f"u1_{i}", [128, 512], F32)) for i in range(2)]
            u2 = [st.enter_context(nc.sbuf_tensor(f"u2_{i}", [128, 512], F32)) for i in range(2)]
            u3 = [st.enter_context(nc.sbuf_tensor(f"u3_{i}", [128, 512], F32)) for i in range(2)]
            outb = [st.enter_context(nc.sbuf_tensor(f"outb{i}", [128, 512], BF16)) for i in range(2)]
            p1 = [st.enter_context(nc.psum_tensor(f"p1_{i}", [128, 512], F32)) for i in range(2)]
            p2 = [st.enter_context(nc.psum_tensor(f"p2_{i}", [128, 512], F32)) for i in range(2)]
            in_full = Cnt(nc, st, "b_in_full"); cs_full = Cnt(nc, st, "b_cs_full")
            a_done = Cnt(nc, st, "b_a_done")
            a3_done = Cnt(nc, st, "b_a3_done")
            pe_done = Cnt(nc, st, "b_pe_done")
            d_done = Cnt(nc, st, "b_d_done")
            st_done = Cnt(nc, st, "b_st_done")
            with nc.Block() as blk:
                @blk.sync
                def _(e):
                    for n, (kind, idx, g) in enumerate(items):
                        if grp_first[g] == n:
                            if g >= 2:
                                e.wait_ge(d_done.s, 6 * grp_first[g - 1])
                            e.dma_start(out=cs[g % 2][:, 0, :], in_=ropeC[:, g * 512:(g + 1) * 512]).then_inc(cs_full.s, 16)
                            e.dma_start(out=cs[g % 2][:, 1, :], in_=ropeS[:, g * 512:(g + 1) * 512]).then_inc(cs_full.s, 16)
                        if n >= 2:
                            e.wait_ge(a_done.s, 2 * (n - 1))
                        src = kraw_d[idx, :, g * 512:(g + 1) * 512] if kind == 'k' else qraw_d[idx, :, g * 512:(g + 1) * 512]
                        e.dma_start(out=ib[n % 2][:], in_=src).then_inc(in_full.s, 16)

                @blk.scalar
                def _(e):
                    for n, (kind, idx, g) in enumerate(items):
                        b = n % 2
                        e.wait_ge(in_full.s, 16 * (n + 1))
                        if n >= 2:
                            e.wait_ge(pe_done.s, n - 1)
                        e.activation(out=sqv[b][:], in_=ib[b][:], func=AF.Square).then_inc(a_done.s, 1)
                        ng = kng if kind == 'k' else qng
                        e.activation(out=yg[b][:], in_=ib[b][:], func=AF.Copy, scale=ng[:, 0:1]).then_inc(a_done.s, 1)
                        e.wait_ge(d_done.s, 6 * n + 1)
                        e.activation(out=t1[b][:], in_=t0[b][:], func=AF.Sqrt).then_inc(a3_done.s, 1)

                @blk.tensor
                def _(e):
                    for n, (kind, idx, g) in enumerate(items):
                        b = n % 2
                        e.wait_ge(a_done.s, 2 * (n + 1))
                        if n >= 2:
                            e.wait_ge(d_done.s, 6 * (n - 2) + 4)
                        e.matmul(p1[b][:], lhsT=onesb[:], rhs=sqv[b][:], start=True, stop=True)
                        e.matmul(p2[b][:], lhsT=rotm[:], rhs=yg[b][:], start=True, stop=True).then_inc(pe_done.s, 1)

                @blk.vector
                def _(e):
                    for n, (kind, idx, g) in enumerate(items):
                        b = n % 2
                        e.wait_ge(pe_done.s, n + 1)
                        e.wait_ge(cs_full.s, 32 * (g + 1))
                        if n >= 2:
                            e.wait_ge(st_done.s, 16 * (n - 1))
                        e.tensor_scalar(out=t0[b][:], in0=p1[b][:], scalar1=1.0 / 128, scalar2=EPS,
                                        op0=ALU.mult, op1=ALU.add).then_inc(d_done.s, 1)
                        e.tensor_tensor(out=u1[b][:], in0=yg[b][:], in1=cs[g % 2][:, 0, :], op=ALU.mult
                                        ).then_inc(d_done.s, 1)
                        e.tensor_tensor(out=u2[b][:], in0=p2[b][:], in1=cs[g % 2][:, 1, :], op=ALU.mult
                                        ).then_inc(d_done.s, 1)
                        e.wait_ge(d_done.s, 6 * n + 3)
                        e.tensor_tensor(out=u3[b][:], in0=u1[b][:], in1=u2[b][:], op=ALU.add
                                        ).then_inc(d_done.s, 1)
                        e.wait_ge(a3_done.s, n + 1)
                        e.reciprocal(out=rstd[b][:], in_=t1[b][:]).then_inc(d_done.s, 1)
                        e.wait_ge(d_done.s, 6 * n + 5)
                        e.tensor_tensor(out=outb[b][:], in0=u3[b][:], in1=rstd[b][:], op=ALU.mult
                                        ).then_inc(d_done.s, 1)

                @blk.gpsimd
                def _(e):
                    for n, (kind, idx, g) in enumerate(items):
                        e.wait_ge(d_done.s, 6 * (n + 1))
                        dst = kT_d[idx, :, g * 512:(g + 1) * 512] if kind == 'k' else qT_d[idx, :, g * 512:(g + 1) * 512]
                        e.dma_start(out=dst, in_=outb[n % 2][:]).then_inc(st_done.s, 16)
                    e.wait_ge(st_done.s, 16 * NI)
            clear_sems(nc)

    if "2" not in SKIP:
        aitems = [(kv, qt, hg) for kv in range(4) for qt in range(NOWN) for hg in range(4)]
        NA = len(aitems)
        NSC = S // 128
        SCALE = 128 ** -0.5
        with ExitStack() as st:
            kb = [st.enter_context(nc.sbuf_tensor(f"kb{i}", [128, S], BF16)) for i in range(2)]
            vb = [st.enter_context(nc.sbuf_tensor(f"vb{i}", [128, NSC, 128], BF16)) for i in range(2)]
            qb = [st.enter_context(nc.sbuf_tensor(f"qb{i}", [128, 512], BF16)) for i in range(2)]
            pb = [st.enter_context(nc.sbuf_tensor(f"pb{i}", [128, 512], BF16)) for i in range(2)]
            rden = [st.enter_context(nc.sbuf_tensor(f"rden{i}", [128, 512], F32)) for i in range(2)]
            aob = [st.enter_context(nc.sbuf_tensor(f"aob{i}", [128, 512], BF16)) for i in range(2)]
            sp = [st.enter_context(nc.psum_tensor(f"sp{i}", [128, 512], F32)) for i in range(2)]
            oacc = [st.enter_context(nc.psum_tensor(f"oacc{i}", [128, 512], F32)) for i in range(2)]
            dacc = [st.enter_context(nc.psum_tensor(f"dacc{i}", [128, 512], F32)) for i in range(2)]
            kv_full = Cnt(nc, st, "kv_full"); q_full = Cnt(nc, st, "q_full")
            s_done = Cnt(nc, st, "s_done"); p_done = Cnt(nc, st, "p_done"); od_done = Cnt(nc, st, "od_done")
            fin = Cnt(nc, st, "a_fin"); ast = Cnt(nc, st, "a_st")
            with nc.Block() as blk:
                @blk.sync
                def _(e):
                    with nc.allow_non_contiguous_dma(reason="kv tiles"):
                        for n, (kv, qt, hg) in enumerate(aitems):
                            if qt == 0 and hg == 0:
                                if kv >= 2:
                                    e.wait_ge(od_done.s, NSC * (NOWN * 4) * (kv - 1))
                                e.dma_start(out=kb[kv % 2][:], in_=kT_d[kv, :, :]).then_inc(kv_full.s, 16)
                                e.dma_start(out=vb[kv % 2][:], in_=v_d[kv, :, :, :]).then_inc(kv_full.s, 16)
                            if n >= 2:
                                e.wait_ge(s_done.s, NSC * (n - 1))
                            h = kv * 4 + hg
                            e.dma_start(out=qb[n % 2][:], in_=qT_d[h, :, qt * 512:(qt + 1) * 512]).then_inc(q_full.s, 16)

                @blk.tensor
                def _(e):
                    def s_mm(n, sc):
                        kv = aitems[n][0]
                        c = n * NSC + sc
                        if sc == 0:
                            e.wait_ge(q_full.s, 16 * (n + 1))
                            if aitems[n][1] == 0 and aitems[n][2] == 0:
                                e.wait_ge(kv_full.s, 32 * (kv + 1))
                        if c >= 2:
                            e.wait_ge(p_done.s, c - 1)
                        e.matmul(sp[c % 2][:], lhsT=kb[kv % 2][:, sc * 128:(sc + 1) * 128], rhs=qb[n % 2][:],
                                 start=True, stop=True).then_inc(s_done.s, 1)
                    def od_mm(n, sc):
                        kv = aitems[n][0]
                        c = n * NSC + sc
                        e.wait_ge(p_done.s, c + 1)
                        if sc == 0 and n >= 2:
                            e.wait_ge(fin.s, 2 * (n - 1))
                        e.matmul(oacc[n % 2][:], lhsT=vb[kv % 2][:, sc, :], rhs=pb[c % 2][:],
                                 start=(sc == 0), stop=(sc == NSC - 1))
                        e.matmul(dacc[n % 2][:], lhsT=onesb[:], rhs=pb[c % 2][:],
                                 start=(sc == 0), stop=(sc == NSC - 1)).then_inc(od_done.s, 1)
                    total = NA * NSC
                    for c in range(total + 1):
                        if c < total:
                            s_mm(c // NSC, c % NSC)
                        if c >= 1:
                            od_mm((c - 1) // NSC, (c - 1) % NSC)

                @blk.scalar
                def _(e):
                    for c in range(NA * NSC):
                        e.wait_ge(s_done.s, c + 1)
                        if c >= 2:
                            e.wait_ge(od_done.s, c - 1)
                        e.activation(out=pb[c % 2][:], in_=sp[c % 2][:], func=AF.Exp, scale=SCALE).then_inc(p_done.s, 1)

                @blk.vector
                def _(e):
                    for n in range(NA):
                        e.wait_ge(od_done.s, NSC * (n + 1))
                        e.reciprocal(out=rden[n % 2][:], in_=dacc[n % 2][:]).then_inc(fin.s, 1)
                        e.wait_ge(fin.s, 2 * n + 1)
                        if n >= 2:
                            e.wait_ge(ast.s, 16 * (n - 1))
                        e.tensor_tensor(out=aob[n % 2][:], in0=oacc[n % 2][:], in1=rden[n % 2][:], op=ALU.mult
                                        ).then_inc(fin.s, 1)

                @blk.gpsimd
                def _(e):
                    for n, (kv, qt, hg) in enumerate(aitems):
                        e.wait_ge(fin.s, 2 * (n + 1))
                        h = kv * 4 + hg
                        e.dma_start(out=mixT_d[h, :, qt * 512:(qt + 1) * 512], in_=aob[n % 2][:]).then_inc(ast.s, 16)
                    e.wait_ge(ast.s, 16 * NA)
            clear_sems(nc)

    if upto <= 2:
        _finish_dummy(nc, out)
        return nc

    if "3" not in SKIP:
        L = T + 2 * HALO
        with ExitStack() as st3:
            pmk = st3.enter_context(nc.sbuf_tensor("pmk", [128, L], F32))
            ps_sb = st3.enter_context(nc.sbuf_tensor("ps_sb", [128, 16], F32))
            pin = [st3.enter_context(nc.sbuf_tensor(f"pin{i}", [128, L], F32)) for i in range(2)]
            pmb = st3.enter_context(nc.sbuf_tensor("pmb", [128, L], F32))
            aw = [st3.enter_context(nc.sbuf_tensor(f"aw{i}", [128, L], F32)) for i in range(4)]
            ptmp = st3.enter_context(nc.sbuf_tensor("ptmp", [128, T], F32))
            invc = st3.enter_context(nc.sbuf_tensor("invc", [128, T], F32))
            dT = st3.enter_context(nc.sbuf_tensor("dT", [128, 4, T], BF16))
            wp = st3.enter_context(nc.sbuf_tensor("wp", [128, 4, 512], F32))
            pob = [st3.enter_context(nc.sbuf_tensor(f"pob{i}", [128, 512], BF16)) for i in range(2)]
            pacc = [st3.enter_context(nc.psum_tensor(f"pacc{i}", [128, 512], F32)) for i in range(2)]
            with ExitStack() as st:
                sc = Sched(nc, st, "p3pre")
                with nc.allow_non_contiguous_dma(reason="small"):
                    a = sc.add("gpsimd", lambda e: e.dma_start(out=pmk[:], in_=pmask[:, :]), dma_key="pmk")
                    b = sc.add("gpsimd", lambda e: e.dma_start(out=ps_sb[:], in_=pool_scale.rearrange("o (j p) -> p (o j)", p=128)),
                               dma_key="ps")
                    sc.run(final_waits=[a, b])
            for gi in range(4):
                wlog = gi + 1
                with ExitStack() as st:
                    sc = Sched(nc, st, f"p3a{gi}")
                    s_inv = sc.add("sync", lambda e: e.dma_start(out=invc[:], in_=invcnt[gi, :, :]), dma_key="invc")
                    last_pin = [None, None]
                    last = None
                    for j in range(4):
                        cc = gi * 4 + j
                        s_ld = sc.add("sync", lambda e, cc=cc, j=j: e.dma_start(out=pin[j % 2][:], in_=pi_d[cc, :, :]),
                                      deps=[last_pin[j % 2]], dma_key=f"pin{j % 2}")
                        s = sc.add("vector", lambda e, j=j: e.tensor_tensor(out=pmb[:], in0=pin[j % 2][:], in1=pmk[:], op=ALU.mult),
                                   deps=[s_ld, last])
                        last_pin[j % 2] = s
                        src = pmb
                        lo, hi = 0, L
                        for lv in range(wlog):
                            sh = 1 if lv == 0 else 2 ** (lv - 1)
                            dst = aw[lv]
                            if lv == 0:
                                nlo, nhi = lo + 1, hi
                                s = sc.add("vector", lambda e, dst=dst, src=src, nlo=nlo, nhi=nhi:
                                           e.tensor_tensor(out=dst[:, nlo:nhi], in0=src[:, nlo - 1:nhi - 1], in1=src[:, nlo:nhi], op=ALU.add),
                                           deps=[s])
                            else:
                                nlo, nhi = lo + sh, hi - sh
                                s = sc.add("vector", lambda e, dst=dst, src=src, nlo=nlo, nhi=nhi, sh=sh:
                                           e.tensor_tensor(out=dst[:, nlo:nhi], in0=src[:, nlo - sh:nhi - sh], in1=src[:, nlo + sh:nhi + sh], op=ALU.add),
                                           deps=[s])
                            src = dst; lo, hi = nlo, nhi
                        assert lo <= HALO and hi >= HALO + T
                        s = sc.add("vector", lambda e, src=src: e.tensor_tensor(out=ptmp[:], in0=src[:, HALO:HALO + T], in1=invc[:], op=ALU.mult),
                                   deps=[s, s_inv])
                        s = sc.add("vector", lambda e, j=j: e.tensor_tensor(out=dT[:, j, :], in0=ptmp[:], in1=pmb[:, HALO:HALO + T], op=ALU.subtract),
                                   deps=[s])
                        last = s
                    sc.run()
                with ExitStack() as st:
                    sc = Sched(nc, st, f"p3b{gi}")
                    s_w = sc.add("sync", lambda e: e.dma_start(out=wp[:], in_=w_pool[gi, :, :].rearrange("(j p) n -> p j n", p=128)),
                                 dma_key="wp")
                    evs = []; sts = []
                    cidx = 0
                    for jo in range(4):
                        for tt in range(NOWN):
                            c = cidx; cidx += 1
                            def mmf(e, jo=jo, tt=tt, c=c):
                                for ji in range(4):
                                    mm = e.matmul(pacc[c % 2][:], lhsT=hi16(wp[:, ji, jo * 128:(jo + 1) * 128]),
                                                  rhs=dT[:, ji, tt * 512:(tt + 1) * 512], start=(ji == 0), stop=(ji == 3))
                                return mm
                            s_mm = sc.add("tensor", mmf, deps=[s_w, evs[c - 2] if c >= 2 else None])
                            col = gi * 4 + jo
                            s_ev = sc.add("vector", lambda e, c=c, col=col: e.tensor_scalar(out=pob[c % 2][:], in0=pacc[c % 2][:],
                                          scalar1=ps_sb[:, col:col + 1], scalar2=None, op0=ALU.mult),
                                          deps=[s_mm, sts[c - 2] if c >= 2 else None])
                            evs.append(s_ev)
                            s_st = sc.add("gpsimd", lambda e, c=c, col=col, tt=tt: e.dma_start(
                                          out=mixT_d[16 + col, :, tt * 512:(tt + 1) * 512], in_=pob[c % 2][:]),
                                          deps=[s_ev], dma_key=f"pob{c % 2}")
                            sts.append(s_st)
                    sc.run(final_waits=sts[-2:])

    if upto <= 3:
        _finish_dummy(nc, out)
        return nc

    if "4a" not in SKIP:
        with ExitStack() as st:
            g1bc = st.enter_context(nc.sbuf_tensor("g1bc", [128, D], F32))
            mixb = [st.enter_context(nc.sbuf_tensor(f"mixb{i}", [128, KC, 512], BF16)) for i in range(2)]
            wob = [st.enter_context(nc.sbuf_tensor(f"wob{i}", [128, KC, 256], F32)) for i in range(2)]
            xp = [st.enter_context(nc.sbuf_tensor(f"xp{i}", [128, 256], F32)) for i in range(4)]
            tq = [st.enter_context(nc.sbuf_tensor(f"tq{i}", [128, 256], F32)) for i in range(2)]
            om = [st.enter_context(nc.sbuf_tensor(f"om{i}", [128, 256], F32)) for i in range(4)]
            oacc4 = [st.enter_context(nc.psum_tensor(f"oacc4_{i}", [128, 256], F32)) for i in range(2)]
            sc = Sched(nc, st, "p4a")
            s_g1 = sc.add("sync", lambda e: e.dma_start(out=g1bc[:], in_=mod_d[0:1, 2 * D:3 * D].partition_broadcast(128)),
                          dma_key="g1bc")
            mm_steps = []; t_steps = []; xm_steps = []; st_steps = []
            ci = 0
            last_mm_of_tg = {}
            last_mm_of_blk = {}
            for tg in range(NOWN):
                s_mix = sc.add("sync", lambda e, tg=tg: e.dma_start(out=mixb[tg % 2][:],
                               in_=mixT_d[:, :, tg * 512:(tg + 1) * 512].rearrange("m p t -> p m t")),
                               deps=[last_mm_of_tg.get(tg - 2)], dma_key=f"mixb{tg % 2}")
                for nb in range(16):
                    k = tg * 16 + nb
                    s_wo = sc.add("sync", lambda e, nb=nb, k=k: e.dma_start(out=wob[k % 2][:],
                                  in_=w_out[:, nb * 256:(nb + 1) * 256].rearrange("(m p) n -> p m n", p=128)),
                                  deps=[last_mm_of_blk.get(k - 2)], dma_key=f"wob{k % 2}")
                    for sub in range(4):
                        r0 = tg * 512 + sub * 128
                        s_x = sc.add("sync", lambda e, r0=r0, nb=nb, ci=ci: e.dma_start(out=xp[ci % 4][:],
                                     in_=x[r0:r0 + 128, nb * 256:(nb + 1) * 256]),
                                     deps=[xm_steps[ci - 4] if ci >= 4 else None], dma_key=f"xp{ci % 4}")
                        def mmf(e, tg=tg, k=k, sub=sub, ci=ci):
                            for m in range(KC):
                                mm = e.matmul(oacc4[ci % 2][:], lhsT=mixb[tg % 2][:, m, sub * 128:(sub + 1) * 128],
                                              rhs=hi16(wob[k % 2][:, m, :]), start=(m == 0), stop=(m == KC - 1))
                            return mm
                        s_mm = sc.add("tensor", mmf, deps=[s_mix, s_wo, t_steps[ci - 2] if ci >= 2 else None])
                        s_t = sc.add("vector", lambda e, ci=ci, nb=nb: e.tensor_tensor(out=tq[ci % 2][:], in0=oacc4[ci % 2][:],
                                     in1=g1bc[:, nb * 256:(nb + 1) * 256], op=ALU.mult),
                                     deps=[s_mm, s_g1, xm_steps[ci - 2] if ci >= 2 else None])
                        s_xm = sc.add("vector", lambda e, ci=ci: e.tensor_tensor(out=om[ci % 4][:], in0=tq[ci % 2][:],
                                      in1=xp[ci % 4][:], op=ALU.add),
                                      deps=[s_t, s_x, st_steps[ci - 4] if ci >= 4 else None])
                        s_st = sc.add("gpsimd", lambda e, ci=ci, r0=r0, nb=nb: e.dma_start(
                                      out=out[r0:r0 + 128, nb * 256:(nb + 1) * 256], in_=om[ci % 4][:]),
                                      deps=[s_xm], dma_key=f"om{ci % 4}")
                        mm_steps.append(s_mm); t_steps.append(s_t); xm_steps.append(s_xm); st_steps.append(s_st)
                        last_mm_of_tg[tg] = s_mm; last_mm_of_blk[k] = s_mm
                        ci += 1
            with nc.allow_non_contiguous_dma(reason="p4a tiles"):
                sc.run(final_waits=st_steps[-4:])

    if upto <= 4 and not os.environ.get("K_P4B"):
        return nc

    NT4 = T // 128
    with ExitStack() as st:
        xs = [st.enter_context(nc.sbuf_tensor(f"xs{i}", [128, D], F32)) for i in range(2)]
        junk2 = st.enter_context(nc.sbuf_tensor("junk2", [128, D], BF16))
        xn2 = [st.enter_context(nc.sbuf_tensor(f"xn2_{i}", [128, D], BF16)) for i in range(2)]
        h2t = [st.enter_context(nc.sbuf_tensor(f"h2t{i}", [128, KC, 128], BF16)) for i in range(2)]
        wr = st.enter_context(nc.sbuf_tensor("wr", [128, KC, NE], F32))
        brt = st.enter_context(nc.sbuf_tensor("brt", [128, NE], F32))
        b2s = st.enter_context(nc.sbuf_tensor("b2s", [NE, D], F32))
        g2bc = st.enter_context(nc.sbuf_tensor("g2bc", [128, D], F32))
        ss2 = st.enter_context(nc.sbuf_tensor("ss2", [128, NT4], F32))
        rsA = st.enter_context(nc.sbuf_tensor("rsA", [128, NT4], F32))
        rsB = st.enter_context(nc.sbuf_tensor("rsB", [128, NT4], F32))
        rsC = st.enter_context(nc.sbuf_tensor("rsC", [128, NT4], F32))
        lgs = st.enter_context(nc.sbuf_tensor("lgs", [128, NE], F32))
        mx8 = st.enter_context(nc.sbuf_tensor("mx8", [128, 8], F32))
        nmx = st.enter_context(nc.sbuf_tensor("nmx", [128, 1], F32))
        msk = st.enter_context(nc.sbuf_tensor("msk", [128, NE], F32))
        exv = st.enter_context(nc.sbuf_tensor("exv", [128, NE], F32))
        exm = st.enter_context(nc.sbuf_tensor("exm", [128, NE], F32))
        ssum = st.enter_context(nc.sbuf_tensor("ssum", [128, 1], F32))
        rsum = st.enter_context(nc.sbuf_tensor("rsum", [128, 1], F32))
        gts = st.enter_context(nc.sbuf_tensor("gts", [128, NE], F32))
        tb = [st.enter_context(nc.sbuf_tensor(f"tb{i}", [128, 512], F32)) for i in range(2)]
        tp2 = [st.enter_context(nc.psum_tensor(f"tp2_{i}", [128, 4, 128], BF16)) for i in range(2)]
        lgp = st.enter_context(nc.psum_tensor("lgp", [128, NE], F32))
        gtp = st.enter_context(nc.psum_tensor("gtp", [NE, 128], F32))
        bp = [st.enter_context(nc.psum_tensor(f"bp{i}", [128, 512], F32)) for i in range(2)]
        sc = Sched(nc, st, "p4b")
        s_wr = sc.add("sync", lambda e: e.dma_start(out=wr[:], in_=w_router.rearrange("(k p) n -> p k n", p=128)), dma_key="wr")
        s_br = sc.add("sync", lambda e: e.dma_start(out=brt[:], in_=b_router[0:1, :].partition_broadcast(128)), dma_key="brt")
        s_b2 = sc.add("sync", lambda e: e.dma_start(out=b2s[:], in_=b2[:, :]), dma_key="b2s")
        s_g2 = sc.add("sync", lambda e: e.dma_start(out=g2bc[:], in_=mod_d[0:1, 5 * D:6 * D].partition_broadcast(128)), dma_key="g2bc")
        s_z = sc.add("vector", lambda e: e.memset(ss2[:], 0.0))
        store = {}; xn_s = {}; last_tr = {}; h2_free = {}; lg_s = None; gcp_s = None
        u2_hist = []
        nbt = 0; evt_hist = []
        nq = 0
        for i in range(NT4):
            b = i % 2
            r0 = i * 128
            s_ld = sc.add("sync", lambda e, b=b, r0=r0: e.dma_start(out=xs[b][:], in_=out[r0:r0 + 128, :]),
                          deps=[store.get(i - 2)], dma_key=f"xs{b}")
            s_sq = sc.add("scalar", lambda e, b=b, i=i: e.activation(out=junk2[:], in_=xs[b][:], func=AF.Square,
                          accum_out=ss2[:, i:i + 1]), deps=[s_ld, s_z])
            s_r1 = sc.add("vector", lambda e, i=i: e.tensor_scalar(out=rsA[:, i:i + 1], in0=ss2[:, i:i + 1], scalar1=1.0 / D,
                          scalar2=EPS, op0=ALU.mult, op1=ALU.add), deps=[s_sq])
            s_r2 = sc.add("scalar", lambda e, i=i: e.activation(out=rsB[:, i:i + 1], in_=rsA[:, i:i + 1], func=AF.Sqrt), deps=[s_r1])
            s_r3 = sc.add("vector", lambda e, i=i: e.reciprocal(out=rsC[:, i:i + 1], in_=rsB[:, i:i + 1]), deps=[s_r2])
            s_xn = sc.add("vector", lambda e, b=b, i=i: e.tensor_scalar(out=xn2[b][:], in0=xs[b][:], scalar1=rsC[:, i:i + 1],
                          scalar2=None, op0=ALU.mult), deps=[s_r3, last_tr.get(i - 2)])
            xn_s[i] = s_xn
            s_evt = None
            for bt in range(8):
                def trf(e, b=b, bt=bt, nbt=nbt):
                    for j in range(4):
                        kc = bt * 4 + j
                        tr = e.transpose(tp2[nbt % 2][:, j, :], xn2[b][:, kc * 128:(kc + 1) * 128], identb[:])
                    return tr
                s_tr = sc.add("tensor", trf, deps=[s_xn, evt_hist[nbt - 2] if nbt >= 2 else None])
                def evf(e, b=b, bt=bt, nbt=nbt):
                    for j in range(4):
                        kc = bt * 4 + j
                        ins = e.activation(out=h2t[b][:, kc, :], in_=tp2[nbt % 2][:, j, :], func=AF.Identity,
                                           bias=SH2[:, kc:kc + 1], scale=G2[:, kc:kc + 1])
                    return ins
                s_evt = sc.add("scalar", evf, deps=[s_tr] + (list(h2_free.get(i - 2, ())) if bt == 0 else []))
                evt_hist.append(s_evt)
                nbt += 1
            last_tr[i] = s_tr
            s_sth = sc.add("gpsimd", lambda e, b=b, r0=r0: e.dma_start(
                           out=h2T_d[:, :, r0:r0 + 128].rearrange("k p t -> p k t"), in_=h2t[b][:]),
                           deps=[s_evt], dma_key=f"h2t{b}")
            def rtf(e, b=b):
                for kc in range(KC):
                    mm = e.matmul(lgp[:], lhsT=h2t[b][:, kc, :], rhs=hi16(wr[:, kc, :]), start=(kc == 0), stop=(kc == KC - 1))
                return mm
            s_rt = sc.add("tensor", rtf, deps=[s_evt, s_wr, lg_s])
            s_lg = sc.add("vector", lambda e: e.tensor_tensor(out=lgs[:], in0=lgp[:], in1=brt[:], op=ALU.add), deps=[s_rt, s_br, gcp_s])
            lg_s = s_lg
            s_mx = sc.add("vector", lambda e: e.max(out=mx8[:], in_=lgs[:]), deps=[s_lg])
            s_nm = sc.add("vector", lambda e: e.tensor_scalar(out=nmx[:], in0=mx8[:, 0:1], scalar1=-1.0, scalar2=None, op0=ALU.mult), deps=[s_mx])
            s_mk = sc.add("vector", lambda e: e.tensor_scalar(out=msk[:], in0=lgs[:], scalar1=mx8[:, 3:4], scalar2=None, op0=ALU.is_ge), deps=[s_mx])
            s_ex = sc.add("scalar", lambda e: e.activation(out=exv[:], in_=lgs[:], func=AF.Exp, bias=nmx[:, 0:1], scale=1.0), deps=[s_nm])
            s_em = sc.add("vector", lambda e: e.tensor_tensor(out=exm[:], in0=exv[:], in1=msk[:], op=ALU.mult), deps=[s_ex, s_mk])
            s_sm = sc.add("vector", lambda e: e.reduce_sum(out=ssum[:], in_=exm[:], axis=AX.X), deps=[s_em])
            s_rc = sc.add("vector", lambda e: e.reciprocal(out=rsum[:], in_=ssum[:]), deps=[s_sm])
            s_gt = sc.add("vector", lambda e: e.tensor_scalar(out=gts[:], in0=exm[:], scalar1=rsum[:, 0:1], scalar2=None, op0=ALU.mult), deps=[s_rc])
            s_gtt = sc.add("tensor", lambda e: e.transpose(gtp[:], gts[:], identf[:]), deps=[s_gt, gcp_s])
            s_gcp = sc.add("vector", lambda e, r0=r0: e.tensor_copy(out=gatesT[:, r0:r0 + 128], in_=gtp[:]), deps=[s_gtt])
            gcp_s = s_gcp
            s_u2 = None
            for n8 in range(8):
                q = nq; nq += 1
                s_bm = sc.add("tensor", lambda e, q=q, r0=r0, n8=n8: e.matmul(bp[q % 2][:], lhsT=gatesT[:, r0:r0 + 128],
                              rhs=b2s[:, n8 * 512:(n8 + 1) * 512], start=True, stop=True),
                              deps=[s_gcp, s_b2, u2_hist[q - 2] if q >= 2 else None])
                s_u1 = sc.add("vector", lambda e, q=q, n8=n8: e.tensor_tensor(out=tb[q % 2][:], in0=bp[q % 2][:],
                              in1=g2bc[:, n8 * 512:(n8 + 1) * 512], op=ALU.mult), deps=[s_bm, s_g2])
                s_u2 = sc.add("vector", lambda e, q=q, n8=n8, b=b: e.tensor_tensor(out=xs[b][:, n8 * 512:(n8 + 1) * 512],
                              in0=tb[q % 2][:], in1=xs[b][:, n8 * 512:(n8 + 1) * 512], op=ALU.add), deps=[s_u1, s_xn, s_sq])
                u2_hist.append(s_u2)
            s_st = sc.add("gpsimd", lambda e, b=b, r0=r0: e.dma_start(out=out[r0:r0 + 128, :], in_=xs[b][:]),
                          deps=[s_u2], dma_key=f"xo{b}")
            store[i] = s_st
            h2_free[i] = (s_sth, s_rt)
            sth_last = s_sth
        s_gd = sc.add("gpsimd", lambda e: e.dma_start(out=gates_dbg[:, :], in_=gatesT[:]), deps=[gcp_s], dma_key="gdbg")
        with nc.allow_non_contiguous_dma(reason="p4b tiles"):
            sc.run(final_waits=[store[NT4 - 1], store[NT4 - 2], h2_free[NT4 - 1][0], h2_free[NT4 - 2][0], s_gd])

    if upto <= 4:
        return nc

    class Ring:
        def __init__(self, tiles):
            self.tiles = tiles; self.i = -1; self.users = [[] for _ in tiles]
        def acquire(self):
            self.i += 1
            k = self.i % len(self.tiles)
            deps = self.users[k]; self.users[k] = []
            return k, self.tiles[k], deps
        def use(self, k, step):
            self.users[k].append(step)

    TCH = 1024
    NCH = T // TCH
    NEXP = int(os.environ.get("K_NEXP", NE))
    with ExitStack() as st:
        sbt = lambda name, shape, dt=F32: st.enter_context(nc.sbuf_tensor(name, list(shape), dt))
        h2c = sbt("h2c", [128, KC, TCH], BF16)
        w1r = Ring([sbt(f"w1r{i}", [128, KC, 128]) for i in range(4)])
        w2r = Ring([sbt(f"w2r{i}", [128, 8, 256]) for i in range(2)])
        actT = sbt("actT", [128, 8, TCH], BF16)
        gbc = Ring([sbt(f"gbc{i}", [128, 512]) for i in range(4)])
        tglu = Ring([sbt(f"tglu{i}", [128, 512]) for i in range(2)])
        tsg = Ring([sbt(f"tsg{i}", [128, 512]) for i in range(2)])
        tlin = Ring([sbt(f"tlin{i}", [128, 512]) for i in range(2)])
        prev = Ring([sbt(f"prev{i}", [128, 256]) for i in range(4)])
        obuf = Ring([sbt(f"obuf{i}", [128, 256]) for i in range(4)])
        eselr = Ring([sbt(f"eselr{i}", [NE, 128]) for i in range(2)])
        b1r = Ring([sbt(f"b1r{i}", [128, 16]) for i in range(2)])
        pg = Ring([st.enter_context(nc.psum_tensor(f"pg{i}", [128, 512], F32)) for i in range(2)])
        pl = Ring([st.enter_context(nc.psum_tensor(f"pl{i}", [128, 512], F32)) for i in range(2)])
        gbp = Ring([st.enter_context(nc.psum_tensor("gbp0", [128, 512], F32))])
        py = Ring([st.enter_context(nc.psum_tensor(f"py{i}", [128, 256], F32)) for i in range(2)])
        sc = Sched(nc, st, "p5")
        w1_pref = {}

        def load_w1(ex, j):
            kwg, wg, dg = w1r.acquire()
            s_wg = sc.add("sync", lambda e, wg=wg, ex=ex, j=j: e.dma_start(out=wg[:],
                          in_=w1[ex, :, j * 128:(j + 1) * 128].rearrange("(k p) n -> p k n", p=128)),
                          deps=dg, dma_key=f"w1r{kwg}")
            w1r.use(kwg, s_wg)
            kwl, wl, dl = w1r.acquire()
            s_wl = sc.add("sync", lambda e, wl=wl, ex=ex, j=j: e.dma_start(out=wl[:],
                          in_=w1[ex, :, DFF + j * 128:DFF + (j + 1) * 128].rearrange("(k p) n -> p k n", p=128)),
                          deps=dl, dma_key=f"w1r{kwl}")
            w1r.use(kwl, s_wl)
            return kwg, wg, s_wg, kwl, wl, s_wl

        last_store = {}
        h2_users = []
        act_readers = []
        stores = []
        for c in range(NCH):
            t0 = c * TCH
            s_h2 = sc.add("sync", lambda e, t0=t0: e.dma_start(out=h2c[:], in_=h2T_d[:, :, t0:t0 + TCH].rearrange("k p t -> p k t")),
                          deps=h2_users[-1:], dma_key="h2c")
            for ex in range(NEXP):
                ke, et, ed = eselr.acquire()
                s_es = sc.add("sync", lambda e, et=et, ex=ex: e.dma_start(out=et[:], in_=esel_in[:, ex * 128:(ex + 1) * 128]),
                              deps=ed, dma_key=f"esel{ke}")
                eselr.use(ke, s_es)
                kb1, b1t, b1d = b1r.acquire()
                with nc.allow_non_contiguous_dma(reason="b1"):
                    pass
                s_b1 = sc.add("sync", lambda e, b1t=b1t, ex=ex: e.dma_start(out=b1t[:],
                              in_=b1[ex:ex + 1, :].rearrange("o (h p) -> p (o h)", p=128)), deps=b1d, dma_key=f"b1r{kb1}")
                b1r.use(kb1, s_b1)
                gb = []
                for tt in range(TCH // 512):
                    kp, pt, pd = gbp.acquire()
                    s_gm = sc.add("tensor", lambda e, pt=pt, et=et, t0=t0, tt=tt: e.matmul(pt[:], lhsT=et[:],
                                  rhs=gatesT[:, t0 + tt * 512:t0 + (tt + 1) * 512], start=True, stop=True), deps=[s_es] + pd)
                    eselr.use(ke, s_gm)
                    kg, gt_, gd = gbc.acquire()
                    s_gc = sc.add("vector", lambda e, gt_=gt_, pt=pt: e.tensor_copy(out=gt_[:], in_=pt[:]), deps=[s_gm] + gd)
                    gbp.use(kp, s_gc); gbc.use(kg, s_gc)
                    gb.append((kg, gt_, s_gc))
                first_act_deps = list(act_readers); act_readers = []
                for j in range(8):
                    if j == 0 and (c, ex) in w1_pref:
                        kwg, wg, s_wg, kwl, wl, s_wl = w1_pref.pop((c, ex))
                    else:
                        kwg, wg, s_wg, kwl, wl, s_wl = load_w1(ex, j)
                    for tt in range(TCH // 512):
                        cs_ = slice(tt * 512, (tt + 1) * 512)
                        kpg, pgt, pgd = pg.acquire()
                        def mmg(e, pgt=pgt, wg=wg, cs_=cs_):
                            for k in range(KC):
                                mm = e.matmul(pgt[:], lhsT=hi16(wg[:, k, :]), rhs=h2c[:, k, cs_], start=(k == 0), stop=(k == KC - 1))
                            return mm
                        s_mg = sc.add("tensor", mmg, deps=[s_wg, s_h2] + pgd)
                        w1r.use(kwg, s_mg)
                        kpl, plt, pld = pl.acquire()
                        def mml(e, plt=plt, wl=wl, cs_=cs_):
                            for k in range(KC):
                                mm = e.matmul(plt[:], lhsT=hi16(wl[:, k, :]), rhs=h2c[:, k, cs_], start=(k == 0), stop=(k == KC - 1))
                            return mm
                        s_ml = sc.add("tensor", mml, deps=[s_wl, s_h2] + pld)
                        w1r.use(kwl, s_ml)
                        h2_users.append(s_ml)
                        k1, g_t, d1 = tglu.acquire()
                        s_glu = sc.add("vector", lambda e, g_t=g_t, pgt=pgt, b1t=b1t, j=j: e.tensor_scalar(out=g_t[:], in0=pgt[:],
                                       scalar1=b1t[:, j:j + 1], scalar2=SW_LIMIT, op0=ALU.add, op1=ALU.min), deps=[s_mg, s_b1] + d1)
                        pg.use(kpg, s_glu); b1r.use(kb1, s_glu)
                        k2, s_t, d2 = tsg.acquire()
                        s_sg = sc.add("scalar", lambda e, s_t=s_t, g_t=g_t: e.activation(out=s_t[:], in_=g_t[:], func=AF.Sigmoid,
                                      scale=SW_ALPHA), deps=[s_glu] + d2)
                        k3, l_t, d3 = tlin.acquire()
                        s_l1 = sc.add("vector", lambda e, l_t=l_t, plt=plt, b1t=b1t, j=j: e.tensor_scalar(out=l_t[:], in0=plt[:],
                                      scalar1=b1t[:, 8 + j:9 + j], scalar2=SW_LIMIT, op0=ALU.add, op1=ALU.min), deps=[s_ml, s_b1] + d3)
                        pl.use(kpl, s_l1); b1r.use(kb1, s_l1)
                        s_l2 = sc.add("vector", lambda e, l_t=l_t: e.tensor_scalar(out=l_t[:], in0=l_t[:], scalar1=-SW_LIMIT,
                                      scalar2=1.0, op0=ALU.max, op1=ALU.add), deps=[s_l1])
                        s_m1 = sc.add("vector", lambda e, g_t=g_t, s_t=s_t: e.tensor_tensor(out=g_t[:], in0=g_t[:], in1=s_t[:],
                                      op=ALU.mult), deps=[s_sg])
                        tsg.use(k2, s_m1)
                        s_m2 = sc.add("vector", lambda e, g_t=g_t, l_t=l_t: e.tensor_tensor(out=g_t[:], in0=g_t[:], in1=l_t[:],
                                      op=ALU.mult), deps=[s_m1, s_l2])
                        tlin.use(k3, s_m2)
                        kg, gt_, s_gc = gb[tt]
                        s_act = sc.add("vector", lambda e, g_t=g_t, gt_=gt_, j=j, cs_=cs_: e.tensor_tensor(out=actT[:, j, cs_],
                                       in0=g_t[:], in1=gt_[:], op=ALU.mult), deps=[s_m2, s_gc] + first_act_deps)
                        first_act_deps = []
                        tglu.use(k1, s_act); gbc.use(kg, s_act)
                        last_act = s_act
                if ex + 1 < NEXP:
                    w1_pref[(c, ex + 1)] = load_w1(ex + 1, 0)
                elif c + 1 < NCH:
                    w1_pref[(c + 1, 0)] = load_w1(0, 0)
                for n16 in range(16):
                    kw2, w2t, d2w = w2r.acquire()
                    s_w2 = sc.add("sync", lambda e, w2t=w2t, ex=ex, n16=n16: e.dma_start(out=w2t[:],
                                  in_=w2[ex, :, n16 * 256:(n16 + 1) * 256].rearrange("(j p) n -> p j n", p=128)),
                                  deps=d2w, dma_key=f"w2r{kw2}")
                    w2r.use(kw2, s_w2)
                    for ts in range(TCH // 128):
                        r0 = t0 + ts * 128
                        region = (r0, n16)
                        first = (ex == 0)
                        if not first:
                            kpv, pv, pvd = prev.acquire()
                            s_pv = sc.add("scalar", lambda e, pv=pv, r0=r0, n16=n16: e.dma_start(out=pv[:],
                                          in_=moe_d[r0:r0 + 128, n16 * 256:(n16 + 1) * 256]),
                                          deps=pvd + [last_store[region]], dma_key=f"prev{kpv}")
                        kpy, pyt, pyd = py.acquire()
                        def mmy(e, pyt=pyt, w2t=w2t, ts=ts):
                            for j in range(8):
                                mm = e.matmul(pyt[:], lhsT=actT[:, j, ts * 128:(ts + 1) * 128], rhs=hi16(w2t[:, j, :]),
                                              start=(j == 0), stop=(j == 7))
                            return mm
                        s_my = sc.add("tensor", mmy, deps=[s_w2, last_act] + pyd)
                        w2r.use(kw2, s_my)
                        act_readers.append(s_my)
                        if len(act_readers) > 2:
                            act_readers = act_readers[-2:]
                        ko, ot, od = obuf.acquire()
                        if first:
                            s_o = sc.add("vector", lambda e, ot=ot, pyt=pyt: e.tensor_copy(out=ot[:], in_=pyt[:]), deps=[s_my] + od)
                        else:
                            s_o = sc.add("vector", lambda e, ot=ot, pyt=pyt, pv=pv: e.tensor_tensor(out=ot[:], in0=pyt[:], in1=pv[:],
                                         op=ALU.add), deps=[s_my, s_pv] + od)
                            prev.use(kpv, s_o)
                        py.use(kpy, s_o)
                        s_st = sc.add("gpsimd", lambda e, ot=ot, r0=r0, n16=n16: e.dma_start(
                                      out=moe_d[r0:r0 + 128, n16 * 256:(n16 + 1) * 256], in_=ot[:]), deps=[s_o], dma_key=f"obuf{ko}")
                        obuf.use(ko, s_st)
                        last_store[region] = s_st
                        stores.append(s_st)
        with nc.allow_non_contiguous_dma(reason="p5 tiles"):
            sc.run(final_waits=stores[-4:])

    with ExitStack() as st:
        g2b = st.enter_context(nc.sbuf_tensor("g2b6", [128, D], F32))
        oi = Ring([st.enter_context(nc.sbuf_tensor(f"oi{i}", [128, D], F32)) for i in range(2)])
        ma = Ring([st.enter_context(nc.sbuf_tensor(f"ma{i}", [128, D], F32)) for i in range(2)])
        sc = Sched(nc, st, "p6")
        s_g2 = sc.add("sync", lambda e: e.dma_start(out=g2b[:], in_=mod_d[0:1, 5 * D:6 * D].partition_broadcast(128)), dma_key="g2b")
        fin = []
        for i in range(T // 128):
            r0 = i * 128
            ko, ot, od = oi.acquire()
            s_lo = sc.add("sync", lambda e, ot=ot, r0=r0: e.dma_start(out=ot[:], in_=out[r0:r0 + 128, :]), deps=od, dma_key=f"oi{ko}")
            km, mt, md = ma.acquire()
            s_lm = sc.add("sync", lambda e, mt=mt, r0=r0: e.dma_start(out=mt[:], in_=moe_d[r0:r0 + 128, :]), deps=md, dma_key=f"ma{km}")
            s_a = sc.add("vector", lambda e, mt=mt: e.tensor_tensor(out=mt[:], in0=mt[:], in1=g2b[:], op=ALU.mult), deps=[s_lm, s_g2])
            s_b = sc.add("vector", lambda e, mt=mt, ot=ot: e.tensor_tensor(out=ot[:], in0=ot[:], in1=mt[:], op=ALU.add), deps=[s_a, s_lo])
            ma.use(km, s_b)
            s_st = sc.add("gpsimd", lambda e, ot=ot, r0=r0: e.dma_start(out=out[r0:r0 + 128, :], in_=ot[:]), deps=[s_b], dma_key=f"oo{ko}")
            oi.use(ko, s_st)
            fin.append(s_st)
        with nc.allow_non_contiguous_dma(reason="p6"):
            sc.run(final_waits=fin[-2:])
    return nc


def _finish_dummy(nc, out):
    with ExitStack() as st:
        z = st.enter_context(nc.sbuf_tensor("zz", [128, D], F32))
        s = Cnt(nc, st, "zz_s"); s2 = Cnt(nc, st, "zz_s2")
        with nc.Block() as blk:
            @blk.vector
            def _(e):
                e.memset(z[:], 0.0).then_inc(s.s, 1)

            @blk.gpsimd
            def _(e):
                e.wait_ge(s.s, 1)
                for i in range(T // 128):
                    e.dma_start(out=out[i * 128:(i + 1) * 128, :], in_=z[:]).then_inc(s2.s, 16)
                e.wait_ge(s2.s, 16 * (T // 128))
        clear_sems(nc)


def make_in_maps(inputs, cores, names=None):
    rot, identb, identf, esel = _consts()
    x = np.asarray(inputs["x"])[0]
    shapes = {"c": (1, D), "b_mod": (1, 6 * D), "norm1_g": (1, D), "q_norm_g": (1, 128), "k_norm_g": (1, 128),
              "pool_scale": (1, 2048), "norm2_g": (1, D), "b_router": (1, NE)}
    shared = {"rotm": rot, "identb": identb, "identf": identf, "onesb": np.ones((128, 128), ml_dtypes.bfloat16), "esel": esel.reshape(32, 32 * 128)}
    for k, v in inputs.items():
        if k == "x" or (names is not None and k not in names):
            continue
        a = np.asarray(v)
        shared[k] = a.reshape(shapes[k]) if k in shapes else np.ascontiguousarray(a[0])
        if k in ("w1", "w2") and os.environ.get("K_NEXP"):
            shared[k] = np.ascontiguousarray(shared[k][:int(os.environ["K_NEXP"])])
    maps = []
    for core in cores:
        C, Sn, pm, ic = _tables(core)
        m = dict(shared)
        m["x"] = np.ascontiguousarray(np.roll(x, -core * T, axis=0))
        m["ropeC"] = C; m["ropeS"] = Sn; m["pmask"] = pm; m["invcnt"] = ic
        if names is not None:
            m = {k: v for k, v in m.items() if k in names}
        maps.append(m)
    return maps


def kernel(**inputs):
    nc = build()
    maps = make_in_maps(inputs, list(range(NC)))
    res = run_bass_kernel_spmd(nc, maps, core_ids=list(range(NC)))
    outs = [res.results[i]["out"] for i in range(NC)]
    return np.concatenate(outs, axis=0).reshape(1, S, D).astype(np.float32)
```

```python
import os
import numpy as np
from contextlib import ExitStack
import ml_dtypes
import concourse.bass as bass
import concourse.mybir as mybir
from concourse.bass_utils import run_bass_kernel_spmd

F32 = mybir.dt.float32
BF16 = mybir.dt.bfloat16
ALU = mybir.AluOpType
AF = mybir.ActivationFunctionType
AX = mybir.AxisListType

NC = 8
SKIP = set(filter(None, os.environ.get("K_SKIP", "").split(",")))
S = 8192
D = 4096
T = S // NC
HALO = 8
NG = S // 512
NOWN = T // 512
KC = D // 128
EPS = 1e-6
NE = 32
DFF = 1024
SW_ALPHA = 1.702
SW_LIMIT = 7.0


def _tables(core):
    tok = (core * T + np.arange(S)) % S
    row = (tok // 64).astype(np.float32)
    col = (tok % 64).astype(np.float32)
    inv_freq = (10000.0 ** (-np.arange(0, 64, 2, dtype=np.float32) / 64.0)).astype(np.float32)
    ang_r = row[None, :] * inv_freq[:, None]
    ang_c = col[None, :] * inv_freq[:, None]
    C = np.concatenate([np.cos(ang_r), np.cos(ang_r), np.cos(ang_c), np.cos(ang_c)], 0).astype(np.float32)
    Sn = np.concatenate([np.sin(ang_r), np.sin(ang_r), np.sin(ang_c), np.sin(ang_c)], 0).astype(np.float32)
    t_ext = core * T - HALO + np.arange(T + 2 * HALO)
    valid = ((t_ext >= 0) & (t_ext < S)).astype(np.float32)
    pmask = np.broadcast_to(valid[None, :], (128, T + 2 * HALO)).copy()
    t_own = core * T + np.arange(T)
    inv = np.zeros((4, T), np.float32)
    for gi, w in enumerate((2, 4, 8, 16)):
        lo = np.clip(t_own - w // 2, 0, S - 1)
        hi = np.clip(t_own + w // 2 - 1, 0, S - 1)
        inv[gi] = 1.0 / (hi - lo + 1).astype(np.float32)
    invcnt = np.broadcast_to(inv[:, None, :], (4, 128, T)).copy()
    return C, Sn, pmask, invcnt


def _consts():
    rot = np.zeros((128, 128), np.float32)
    for m in range(128):
        if (m % 64) < 32:
            rot[m + 32, m] = -1.0
        else:
            rot[m - 32, m] = 1.0
    ident = np.eye(128, dtype=np.float32)
    esel = np.zeros((32, 32, 128), np.float32)
    for e in range(32):
        esel[e, e, :] = 1.0
    return rot.astype(ml_dtypes.bfloat16), ident.astype(ml_dtypes.bfloat16), ident, esel


_G = {"st0": None, "pool": [], "resid": {}}


class Cnt:
    def __init__(self, nc, st, name):
        self.s = _G["st0"].enter_context(nc.semaphore(name))
        self.n = 0


def clear_sems(nc, sems=None):
    return


def hi16(ap):
    v = ap.bitcast(BF16)
    return v[:, 1::2]


class Sched:
    ENG = ("sync", "scalar", "vector", "tensor", "gpsimd")

    def __init__(self, nc, st, name):
        self.nc = nc; self.st = st; self.name = name
        self.steps = []
        self.sems = {}
        self.counts = {}

    def _sem(self, key):
        if key not in self.sems:
            h = _G["pool"].pop(0)
            self.sems[key] = h
            self.counts[key] = _G["resid"][h.num]
        return self.sems[key]

    def add(self, owner, fn, deps=(), dma_key=None):
        key = f"d_{dma_key}" if dma_key is not None else f"e_{owner}"
        self._sem(key)
        self.counts[key] += 16 if dma_key is not None else 1
        self.steps.append((owner, fn, tuple(d for d in deps if d is not None), key, self.counts[key]))
        return len(self.steps) - 1

    def run(self, final_waits=()):
        nc = self.nc
        steps = self.steps
        trunc = os.environ.get("K_TRUNC_" + self.name)
        if trunc is not None:
            n = int(trunc)
            steps = steps[:n]
            self.steps = steps
            final_waits = [i for i in range(max(0, n - 12), n)]
        with nc.Block() as blk:
            for owner in self.ENG:
                mine = [i for i, s in enumerate(steps) if s[0] == owner]
                if not mine and not (owner == "gpsimd" and final_waits):
                    continue
                def body(e, owner=owner, mine=mine):
                    waited = {}
                    for i in mine:
                        _, fn, deps, key, cnt = steps[i]
                        for d in deps:
                            dk, dc = steps[d][3], steps[d][4]
                            if waited.get(dk, 0) < dc:
                                e.wait_ge(self.sems[dk], dc)
                                waited[dk] = dc
                        ins = fn(e)
                        ins.then_inc(self.sems[key], 16 if key.startswith("d_") else 1)
                    if owner == "gpsimd":
                        for d in final_waits:
                            e.wait_ge(self.sems[steps[d][3]], steps[d][4])
                getattr(blk, owner)(body)
        for key, h in self.sems.items():
            _G["resid"][h.num] = self.counts[key]
            _G["pool"].append(h)


def build(upto=99, debug_outs=False):
    nc = bass.Bass("TRN2", target_bir_lowering=False)
    declared = []
    nc._declared_inputs = declared
    def di(name, shape, dt=F32, need=0):
        if upto < need:
            return None
        declared.append(name)
        return nc.dram_tensor(name, list(shape), dt, kind="ExternalInput").ap()
    x = di("x", [S, D])
    c_in = di("c", [1, D])
    w_mod = di("w_mod", [D, 6 * D])
    b_mod = di("b_mod", [1, 6 * D])
    norm1_g = di("norm1_g", [1, D])
    w_in = di("w_in", [D, 5120])
    q_norm_g = di("q_norm_g", [1, 128])
    k_norm_g = di("k_norm_g", [1, 128])
    w_pool = di("w_pool", [4, 512, 512], need=3)
    pool_scale = di("pool_scale", [1, 2048], need=3)
    w_out = di("w_out", [D, D], need=4)
    norm2_g = di("norm2_g", [1, D])
    w_router = di("w_router", [D, NE], need=4)
    b_router = di("b_router", [1, NE], need=4)
    NEd = int(os.environ.get("K_NEXP", NE))
    w1 = di("w1", [NEd, D, 2 * DFF], need=5)
    b1 = di("b1", [NE, 2 * DFF], need=5)
    w2 = di("w2", [NEd, DFF, D], need=5)
    b2 = di("b2", [NE, D], need=4)
    ropeC = di("ropeC", [128, S], need=2)
    ropeS = di("ropeS", [128, S], need=2)
    pmask = di("pmask", [128, T + 2 * HALO], need=3)
    invcnt = di("invcnt", [4, 128, T], need=3)
    rot_in = di("rotm", [128, 128], BF16)
    identb_in = di("identb", [128, 128], BF16)
    identf_in = di("identf", [128, 128])
    onesb_in = di("onesb", [128, 128], BF16)
    esel_in = di("esel", [32, 32 * 128], need=4)

    out = nc.dram_tensor("out", [T, D], F32, kind="ExternalOutput").ap()

    kind_dbg = "ExternalOutput" if debug_outs else "Internal"
    mod_d = nc.dram_tensor("mod_d", [1, 6 * D], F32, kind=kind_dbg).ap()
    qraw_d = nc.dram_tensor("qraw_d", [16, 128, T], F32, kind=kind_dbg).ap()
    kraw_d = nc.dram_tensor("kraw_d", [4, 128, S], F32, kind=kind_dbg).ap()
    v_d = nc.dram_tensor("v_d", [4, 128, S // 128, 128], BF16, kind=kind_dbg).ap()
    pi_d = nc.dram_tensor("pi_d", [16, 128, T + 2 * HALO], F32, kind=kind_dbg).ap()
    qT_d = nc.dram_tensor("qT_d", [16, 128, T], BF16).ap()
    kT_d = nc.dram_tensor("kT_d", [4, 128, S], BF16).ap()
    mixT_d = nc.dram_tensor("mixT_d", [32, 128, T], BF16, kind=kind_dbg).ap()
    h2T_d = nc.dram_tensor("h2T_d", [32, 128, T], BF16, kind=kind_dbg).ap()
    lgs_d = nc.dram_tensor("lgs_d", [1, 16], F32).ap()
    moe_d = nc.dram_tensor("moe_d", [T, D], F32, kind=kind_dbg).ap()
    gates_dbg = nc.dram_tensor("gates_dbg", [32, T], F32, kind=kind_dbg).ap()

    st0 = ExitStack()
    _G["st0"] = st0
    _G["pool"] = [st0.enter_context(nc.semaphore(f"pool{i}")) for i in range(52)]
    _G["resid"] = {h.num: 0 for h in _G["pool"]}
    sb = lambda name, shape, dt=F32: st0.enter_context(nc.sbuf_tensor(name, list(shape), dt))
    G1 = sb("G1", [128, 32]); SH1 = sb("SH1", [128, 32])
    G2 = sb("G2", [128, 32]); SH2 = sb("SH2", [128, 32])
    identb = sb("identb_s", [128, 128], BF16)
    identf = sb("identf_s", [128, 128])
    rotm = sb("rotm_s", [128, 128], BF16)
    onesb = sb("onesb_s", [128, 128], BF16)
    qng = sb("qng", [128, 1]); kng = sb("kng", [128, 1])
    gatesT = sb("gatesT", [32, T])

    cT = sb("cT", [128, KC]); cact = sb("cact", [128, KC])
    HM = 3 * D
    WM = 384
    for half in range(2):
        with ExitStack() as st:
            wst = [st.enter_context(nc.sbuf_tensor(f"wmod{half}_{i}", [128, KC, WM], F32)) for i in range(2)]
            mo = st.enter_context(nc.sbuf_tensor(f"mo{half}", [1, HM], F32))
            bm = st.enter_context(nc.sbuf_tensor(f"bm{half}", [1, HM], F32))
            ps = [st.enter_context(nc.psum_tensor(f"pm{half}_{i}", [128, 512], F32)) for i in range(2)]
            ld = Cnt(nc, st, f"p0_ld{half}"); wf = Cnt(nc, st, f"p0_wf{half}")
            ca = Cnt(nc, st, f"p0_ca{half}"); pd = Cnt(nc, st, f"p0_pd{half}"); pfree = Cnt(nc, st, f"p0_pfree{half}")
            stc = Cnt(nc, st, f"p0_st{half}")
            NT = HM // WM
            nld = 8 if half == 0 else 1
            with nc.Block() as blk:
                @blk.sync
                def _(e):
                    if half == 0:
                        with nc.allow_non_contiguous_dma(reason="small param transposes"):
                            e.dma_start(out=cT[:], in_=c_in.rearrange("o (k p) -> p (o k)", p=128)).then_inc(ld.s, 16)
                            e.dma_start(out=qng[:], in_=q_norm_g.rearrange("o p -> p o")).then_inc(ld.s, 16)
                            e.dma_start(out=kng[:], in_=k_norm_g.rearrange("o p -> p o")).then_inc(ld.s, 16)
                        e.dma_start(out=identb[:], in_=identb_in[:, :]).then_inc(ld.s, 16)
                        e.dma_start(out=identf[:], in_=identf_in[:, :]).then_inc(ld.s, 16)
                        e.dma_start(out=rotm[:], in_=rot_in[:, :]).then_inc(ld.s, 16)
                        e.dma_start(out=onesb[:], in_=onesb_in[:, :]).then_inc(ld.s, 16)
                    e.dma_start(out=bm[:], in_=b_mod[:, half * HM:(half + 1) * HM]).then_inc(ld.s, 16)
                    for i in range(NT):
                        if i >= 2:
                            e.wait_ge(pfree.s, i - 1)
                        c0 = half * HM + i * WM
                        e.dma_start(out=wst[i % 2][:],
                                    in_=w_mod[:, c0:c0 + WM].rearrange("(k p) n -> p k n", p=128)
                                    ).then_inc(wf.s, 16)

                @blk.scalar
                def _(e):
                    e.wait_ge(ld.s, 16 * nld)
                    if half == 0:
                        e.activation(out=cact[:], in_=cT[:], func=AF.Silu).then_inc(ca.s, 1)

                @blk.tensor
                def _(e):
                    if half == 0:
                        e.wait_ge(ca.s, 1)
                    for i in range(NT):
                        e.wait_ge(wf.s, 16 * (i + 1))
                        if i >= 2:
                            e.wait_ge(pfree.s, i - 1)
                        for k in range(KC):
                            mm = e.matmul(ps[i % 2][0:1, 0:WM], lhsT=cact[:, k:k + 1], rhs=wst[i % 2][:, k, :],
                                          start=(k == 0), stop=(k == KC - 1))
                        mm.then_inc(pd.s, 1)

                @blk.vector
                def _(e):
                    e.wait_ge(ld.s, 16 * nld)
                    for i in range(NT):
                        e.wait_ge(pd.s, i + 1)
                        e.tensor_tensor(out=mo[0:1, i * WM:(i + 1) * WM], in0=ps[i % 2][0:1, 0:WM],
                                        in1=bm[0:1, i * WM:(i + 1) * WM], op=ALU.add).then_inc(pfree.s, 1)

                @blk.gpsimd
                def _(e):
                    e.wait_ge(pfree.s, NT)
                    e.dma_start(out=mod_d[:, half * HM:(half + 1) * HM], in_=mo[:]).then_inc(stc.s, 16)
                    e.wait_ge(stc.s, 16)
            clear_sems(nc)

    with ExitStack() as st:
        modF = st.enter_context(nc.sbuf_tensor("modF", [128, 192], F32))
        n1g = st.enter_context(nc.sbuf_tensor("n1g", [128, 32], F32))
        n2g = st.enter_context(nc.sbuf_tensor("n2g", [128, 32], F32))
        ld = Cnt(nc, st, "p0b_ld"); dv = Cnt(nc, st, "p0b_dv")
        with nc.Block() as blk:
            @blk.sync
            def _(e):
                with nc.allow_non_contiguous_dma(reason="small param transposes"):
                    e.dma_start(out=modF[:], in_=mod_d.rearrange("o (j p) -> p (o j)", p=128)).then_inc(ld.s, 16)
                    e.dma_start(out=n1g[:], in_=norm1_g.rearrange("o (j p) -> p (o j)", p=128)).then_inc(ld.s, 16)
                    e.dma_start(out=n2g[:], in_=norm2_g.rearrange("o (j p) -> p (o j)", p=128)).then_inc(ld.s, 16)

            @blk.vector
            def _(e):
                e.wait_ge(ld.s, 48)
                e.scalar_tensor_tensor(out=G1[:], in0=modF[:, 32:64], scalar=1.0, in1=n1g[:],
                                       op0=ALU.add, op1=ALU.mult)
                e.scalar_tensor_tensor(out=G2[:], in0=modF[:, 128:160], scalar=1.0, in1=n2g[:],
                                       op0=ALU.add, op1=ALU.mult)
                e.tensor_copy(out=SH1[:], in_=modF[:, 0:32])
                e.tensor_copy(out=SH2[:], in_=modF[:, 96:128]).then_inc(dv.s, 1)
                e.wait_ge(dv.s, 1)
        clear_sems(nc)

    if upto <= 0:
        _finish_dummy(nc, out)
        return nc

    if "1" not in SKIP:
        def blocks_for(g):
            if g < NOWN:
                return list(range(20))
            if g == NOWN or g == NG - 1:
                return list(range(8)) + [16, 17, 18, 19]
            return [16, 17, 18, 19]

        with ExitStack() as st:
            xb = [st.enter_context(nc.sbuf_tensor(f"xb{i}", [128, D], F32)) for i in range(2)]
            xn = [st.enter_context(nc.sbuf_tensor(f"xn{i}", [128, D], BF16)) for i in range(2)]
            hT = [st.enter_context(nc.sbuf_tensor(f"hT{i}", [128, KC, 512], BF16)) for i in range(2)]
            wb = [st.enter_context(nc.sbuf_tensor(f"wb{i}", [128, KC, 256], F32)) for i in range(2)]
            ob = [st.enter_context(nc.sbuf_tensor(f"ob{i}", [128, 512], F32)) for i in range(4)]
            NTILE = S // 128
            ss = st.enter_context(nc.sbuf_tensor("ss", [128, NTILE], F32))
            rs = st.enter_context(nc.sbuf_tensor("rs", [128, NTILE], F32))
            rs2 = st.enter_context(nc.sbuf_tensor("rs2", [128, NTILE], F32))
            sq = st.enter_context(nc.sbuf_tensor("sq", [128, NTILE], F32))
            sq_done = Cnt(nc, st, "sq_done")
            junk = st.enter_context(nc.sbuf_tensor("junk", [128, D], BF16))
            tp = [st.enter_context(nc.psum_tensor(f"tp{i}", [128, 4, 128], BF16)) for i in range(2)]
            acc = [st.enter_context(nc.psum_tensor(f"acc{i}", [128, 512], F32)) for i in range(2)]
            z_done = Cnt(nc, st, "z_done")
            x_full = Cnt(nc, st, "x_full")
            ss_done = Cnt(nc, st, "ss_done")
            rs_done = Cnt(nc, st, "rs_done")
            xn_done = Cnt(nc, st, "xn_done")
            tp_done = Cnt(nc, st, "tp_done")
            tp_free = Cnt(nc, st, "tp_free")
            w_full = Cnt(nc, st, "w_full")
            acc_done = Cnt(nc, st, "acc_done")
            ev_done = Cnt(nc, st, "ev_done")
            o_free = Cnt(nc, st, "o_free")

            sched = []
            blk_end_chunk = []
            grp_end_chunk = []
            for g in range(NG):
                for b in blocks_for(g):
                    if b >= 18:
                        for sub in range(4):
                            sched.append((g, b, 'V', sub))
                    else:
                        for m in range(2):
                            sched.append((g, b, 'F', m))
                    blk_end_chunk.append(len(sched))
                grp_end_chunk.append(len(sched))

            with nc.Block() as blk:
                @blk.sync
                def _(e):
                    bi = 0
                    def load_x(g):
                        for sub in range(4):
                            i = g * 4 + sub
                            if i >= 2:
                                e.wait_ge(xn_done.s, i - 1)
                            e.dma_start(out=xb[i % 2][:], in_=x[i * 128:(i + 1) * 128, :]).then_inc(x_full.s, 16)
                    def load_w(g):
                        nonlocal bi
                        for b in blocks_for(g):
                            if bi >= 2:
                                e.wait_ge(acc_done.s, blk_end_chunk[bi - 2])
                            e.dma_start(out=wb[bi % 2][:],
                                        in_=w_in[:, b * 256:(b + 1) * 256].rearrange("(k p) n -> p k n", p=128)
                                        ).then_inc(w_full.s, 16)
                            bi += 1
                    load_x(0)
                    for g in range(1, NG):
                        load_x(g)
                        load_w(g - 1)
                    load_w(NG - 1)

                @blk.scalar
                def _(e):
                    nb = 0
                    e.wait_ge(z_done.s, 1)
                    for i in range(NTILE):
                        g = i // 4; sub = i % 4
                        e.wait_ge(x_full.s, 16 * (i + 1))
                        e.activation(out=junk[:], in_=xb[i % 2][:], func=AF.Square,
                                     accum_out=ss[:, i:i + 1]).then_inc(ss_done.s, 1)
                        e.wait_ge(rs_done.s, 2 * i + 1)
                        e.activation(out=sq[:, i:i + 1], in_=rs[:, i:i + 1], func=AF.Sqrt).then_inc(sq_done.s, 1)
                        if sub == 0 and g >= 2:
                            e.wait_ge(acc_done.s, grp_end_chunk[g - 2])
                        for bt in range(8):
                            e.wait_ge(tp_done.s, nb + 1)
                            for j in range(4):
                                kc = bt * 4 + j
                                ins = e.activation(out=hT[g % 2][:, kc, sub * 128:(sub + 1) * 128],
                                                   in_=tp[nb % 2][:, j, :], func=AF.Identity,
                                                   bias=SH1[:, kc:kc + 1], scale=G1[:, kc:kc + 1])
                            ins.then_inc(tp_free.s, 1)
                            nb += 1

                @blk.vector
                def _(e):
                    e.memset(ss[:], 0.0).then_inc(z_done.s, 1)
                    ci = 0
                    def evac_chunks(upto_ci):
                        nonlocal ci
                        while ci < upto_ci:
                            g, b, kind, sidx = sched[ci]
                            e.wait_ge(acc_done.s, ci + 1)
                            if ci >= 4:
                                e.wait_ge(o_free.s, 16 * (ci - 3))
                            if kind == 'V':
                                ins = e.tensor_copy(out=ob[ci % 4][:, 0:128].bitcast(BF16),
                                                    in_=acc[ci % 2][:, 0:256])
                            else:
                                ins = e.tensor_copy(out=ob[ci % 4][:], in_=acc[ci % 2][:])
                            ins.then_inc(ev_done.s, 1)
                            ci += 1
                    for i in range(NTILE):
                        g = i // 4
                        e.wait_ge(ss_done.s, i + 1)
                        col = slice(i, i + 1)
                        e.tensor_scalar(out=rs[:, col], in0=ss[:, col], scalar1=1.0 / D, scalar2=EPS,
                                        op0=ALU.mult, op1=ALU.add).then_inc(rs_done.s, 1)
                        e.wait_ge(sq_done.s, i + 1)
                        e.reciprocal(out=rs2[:, col], in_=sq[:, col]).then_inc(rs_done.s, 1)
                        e.wait_ge(rs_done.s, 2 * i + 2)
                        if i >= 2:
                            e.wait_ge(tp_done.s, 8 * (i - 1))
                        e.tensor_scalar(out=xn[i % 2][:], in0=xb[i % 2][:], scalar1=rs2[:, col], scalar2=None,
                                        op0=ALU.mult).then_inc(xn_done.s, 1)
                        if i % 4 == 3 and g >= 1:
                            evac_chunks(grp_end_chunk[g - 1])
                    evac_chunks(len(sched))

                @blk.tensor
                def _(e):
                    nb = 0
                    ci = 0
                    bi = 0
                    def proj_group(g):
                        nonlocal ci, bi
                        e.wait_ge(tp_free.s, 32 * (g + 1))
                        for b in blocks_for(g):
                            e.wait_ge(w_full.s, 16 * (bi + 1))
                            w = wb[bi % 2]
                            if b >= 18:
                                for sub in range(4):
                                    if ci >= 2:
                                        e.wait_ge(ev_done.s, ci - 1)
                                    for k in range(KC):
                                        mm = e.matmul(acc[ci % 2][:, 0:256],
                                                      lhsT=hT[g % 2][:, k, sub * 128:(sub + 1) * 128],
                                                      rhs=hi16(w[:, k, :]), start=(k == 0), stop=(k == KC - 1))
                                    mm.then_inc(acc_done.s, 1)
                                    ci += 1
                            else:
                                for m in range(2):
                                    if ci >= 2:
                                        e.wait_ge(ev_done.s, ci - 1)
                                    for k in range(KC):
                                        mm = e.matmul(acc[ci % 2][:, :],
                                                      lhsT=hi16(w[:, k, m * 128:(m + 1) * 128]),
                                                      rhs=hT[g % 2][:, k, :], start=(k == 0), stop=(k == KC - 1))
                                    mm.then_inc(acc_done.s, 1)
                                    ci += 1
                            bi += 1

                    for i in range(NTILE):
                        g = i // 4
                        e.wait_ge(xn_done.s, i + 1)
                        for bt in range(8):
                            if nb >= 2:
                                e.wait_ge(tp_free.s, nb - 1)
                            for j in range(4):
                                kc = bt * 4 + j
                                tr = e.transpose(tp[nb % 2][:, j, :], xn[i % 2][:, kc * 128:(kc + 1) * 128], identb[:])
                            tr.then_inc(tp_done.s, 1)
                            nb += 1
                        if i % 4 == 3 and g >= 1:
                            proj_group(g - 1)
                    proj_group(NG - 1)

                @blk.gpsimd
                def _(e):
                    with nc.allow_non_contiguous_dma(reason="scratch layouts"):
                        for ci, (g, b, kind, sidx) in enumerate(sched):
                            e.wait_ge(ev_done.s, ci + 1)
                            if kind == 'V':
                                half = b - 18
                                chunk = g * 4 + sidx
                                src = ob[ci % 4][:, 0:128].bitcast(BF16)
                                e.dma_start(out=v_d[half * 2:half * 2 + 2, :, chunk, :].rearrange("h p d -> p h d"),
                                            in_=src.rearrange("p (h d) -> p h d", h=2)).then_inc(o_free.s, 16)
                            else:
                                f = b * 2 + sidx
                                if f < 16:
                                    if g < NOWN:
                                        e.dma_start(out=pi_d[f, :, HALO + g * 512:HALO + (g + 1) * 512],
                                                    in_=ob[ci % 4][:]).then_inc(o_free.s, 16)
                                    elif g == NOWN:
                                        e.dma_start(out=pi_d[f, :, HALO + T:HALO + T + HALO],
                                                    in_=ob[ci % 4][:, 0:HALO]).then_inc(o_free.s, 16)
                                    else:
                                        e.dma_start(out=pi_d[f, :, 0:HALO],
                                                    in_=ob[ci % 4][:, 512 - HALO:512]).then_inc(o_free.s, 16)
                                elif f < 32:
                                    e.dma_start(out=qraw_d[f - 16, :, g * 512:(g + 1) * 512],
                                                in_=ob[ci % 4][:]).then_inc(o_free.s, 16)
                                else:
                                    e.dma_start(out=kraw_d[f - 32, :, g * 512:(g + 1) * 512],
                                                in_=ob[ci % 4][:]).then_inc(o_free.s, 16)
                        e.wait_ge(o_free.s, 16 * len(sched))
            clear_sems(nc)

    if upto <= 1:
        _finish_dummy(nc, out)
        return nc

    if "1b" not in SKIP:
        items = []
        for g in range(NG):
            for kv in range(4):
                items.append(('k', kv, g))
            if g < NOWN:
                for h in range(16):
                    items.append(('q', h, g))
        NI = len(items)
        grp_first = {}
        for n, (kind, idx, g) in enumerate(items):
            grp_first.setdefault(g, n)
        with ExitStack() as st:
            ib = [st.enter_context(nc.sbuf_tensor(f"ib{i}", [128, 512], F32)) for i in range(2)]
            cs = [st.enter_context(nc.sbuf_tensor(f"cs{i}", [128, 2, 512], F32)) for i in range(2)]
            sqv = [st.enter_context(nc.sbuf_tensor(f"sqv{i}", [128, 512], BF16)) for i in range(2)]
            yg = [st.enter_context(nc.sbuf_tensor(f"yg{i}", [128, 512], BF16)) for i in range(2)]
            t0 = [st.enter_context(nc.sbuf_tensor(f"t0_{i}", [128, 512], F32)) for i in range(2)]
            t1 = [st.enter_context(nc.sbuf_tensor(f"t1_{i}", [128, 512], F32)) for i in range(2)]
            rstd = [st.enter_context(nc.sbuf_tensor(f"rstd{i}", [128, 512], F32)) for i in range(2)]
            u1 = [st.enter_context(nc.sbuf_tensor(f"u1_{i}", [128, 512], F32)) for i in range(2)]
            u2 = [st.enter_context(nc.sbuf_tensor(f"u2_{i}", [128, 512], F32)) for i in range(2)]
            u3 = [st.enter_context(nc.sbuf_tensor(f"u3_{i}", [128, 512], F32)) for i in range(2)]
            outb = [st.enter_context(nc.sbuf_tensor(f"outb{i}", [128, 512], BF16)) for i in range(2)]
            p1 = [st.enter_context(nc.psum_tensor(f"p1_{i}", [128, 512], F32)) for i in range(2)]
            p2 = [st.enter_context(nc.psum_tensor(f"p2_{i}", [128, 512], F32)) for i in range(2)]
            in_full = Cnt(nc, st, "b_in_full"); cs_full = Cnt(nc, st, "b_cs_full")
            a_done = Cnt(nc, st, "b_a_done")
            a3_done = Cnt(nc, st, "b_a3_done")
            pe_done = Cnt(nc, st, "b_pe_done")
            d_done = Cnt(nc, st, "b_d_done")
            st_done = Cnt(nc, st, "b_st_done")
            with nc.Block() as blk:
                @blk.sync
                def _(e):
                    for n, (kind, idx, g) in enumerate(items):
                        if grp_first[g] == n:
                            if g >= 2:
                                e.wait_ge(d_done.s, 6 * grp_first[g - 1])
                            e.dma_start(out=cs[g % 2][:, 0, :], in_=ropeC[:, g * 512:(g + 1) * 512]).then_inc(cs_full.s, 16)
                            e.dma_start(out=cs[g % 2][:, 1, :], in_=ropeS[:, g * 512:(g + 1) * 512]).then_inc(cs_full.s, 16)
                        if n >= 2:
                            e.wait_ge(a_done.s, 2 * (n - 1))
                        src = kraw_d[idx, :, g * 512:(g + 1) * 512] if kind == 'k' else qraw_d[idx, :, g * 512:(g + 1) * 512]
                        e.dma_start(out=ib[n % 2][:], in_=src).then_inc(in_full.s, 16)

                @blk.scalar
                def _(e):
                    for n, (kind, idx, g) in enumerate(items):
                        b = n % 2
                        e.wait_ge(in_full.s, 16 * (n + 1))
                        if n >= 2:
                            e.wait_ge(pe_done.s, n - 1)
                        e.activation(out=sqv[b][:], in_=ib[b][:], func=AF.Square).then_inc(a_done.s, 1)
                        ng = kng if kind == 'k' else qng
                        e.activation(out=yg[b][:], in_=ib[b][:], func=AF.Copy, scale=ng[:, 0:1]).then_inc(a_done.s, 1)
                        e.wait_ge(d_done.s, 6 * n + 1)
                        e.activation(out=t1[b][:], in_=t0[b][:], func=AF.Sqrt).then_inc(a3_done.s, 1)

                @blk.tensor
                def _(e):
                    for n, (kind, idx, g) in enumerate(items):
                        b = n % 2
                        e.wait_ge(a_done.s, 2 * (n + 1))
                        if n >= 2:
                            e.wait_ge(d_done.s, 6 * (n - 2) + 4)
                        e.matmul(p1[b][:], lhsT=onesb[:], rhs=sqv[b][:], start=True, stop=True)
                        e.matmul(p2[b][:], lhsT=rotm[:], rhs=yg[b][:], start=True, stop=True).then_inc(pe_done.s, 1)

                @blk.vector
                def _(e):
                    for n, (kind, idx, g) in enumerate(items):
                        b = n % 2
                        e.wait_ge(pe_done.s, n + 1)
                        e.wait_ge(cs_full.s, 32 * (g + 1))
                        if n >= 2:
                            e.wait_ge(st_done.s, 16 * (n - 1))
                        e.tensor_scalar(out=t0[b][:], in0=p1[b][:], scalar1=1.0 / 128, scalar2=EPS,
                                        op0=ALU.mult, op1=ALU.add).then_inc(d_done.s, 1)
                        e.tensor_tensor(out=u1[b][:], in0=yg[b][:], in1=cs[g % 2][:, 0, :], op=ALU.mult
                                        ).then_inc(d_done.s, 1)
                        e.tensor_tensor(out=u2[b][:], in0=p2[b][:], in1=cs[g % 2][:, 1, :], op=ALU.mult
                                        ).then_inc(d_done.s, 1)
                        e.wait_ge(d_done.s, 6 * n + 3)
                        e.tensor_tensor(out=u3[b][:], in0=u1[b][:], in1=u2[b][:], op=ALU.add
                                        ).then_inc(d_done.s, 1)
                        e.wait_ge(a3_done.s, n + 1)
                        e.reciprocal(out=rstd[b][:], in_=t1[b][:]).then_inc(d_done.s, 1)
                        e.wait_ge(d_done.s, 6 * n + 5)
                        e.tensor_tensor(out=outb[b][:], in0=u3[b][:], in1=rstd[b][:], op=ALU.mult
                                        ).then_inc(d_done.s, 1)

                @blk.gpsimd
                def _(e):
                    for n, (kind, idx, g) in enumerate(items):
                        e.wait_ge(d_done.s, 6 * (n + 1))
                        dst = kT_d[idx, :, g * 512:(g + 1) * 512] if kind == 'k' else qT_d[idx, :, g * 512:(g + 1) * 512]
                        e.dma_start(out=dst, in_=outb[n % 2][:]).then_inc(st_done.s, 16)
                    e.wait_ge(st_done.s, 16 * NI)
            clear_sems(nc)

    if "2" not in SKIP:
        aitems = [(kv, qt, hg) for kv in range(4) for qt in range(NOWN) for hg in range(4)]
        NA = len(aitems)
        NSC = S // 128
        SCALE = 128 ** -0.5
        with ExitStack() as st:
            kb = [st.enter_context(nc.sbuf_tensor(f"kb{i}", [128, S], BF16)) for i in range(2)]
            vb = [st.enter_context(nc.sbuf_tensor(f"vb{i}", [128, NSC, 128], BF16)) for i in range(2)]
            qb = [st.enter_context(nc.sbuf_tensor(f"qb{i}", [128, 512], BF16)) for i in range(2)]
            pb = [st.enter_context(nc.sbuf_tensor(f"pb{i}", [128, 512], BF16)) for i in range(2)]
            rden = [st.enter_context(nc.sbuf_tensor(f"rden{i}", [128, 512], F32)) for i in range(2)]
            aob = [st.enter_context(nc.sbuf_tensor(f"aob{i}", [128, 512], BF16)) for i in range(2)]
            sp = [st.enter_context(nc.psum_tensor(f"sp{i}", [128, 512], F32)) for i in range(2)]
            oacc = [st.enter_context(nc.psum_tensor(f"oacc{i}", [128, 512], F32)) for i in range(2)]
            dacc = [st.enter_context(nc.psum_tensor(f"dacc{i}", [128, 512], F32)) for i in range(2)]
            kv_full = Cnt(nc, st, "kv_full"); q_full = Cnt(nc, st, "q_full")
            s_done = Cnt(nc, st, "s_done"); p_done = Cnt(nc, st, "p_done"); od_done = Cnt(nc, st, "od_done")
            fin = Cnt(nc, st, "a_fin"); ast = Cnt(nc, st, "a_st")
            with nc.Block() as blk:
                @blk.sync
                def _(e):
                    with nc.allow_non_contiguous_dma(reason="kv tiles"):
                        for n, (kv, qt, hg) in enumerate(aitems):
                            if qt == 0 and hg == 0:
                                if kv >= 2:
                                    e.wait_ge(od_done.s, NSC * (NOWN * 4) * (kv - 1))
                                e.dma_start(out=kb[kv % 2][:], in_=kT_d[kv, :, :]).then_inc(kv_full.s, 16)
                                e.dma_start(out=vb[kv % 2][:], in_=v_d[kv, :, :, :]).then_inc(kv_full.s, 16)
                            if n >= 2:
                                e.wait_ge(s_done.s, NSC * (n - 1))
                            h = kv * 4 + hg
                            e.dma_start(out=qb[n % 2][:], in_=qT_d[h, :, qt * 512:(qt + 1) * 512]).then_inc(q_full.s, 16)

                @blk.tensor
                def _(e):
                    def s_mm(n, sc):
                        kv = aitems[n][0]
                        c = n * NSC + sc
                        if sc == 0:
                            e.wait_ge(q_full.s, 16 * (n + 1))
                            if aitems[n][1] == 0 and aitems[n][2] == 0:
                                e.wait_ge(kv_full.s, 32 * (kv + 1))
                        if c >= 2:
                            e.wait_ge(p_done.s, c - 1)
                        e.matmul(sp[c % 2][:], lhsT=kb[kv % 2][:, sc * 128:(sc + 1) * 128], rhs=qb[n % 2][:],
                                 start=True, stop=True).then_inc(s_done.s, 1)
                    def od_mm(n, sc):
                        kv = aitems[n][0]
                        c = n * NSC + sc
                        e.wait_ge(p_done.s, c + 1)
                        if sc == 0 and n >= 2:
                            e.wait_ge(fin.s, 2 * (n - 1))
                        e.matmul(oacc[n % 2][:], lhsT=vb[kv % 2][:, sc, :], rhs=pb[c % 2][:],
                                 start=(sc == 0), stop=(sc == NSC - 1))
                        e.matmul(dacc[n % 2][:], lhsT=onesb[:], rhs=pb[c % 2][:],
                                 start=(sc == 0), stop=(sc == NSC - 1)).then_inc(od_done.s, 1)
                    total = NA * NSC
                    for c in range(total + 1):
                        if c < total:
                            s_mm(c // NSC, c % NSC)
                        if c >= 1:
                            od_mm((c - 1) // NSC, (c - 1) % NSC)

                @blk.scalar
                def _(e):
                    for c in range(NA * NSC):
                        e.wait_ge(s_done.s, c + 1)
                        if c >= 2:
                            e.wait_ge(od_done.s, c - 1)
                        e.activation(out=pb[c % 2][:], in_=sp[c % 2][:], func=AF.Exp, scale=SCALE).then_inc(p_done.s, 1)

                @blk.vector
                def _(e):
                    for n in range(NA):
                        e.wait_ge(od_done.s, NSC * (n + 1))
                        e.reciprocal(out=rden[n % 2][:], in_=dacc[n % 2][:]).then_inc(fin.s, 1)
                        e.wait_ge(fin.s, 2 * n + 1)
                        if n >= 2:
                            e.wait_ge(ast.s, 16 * (n - 1))
                        e.tensor_tensor(out=aob[n % 2][:], in0=oacc[n % 2][:], in1=rden[n % 2][:], op=ALU.mult
                                        ).then_inc(fin.s, 1)

                @blk.gpsimd
                def _(e):
                    for n, (kv, qt, hg) in enumerate(aitems):
                        e.wait_ge(fin.s, 2 * (n + 1))
                        h = kv * 4 + hg
                        e.dma_start(out=mixT_d[h, :, qt * 512:(qt + 1) * 512], in_=aob[n % 2][:]).then_inc(ast.s, 16)
                    e.wait_ge(ast.s, 16 * NA)
            clear_sems(nc)

    if upto <= 2:
        _finish_dummy(nc, out)
        return nc

    if "3" not in SKIP:
        L = T + 2 * HALO
        with ExitStack() as st3:
            pmk = st3.enter_context(nc.sbuf_tensor("pmk", [128, L], F32))
            ps_sb = st3.enter_context(nc.sbuf_tensor("ps_sb", [128, 16], F32))
            pin = [st3.enter_context(nc.sbuf_tensor(f"pin{i}", [128, L], F32)) for i in range(2)]
            pmb = st3.enter_context(nc.sbuf_tensor("pmb", [128, L], F32))
            aw = [st3.enter_context(nc.sbuf_tensor(f"aw{i}", [128, L], F32)) for i in range(4)]
            ptmp = st3.enter_context(nc.sbuf_tensor("ptmp", [128, T], F32))
            invc = st3.enter_context(nc.sbuf_tensor("invc", [128, T], F32))
            dT = st3.enter_context(nc.sbuf_tensor("dT", [128, 4, T], BF16))
            wp = st3.enter_context(nc.sbuf_tensor("wp", [128, 4, 512], F32))
            pob = [st3.enter_context(nc.sbuf_tensor(f"pob{i}", [128, 512], BF16)) for i in range(2)]
            pacc = [st3.enter_context(nc.psum_tensor(f"pacc{i}", [128, 512], F32)) for i in range(2)]
            with ExitStack() as st:
                sc = Sched(nc, st, "p3pre")
                with nc.allow_non_contiguous_dma(reason="small"):
                    a = sc.add("gpsimd", lambda e: e.dma_start(out=pmk[:], in_=pmask[:, :]), dma_key="pmk")
                    b = sc.add("gpsimd", lambda e: e.dma_start(out=ps_sb[:], in_=pool_scale.rearrange("o (j p) -> p (o j)", p=128)),
                               dma_key="ps")
                    sc.run(final_waits=[a, b])
            for gi in range(4):
                wlog = gi + 1
                with ExitStack() as st:
                    sc = Sched(nc, st, f"p3a{gi}")
                    s_inv = sc.add("sync", lambda e: e.dma_start(out=invc[:], in_=invcnt[gi, :, :]), dma_key="invc")
                    last_pin = [None, None]
                    last = None
                    for j in range(4):
                        cc = gi * 4 + j
                        s_ld = sc.add("sync", lambda e, cc=cc, j=j: e.dma_start(out=pin[j % 2][:], in_=pi_d[cc, :, :]),
                                      deps=[last_pin[j % 2]], dma_key=f"pin{j % 2}")
                        s = sc.add("vector", lambda e, j=j: e.tensor_tensor(out=pmb[:], in0=pin[j % 2][:], in1=pmk[:], op=ALU.mult),
                                   deps=[s_ld, last])
                        last_pin[j % 2] = s
                        src = pmb
                        lo, hi = 0, L
                        for lv in range(wlog):
                            sh = 1 if lv == 0 else 2 ** (lv - 1)
                            dst = aw[lv]
                            if lv == 0:
                                nlo, nhi = lo + 1, hi
                                s = sc.add("vector", lambda e, dst=dst, src=src, nlo=nlo, nhi=nhi:
                                           e.tensor_tensor(out=dst[:, nlo:nhi], in0=src[:, nlo - 1:nhi - 1], in1=src[:, nlo:nhi], op=ALU.add),
                                           deps=[s])
                            else:
                                nlo, nhi = lo + sh, hi - sh
                                s = sc.add("vector", lambda e, dst=dst, src=src, nlo=nlo, nhi=nhi, sh=sh:
                                           e.tensor_tensor(out=dst[:, nlo:nhi], in0=src[:, nlo - sh:nhi - sh], in1=src[:, nlo + sh:nhi + sh], op=ALU.add),
                                           deps=[s])
                            src = dst; lo, hi = nlo, nhi
                        assert lo <= HALO and hi >= HALO + T
                        s = sc.add("vector", lambda e, src=src: e.tensor_tensor(out=ptmp[:], in0=src[:, HALO:HALO + T], in1=invc[:], op=ALU.mult),
                                   deps=[s, s_inv])
                        s = sc.add("vector", lambda e, j=j: e.tensor_tensor(out=dT[:, j, :], in0=ptmp[:], in1=pmb[:, HALO:HALO + T], op=ALU.subtract),
                                   deps=[s])
                        last = s
                    sc.run()
                with ExitStack() as st:
                    sc = Sched(nc, st, f"p3b{gi}")
                    s_w = sc.add("sync", lambda e: e.dma_start(out=wp[:], in_=w_pool[gi, :, :].rearrange("(j p) n -> p j n", p=128)),
                                 dma_key="wp")
                    evs = []; sts = []
                    cidx = 0
                    for jo in range(4):
                        for tt in range(NOWN):
                            c = cidx; cidx += 1
                            def mmf(e, jo=jo, tt=tt, c=c):
                                for ji in range(4):
                                    mm = e.matmul(pacc[c % 2][:], lhsT=hi16(wp[:, ji, jo * 128:(jo + 1) * 128]),
                                                  rhs=dT[:, ji, tt * 512:(tt + 1) * 512], start=(ji == 0), stop=(ji == 3))
                                return mm
                            s_mm = sc.add("tensor", mmf, deps=[s_w, evs[c - 2] if c >= 2 else None])
                            col = gi * 4 + jo
                            s_ev = sc.add("vector", lambda e, c=c, col=col: e.tensor_scalar(out=pob[c % 2][:], in0=pacc[c % 2][:],
                                          scalar1=ps_sb[:, col:col + 1], scalar2=None, op0=ALU.mult),
                                          deps=[s_mm, sts[c - 2] if c >= 2 else None])
                            evs.append(s_ev)
                            s_st = sc.add("gpsimd", lambda e, c=c, col=col, tt=tt: e.dma_start(
                                          out=mixT_d[16 + col, :, tt * 512:(tt + 1) * 512], in_=pob[c % 2][:]),
                                          deps=[s_ev], dma_key=f"pob{c % 2}")
                            sts.append(s_st)
                    sc.run(final_waits=sts[-2:])

    if upto <= 3:
        _finish_dummy(nc, out)
        return nc

    if "4a" not in SKIP:
        with ExitStack() as st:
            g1bc = st.enter_context(nc.sbuf_tensor("g1bc", [128, D], F32))
            mixb = [st.enter_context(nc.sbuf_tensor(f"mixb{i}", [128, KC, 512], BF16)) for i in range(2)]
            wob = [st.enter_context(nc.sbuf_tensor(f"wob{i}", [128, KC, 256], F32)) for i in range(2)]
            xp = [st.enter_context(nc.sbuf_tensor(f"xp{i}", [128, 256], F32)) for i in range(4)]
            tq = [st.enter_context(nc.sbuf_tensor(f"tq{i}", [128, 256], F32)) for i in range(2)]
            om = [st.enter_context(nc.sbuf_tensor(f"om{i}", [128, 256], F32)) for i in range(4)]
            oacc4 = [st.enter_context(nc.psum_tensor(f"oacc4_{i}", [128, 256], F32)) for i in range(2)]
            sc = Sched(nc, st, "p4a")
            s_g1 = sc.add("sync", lambda e: e.dma_start(out=g1bc[:], in_=mod_d[0:1, 2 * D:3 * D].partition_broadcast(128)),
                          dma_key="g1bc")
            mm_steps = []; t_steps = []; xm_steps = []; st_steps = []
            ci = 0
            last_mm_of_tg = {}
            last_mm_of_blk = {}
            for tg in range(NOWN):
                s_mix = sc.add("sync", lambda e, tg=tg: e.dma_start(out=mixb[tg % 2][:],
                               in_=mixT_d[:, :, tg * 512:(tg + 1) * 512].rearrange("m p t -> p m t")),
                               deps=[last_mm_of_tg.get(tg - 2)], dma_key=f"mixb{tg % 2}")
                for nb in range(16):
                    k = tg * 16 + nb
                    s_wo = sc.add("sync", lambda e, nb=nb, k=k: e.dma_start(out=wob[k % 2][:],
                                  in_=w_out[:, nb * 256:(nb + 1) * 256].rearrange("(m p) n -> p m n", p=128)),
                                  deps=[last_mm_of_blk.get(k - 2)], dma_key=f"wob{k % 2}")
                    for sub in range(4):
                        r0 = tg * 512 + sub * 128
                        s_x = sc.add("sync", lambda e, r0=r0, nb=nb, ci=ci: e.dma_start(out=xp[ci % 4][:],
                                     in_=x[r0:r0 + 128, nb * 256:(nb + 1) * 256]),
                                     deps=[xm_steps[ci - 4] if ci >= 4 else None], dma_key=f"xp{ci % 4}")
                        def mmf(e, tg=tg, k=k, sub=sub, ci=ci):
                            for m in range(KC):
                                mm = e.matmul(oacc4[ci % 2][:], lhsT=mixb[tg % 2][:, m, sub * 128:(sub + 1) * 128],
                                              rhs=hi16(wob[k % 2][:, m, :]), start=(m == 0), stop=(m == KC - 1))
                            return mm
                        s_mm = sc.add("tensor", mmf, deps=[s_mix, s_wo, t_steps[ci - 2] if ci >= 2 else None])
                        s_t = sc.add("vector", lambda e, ci=ci, nb=nb: e.tensor_tensor(out=tq[ci % 2][:], in0=oacc4[ci % 2][:],
                                     in1=g1bc[:, nb * 256:(nb + 1) * 256], op=ALU.mult),
                                     deps=[s_mm, s_g1, xm_steps[ci - 2] if ci >= 2 else None])
                        s_xm = sc.add("vector", lambda e, ci=ci: e.tensor_tensor(out=om[ci % 4][:], in0=tq[ci % 2][:],
                                      in1=xp[ci % 4][:], op=ALU.add),
                                      deps=[s_t, s_x, st_steps[ci - 4] if ci >= 4 else None])
                        s_st = sc.add("gpsimd", lambda e, ci=ci, r0=r0, nb=nb: e.dma_start(
                                      out=out[r0:r0 + 128, nb * 256:(nb + 1) * 256], in_=om[ci % 4][:]),
                                      deps=[s_xm], dma_key=f"om{ci % 4}")
                        mm_steps.append(s_mm); t_steps.append(s_t); xm_steps.append(s_xm); st_steps.append(s_st)
                        last_mm_of_tg[tg] = s_mm; last_mm_of_blk[k] = s_mm
                        ci += 1
            with nc.allow_non_contiguous_dma(reason="p4a tiles"):
                sc.run(final_waits=st_steps[-4:])

    if upto <= 4 and not os.environ.get("K_P4B"):
        return nc

    NT4 = T // 128
    with ExitStack() as st:
        xs = [st.enter_context(nc.sbuf_tensor(f"xs{i}", [128, D], F32)) for i in range(2)]
        junk2 = st.enter_context(nc.sbuf_tensor("junk2", [128, D], BF16))
        xn2 = [st.enter_context(nc.sbuf_tensor(f"xn2_{i}", [128, D], BF16)) for i in range(2)]
        h2t = [st.enter_context(nc.sbuf_tensor(f"h2t{i}", [128, KC, 128], BF16)) for i in range(2)]
        wr = st.enter_context(nc.sbuf_tensor("wr", [128, KC, NE], F32))
        brt = st.enter_context(nc.sbuf_tensor("brt", [128, NE], F32))
        b2s = st.enter_context(nc.sbuf_tensor("b2s", [NE, D], F32))
        g2bc = st.enter_context(nc.sbuf_tensor("g2bc", [128, D], F32))
        ss2 = st.enter_context(nc.sbuf_tensor("ss2", [128, NT4], F32))
        rsA = st.enter_context(nc.sbuf_tensor("rsA", [128, NT4], F32))
        rsB = st.enter_context(nc.sbuf_tensor("rsB", [128, NT4], F32))
        rsC = st.enter_context(nc.sbuf_tensor("rsC", [128, NT4], F32))
        lgs = st.enter_context(nc.sbuf_tensor("lgs", [128, NE], F32))
        mx8 = st.enter_context(nc.sbuf_tensor("mx8", [128, 8], F32))
        nmx = st.enter_context(nc.sbuf_tensor("nmx", [128, 1], F32))
        msk = st.enter_context(nc.sbuf_tensor("msk", [128, NE], F32))
        exv = st.enter_context(nc.sbuf_tensor("exv", [128, NE], F32))
        exm = st.enter_context(nc.sbuf_tensor("exm", [128, NE], F32))
        ssum = st.enter_context(nc.sbuf_tensor("ssum", [128, 1], F32))
        rsum = st.enter_context(nc.sbuf_tensor("rsum", [128, 1], F32))
        gts = st.enter_context(nc.sbuf_tensor("gts", [128, NE], F32))
        tb = [st.enter_context(nc.sbuf_tensor(f"tb{i}", [128, 512], F32)) for i in range(2)]
        tp2 = [st.enter_context(nc.psum_tensor(f"tp2_{i}", [128, 4, 128], BF16)) for i in range(2)]
        lgp = st.enter_context(nc.psum_tensor("lgp", [128, NE], F32))
        gtp = st.enter_context(nc.psum_tensor("gtp", [NE, 128], F32))
        bp = [st.enter_context(nc.psum_tensor(f"bp{i}", [128, 512], F32)) for i in range(2)]
        sc = Sched(nc, st, "p4b")
        s_wr = sc.add("sync", lambda e: e.dma_start(out=wr[:], in_=w_router.rearrange("(k p) n -> p k n", p=128)), dma_key="wr")
        s_br = sc.add("sync", lambda e: e.dma_start(out=brt[:], in_=b_router[0:1, :].partition_broadcast(128)), dma_key="brt")
        s_b2 = sc.add("sync", lambda e: e.dma_start(out=b2s[:], in_=b2[:, :]), dma_key="b2s")
        s_g2 = sc.add("sync", lambda e: e.dma_start(out=g2bc[:], in_=mod_d[0:1, 5 * D:6 * D].partition_broadcast(128)), dma_key="g2bc")
        s_z = sc.add("vector", lambda e: e.memset(ss2[:], 0.0))
        store = {}; xn_s = {}; last_tr = {}; h2_free = {}; lg_s = None; gcp_s = None
        u2_hist = []
        nbt = 0; evt_hist = []
        nq = 0
        for i in range(NT4):
            b = i % 2
            r0 = i * 128
            s_ld = sc.add("sync", lambda e, b=b, r0=r0: e.dma_start(out=xs[b][:], in_=out[r0:r0 + 128, :]),
                          deps=[store.get(i - 2)], dma_key=f"xs{b}")
            s_sq = sc.add("scalar", lambda e, b=b, i=i: e.activation(out=junk2[:], in_=xs[b][:], func=AF.Square,
                          accum_out=ss2[:, i:i + 1]), deps=[s_ld, s_z])
            s_r1 = sc.add("vector", lambda e, i=i: e.tensor_scalar(out=rsA[:, i:i + 1], in0=ss2[:, i:i + 1], scalar1=1.0 / D,
                          scalar2=EPS, op0=ALU.mult, op1=ALU.add), deps=[s_sq])
            s_r2 = sc.add("scalar", lambda e, i=i: e.activation(out=rsB[:, i:i + 1], in_=rsA[:, i:i + 1], func=AF.Sqrt), deps=[s_r1])
            s_r3 = sc.add("vector", lambda e, i=i: e.reciprocal(out=rsC[:, i:i + 1], in_=rsB[:, i:i + 1]), deps=[s_r2])
            s_xn = sc.add("vector", lambda e, b=b, i=i: e.tensor_scalar(out=xn2[b][:], in0=xs[b][:], scalar1=rsC[:, i:i + 1],
                          scalar2=None, op0=ALU.mult), deps=[s_r3, last_tr.get(i - 2)])
            xn_s[i] = s_xn
            s_evt = None
            for bt in range(8):
                def trf(e, b=b, bt=bt, nbt=nbt):
                    for j in range(4):
                        kc = bt * 4 + j
                        tr = e.transpose(tp2[nbt % 2][:, j, :], xn2[b][:, kc * 128:(kc + 1) * 128], identb[:])
                    return tr
                s_tr = sc.add("tensor", trf, deps=[s_xn, evt_hist[nbt - 2] if nbt >= 2 else None])
                def evf(e, b=b, bt=bt, nbt=nbt):
                    for j in range(4):
                        kc = bt * 4 + j
                        ins = e.activation(out=h2t[b][:, kc, :], in_=tp2[nbt % 2][:, j, :], func=AF.Identity,
                                           bias=SH2[:, kc:kc + 1], scale=G2[:, kc:kc + 1])
                    return ins
                s_evt = sc.add("scalar", evf, deps=[s_tr] + (list(h2_free.get(i - 2, ())) if bt == 0 else []))
                evt_hist.append(s_evt)
                nbt += 1
            last_tr[i] = s_tr
            s_sth = sc.add("gpsimd", lambda e, b=b, r0=r0: e.dma_start(
                           out=h2T_d[:, :, r0:r0 + 128].rearrange("k p t -> p k t"), in_=h2t[b][:]),
                           deps=[s_evt], dma_key=f"h2t{b}")
            def rtf(e, b=b):
                for kc in range(KC):
                    mm = e.matmul(lgp[:], lhsT=h2t[b][:, kc, :], rhs=hi16(wr[:, kc, :]), start=(kc == 0), stop=(kc == KC - 1))
                return mm
            s_rt = sc.add("tensor", rtf, deps=[s_evt, s_wr, lg_s])
            s_lg = sc.add("vector", lambda e: e.tensor_tensor(out=lgs[:], in0=lgp[:], in1=brt[:], op=ALU.add), deps=[s_rt, s_br, gcp_s])
            lg_s = s_lg
            s_mx = sc.add("vector", lambda e: e.max(out=mx8[:], in_=lgs[:]), deps=[s_lg])
            s_nm = sc.add("vector", lambda e: e.tensor_scalar(out=nmx[:], in0=mx8[:, 0:1], scalar1=-1.0, scalar2=None, op0=ALU.mult), deps=[s_mx])
            s_mk = sc.add("vector", lambda e: e.tensor_scalar(out=msk[:], in0=lgs[:], scalar1=mx8[:, 3:4], scalar2=None, op0=ALU.is_ge), deps=[s_mx])
            s_ex = sc.add("scalar", lambda e: e.activation(out=exv[:], in_=lgs[:], func=AF.Exp, bias=nmx[:, 0:1], scale=1.0), deps=[s_nm])
            s_em = sc.add("vector", lambda e: e.tensor_tensor(out=exm[:], in0=exv[:], in1=msk[:], op=ALU.mult), deps=[s_ex, s_mk])
            s_sm = sc.add("vector", lambda e: e.reduce_sum(out=ssum[:], in_=exm[:], axis=AX.X), deps=[s_em])
            s_rc = sc.add("vector", lambda e: e.reciprocal(out=rsum[:], in_=ssum[:]), deps=[s_sm])
            s_gt = sc.add("vector", lambda e: e.tensor_scalar(out=gts[:], in0=exm[:], scalar1=rsum[:, 0:1], scalar2=None, op0=ALU.mult), deps=[s_rc])
            s_gtt = sc.add("tensor", lambda e: e.transpose(gtp[:], gts[:], identf[:]), deps=[s_gt, gcp_s])
            s_gcp = sc.add("vector", lambda e, r0=r0: e.tensor_copy(out=gatesT[:, r0:r0 + 128], in_=gtp[:]), deps=[s_gtt])
            gcp_s = s_gcp
            s_u2 = None
            for n8 in range(8):
                q = nq; nq += 1
                s_bm = sc.add("tensor", lambda e, q=q, r0=r0, n8=n8: e.matmul(bp[q % 2][:], lhsT=gatesT[:, r0:r0 + 128],
                              rhs=b2s[:, n8 * 512:(n8 + 1) * 512], start=True, stop=True),
                              deps=[s_gcp, s_b2, u2_hist[q - 2] if q >= 2 else None])
                s_u1 = sc.add("vector", lambda e, q=q, n8=n8: e.tensor_tensor(out=tb[q % 2][:], in0=bp[q % 2][:],
                              in1=g2bc[:, n8 * 512:(n8 + 1) * 512], op=ALU.mult), deps=[s_bm, s_g2])
                s_u2 = sc.add("vector", lambda e, q=q, n8=n8, b=b: e.tensor_tensor(out=xs[b][:, n8 * 512:(n8 + 1) * 512],
                              in0=tb[q % 2][:], in1=xs[b][:, n8 * 512:(n8 + 1) * 512], op=ALU.add), deps=[s_u1, s_xn, s_sq])
                u2_hist.append(s_u2)
            s_st = sc.add("gpsimd", lambda e, b=b, r0=r0: e.dma_start(out=out[r0:r0 + 128, :], in_=xs[b][:]),
                          deps=[s_u2], dma_key=f"xo{b}")
            store[i] = s_st
            h2_free[i] = (s_sth, s_rt)
            sth_last = s_sth
        s_gd = sc.add("gpsimd", lambda e: e.dma_start(out=gates_dbg[:, :], in_=gatesT[:]), deps=[gcp_s], dma_key="gdbg")
        with nc.allow_non_contiguous_dma(reason="p4b tiles"):
            sc.run(final_waits=[store[NT4 - 1], store[NT4 - 2], h2_free[NT4 - 1][0], h2_free[NT4 - 2][0], s_gd])

    if upto <= 4:
        return nc

    class Ring:
        def __init__(self, tiles):
            self.tiles = tiles; self.i = -1; self.users = [[] for _ in tiles]
        def acquire(self):
            self.i += 1
            k = self.i % len(self.tiles)
            deps = self.users[k]; self.users[k] = []
            return k, self.tiles[k], deps
        def use(self, k, step):
            self.users[k].append(step)

    TCH = 1024
    NCH = T // TCH
    NEXP = int(os.environ.get("K_NEXP", NE))
    with ExitStack() as st:
        sbt = lambda name, shape, dt=F32: st.enter_context(nc.sbuf_tensor(name, list(shape), dt))
        h2c = sbt("h2c", [128, KC, TCH], BF16)
        w1r = Ring([sbt(f"w1r{i}", [128, KC, 128]) for i in range(4)])
        w2r = Ring([sbt(f"w2r{i}", [128, 8, 256]) for i in range(2)])
        actT = sbt("actT", [128, 8, TCH], BF16)
        gbc = Ring([sbt(f"gbc{i}", [128, 512]) for i in range(4)])
        tglu = Ring([sbt(f"tglu{i}", [128, 512]) for i in range(2)])
        tsg = Ring([sbt(f"tsg{i}", [128, 512]) for i in range(2)])
        tlin = Ring([sbt(f"tlin{i}", [128, 512]) for i in range(2)])
        prev = Ring([sbt(f"prev{i}", [128, 256]) for i in range(8)])
        obuf = Ring([sbt(f"obuf{i}", [128, 256]) for i in range(8)])
        eselr = Ring([sbt(f"eselr{i}", [NE, 128]) for i in range(2)])
        b1r = Ring([sbt(f"b1r{i}", [128, 16]) for i in range(2)])
        pg = Ring([st.enter_context(nc.psum_tensor(f"pg{i}", [128, 512], F32)) for i in range(2)])
        pl = Ring([st.enter_context(nc.psum_tensor(f"pl{i}", [128, 512], F32)) for i in range(2)])
        gbp = Ring([st.enter_context(nc.psum_tensor("gbp0", [128, 512], F32))])
        py = Ring([st.enter_context(nc.psum_tensor(f"py{i}", [128, 256], F32)) for i in range(3)])
        sc = Sched(nc, st, "p5")
        w1_pref = {}

        def load_w1(ex, j):
            kwg, wg, dg = w1r.acquire()
            s_wg = sc.add("sync", lambda e, wg=wg, ex=ex, j=j: e.dma_start(out=wg[:],
                          in_=w1[ex, :, j * 128:(j + 1) * 128].rearrange("(k p) n -> p k n", p=128)),
                          deps=dg, dma_key=f"w1r{kwg}")
            w1r.use(kwg, s_wg)
            kwl, wl, dl = w1r.acquire()
            s_wl = sc.add("sync", lambda e, wl=wl, ex=ex, j=j: e.dma_start(out=wl[:],
                          in_=w1[ex, :, DFF + j * 128:DFF + (j + 1) * 128].rearrange("(k p) n -> p k n", p=128)),
                          deps=dl, dma_key=f"w1r{kwl}")
            w1r.use(kwl, s_wl)
            return kwg, wg, s_wg, kwl, wl, s_wl

        last_store = {}
        h2_users = []
        act_readers = []
        stores = []
        for c in range(NCH):
            t0 = c * TCH
            s_h2 = sc.add("sync", lambda e, t0=t0: e.dma_start(out=h2c[:], in_=h2T_d[:, :, t0:t0 + TCH].rearrange("k p t -> p k t")),
                          deps=h2_users[-1:], dma_key="h2c")
            for ex in range(NEXP):
                ke, et, ed = eselr.acquire()
                s_es = sc.add("sync", lambda e, et=et, ex=ex: e.dma_start(out=et[:], in_=esel_in[:, ex * 128:(ex + 1) * 128]),
                              deps=ed, dma_key=f"esel{ke}")
                eselr.use(ke, s_es)
                kb1, b1t, b1d = b1r.acquire()
                with nc.allow_non_contiguous_dma(reason="b1"):
                    pass
                s_b1 = sc.add("sync", lambda e, b1t=b1t, ex=ex: e.dma_start(out=b1t[:],
                              in_=b1[ex:ex + 1, :].rearrange("o (h p) -> p (o h)", p=128)), deps=b1d, dma_key=f"b1r{kb1}")
                b1r.use(kb1, s_b1)
                gb = []
                for tt in range(TCH // 512):
                    kp, pt, pd = gbp.acquire()
                    s_gm = sc.add("tensor", lambda e, pt=pt, et=et, t0=t0, tt=tt: e.matmul(pt[:], lhsT=et[:],
                                  rhs=gatesT[:, t0 + tt * 512:t0 + (tt + 1) * 512], start=True, stop=True), deps=[s_es] + pd)
                    eselr.use(ke, s_gm)
                    kg, gt_, gd = gbc.acquire()
                    s_gc = sc.add("vector", lambda e, gt_=gt_, pt=pt: e.tensor_copy(out=gt_[:], in_=pt[:]), deps=[s_gm] + gd)
                    gbp.use(kp, s_gc); gbc.use(kg, s_gc)
                    gb.append((kg, gt_, s_gc))
                first_act_deps = list(act_readers); act_readers = []
                for j in range(8):
                    if j == 0 and (c, ex) in w1_pref:
                        kwg, wg, s_wg, kwl, wl, s_wl = w1_pref.pop((c, ex))
                    else:
                        kwg, wg, s_wg, kwl, wl, s_wl = load_w1(ex, j)
                    for tt in range(TCH // 512):
                        cs_ = slice(tt * 512, (tt + 1) * 512)
                        kpg, pgt, pgd = pg.acquire()
                        def mmg(e, pgt=pgt, wg=wg, cs_=cs_):
                            for k in range(KC):
                                mm = e.matmul(pgt[:], lhsT=hi16(wg[:, k, :]), rhs=h2c[:, k, cs_], start=(k == 0), stop=(k == KC - 1))
                            return mm
                        s_mg = sc.add("tensor", mmg, deps=[s_wg, s_h2] + pgd)
                        w1r.use(kwg, s_mg)
                        kpl, plt, pld = pl.acquire()
                        def mml(e, plt=plt, wl=wl, cs_=cs_):
                            for k in range(KC):
                                mm = e.matmul(plt[:], lhsT=hi16(wl[:, k, :]), rhs=h2c[:, k, cs_], start=(k == 0), stop=(k == KC - 1))
                            return mm
                        s_ml = sc.add("tensor", mml, deps=[s_wl, s_h2] + pld)
                        w1r.use(kwl, s_ml)
                        h2_users.append(s_ml)
                        k1, g_t, d1 = tglu.acquire()
                        s_glu = sc.add("vector", lambda e, g_t=g_t, pgt=pgt, b1t=b1t, j=j: e.tensor_scalar(out=g_t[:], in0=pgt[:],
                                       scalar1=b1t[:, j:j + 1], scalar2=SW_LIMIT, op0=ALU.add, op1=ALU.min), deps=[s_mg, s_b1] + d1)
                        pg.use(kpg, s_glu); b1r.use(kb1, s_glu)
                        k2, s_t, d2 = tsg.acquire()
                        s_sg = sc.add("scalar", lambda e, s_t=s_t, g_t=g_t: e.activation(out=s_t[:], in_=g_t[:], func=AF.Sigmoid,
                                      scale=SW_ALPHA), deps=[s_glu] + d2)
                        k3, l_t, d3 = tlin.acquire()
                        s_l1 = sc.add("vector", lambda e, l_t=l_t, plt=plt, b1t=b1t, j=j: e.tensor_scalar(out=l_t[:], in0=plt[:],
                                      scalar1=b1t[:, 8 + j:9 + j], scalar2=SW_LIMIT, op0=ALU.add, op1=ALU.min), deps=[s_ml, s_b1] + d3)
                        pl.use(kpl, s_l1); b1r.use(kb1, s_l1)
                        s_l2 = sc.add("vector", lambda e, l_t=l_t: e.tensor_scalar(out=l_t[:], in0=l_t[:], scalar1=-SW_LIMIT,
                                      scalar2=1.0, op0=ALU.max, op1=ALU.add), deps=[s_l1])
                        s_m1 = sc.add("vector", lambda e, g_t=g_t, s_t=s_t: e.tensor_tensor(out=g_t[:], in0=g_t[:], in1=s_t[:],
                                      op=ALU.mult), deps=[s_sg])
                        tsg.use(k2, s_m1)
                        s_m2 = sc.add("vector", lambda e, g_t=g_t, l_t=l_t: e.tensor_tensor(out=g_t[:], in0=g_t[:], in1=l_t[:],
                                      op=ALU.mult), deps=[s_m1, s_l2])
                        tlin.use(k3, s_m2)
                        kg, gt_, s_gc = gb[tt]
                        s_act = sc.add("vector", lambda e, g_t=g_t, gt_=gt_, j=j, cs_=cs_: e.tensor_tensor(out=actT[:, j, cs_],
                                       in0=g_t[:], in1=gt_[:], op=ALU.mult), deps=[s_m2, s_gc] + first_act_deps)
                        first_act_deps = []
                        tglu.use(k1, s_act); gbc.use(kg, s_act)
                        last_act = s_act
                if ex + 1 < NEXP:
                    w1_pref[(c, ex + 1)] = load_w1(ex + 1, 0)
                elif c + 1 < NCH:
                    w1_pref[(c + 1, 0)] = load_w1(0, 0)
                for n16 in range(16):
                    kw2, w2t, d2w = w2r.acquire()
                    s_w2 = sc.add("sync", lambda e, w2t=w2t, ex=ex, n16=n16: e.dma_start(out=w2t[:],
                                  in_=w2[ex, :, n16 * 256:(n16 + 1) * 256].rearrange("(j p) n -> p j n", p=128)),
                                  deps=d2w, dma_key=f"w2r{kw2}")
                    w2r.use(kw2, s_w2)
                    for ts in range(TCH // 128):
                        r0 = t0 + ts * 128
                        region = (r0, n16)
                        first = (ex == 0)
                        if not first:
                            kpv, pv, pvd = prev.acquire()
                            s_pv = sc.add("scalar", lambda e, pv=pv, r0=r0, n16=n16: e.dma_start(out=pv[:],
                                          in_=moe_d[r0:r0 + 128, n16 * 256:(n16 + 1) * 256]),
                                          deps=pvd + [last_store[region]], dma_key=f"prev{kpv}")
                        kpy, pyt, pyd = py.acquire()
                        def mmy(e, pyt=pyt, w2t=w2t, ts=ts):
                            for j in range(8):
                                mm = e.matmul(pyt[:], lhsT=actT[:, j, ts * 128:(ts + 1) * 128], rhs=hi16(w2t[:, j, :]),
                                              start=(j == 0), stop=(j == 7))
                            return mm
                        s_my = sc.add("tensor", mmy, deps=[s_w2, last_act] + pyd)
                        w2r.use(kw2, s_my)
                        act_readers.append(s_my)
                        if len(act_readers) > 2:
                            act_readers = act_readers[-2:]
                        ko, ot, od = obuf.acquire()
                        if first:
                            s_o = sc.add("vector", lambda e, ot=ot, pyt=pyt: e.tensor_copy(out=ot[:], in_=pyt[:]), deps=[s_my] + od)
                        else:
                            s_o = sc.add("vector", lambda e, ot=ot, pyt=pyt, pv=pv: e.tensor_tensor(out=ot[:], in0=pyt[:], in1=pv[:],
                                         op=ALU.add), deps=[s_my, s_pv] + od)
                            prev.use(kpv, s_o)
                        py.use(kpy, s_o)
                        s_st = sc.add("gpsimd", lambda e, ot=ot, r0=r0, n16=n16: e.dma_start(
                                      out=moe_d[r0:r0 + 128, n16 * 256:(n16 + 1) * 256], in_=ot[:]), deps=[s_o], dma_key=f"obuf{ko}")
                        obuf.use(ko, s_st)
                        last_store[region] = s_st
                        stores.append(s_st)
        with nc.allow_non_contiguous_dma(reason="p5 tiles"):
            sc.run(final_waits=stores[-8:])

    with ExitStack() as st:
        g2b = st.enter_context(nc.sbuf_tensor("g2b6", [128, D], F32))
        oi = Ring([st.enter_context(nc.sbuf_tensor(f"oi{i}", [128, D], F32)) for i in range(2)])
        ma = Ring([st.enter_context(nc.sbuf_tensor(f"ma{i}", [128, D], F32)) for i in range(2)])
        sc = Sched(nc, st, "p6")
        s_g2 = sc.add("sync", lambda e: e.dma_start(out=g2b[:], in_=mod_d[0:1, 5 * D:6 * D].partition_broadcast(128)), dma_key="g2b")
        fin = []
        for i in range(T // 128):
            r0 = i * 128
            ko, ot, od = oi.acquire()
            s_lo = sc.add("sync", lambda e, ot=ot, r0=r0: e.dma_start(out=ot[:], in_=out[r0:r0 + 128, :]), deps=od, dma_key=f"oi{ko}")
            km, mt, md = ma.acquire()
            s_lm = sc.add("sync", lambda e, mt=mt, r0=r0: e.dma_start(out=mt[:], in_=moe_d[r0:r0 + 128, :]), deps=md, dma_key=f"ma{km}")
            s_a = sc.add("vector", lambda e, mt=mt: e.tensor_tensor(out=mt[:], in0=mt[:], in1=g2b[:], op=ALU.mult), deps=[s_lm, s_g2])
            s_b = sc.add("vector", lambda e, mt=mt, ot=ot: e.tensor_tensor(out=ot[:], in0=ot[:], in1=mt[:], op=ALU.add), deps=[s_a, s_lo])
            ma.use(km, s_b)
            s_st = sc.add("gpsimd", lambda e, ot=ot, r0=r0: e.dma_start(out=out[r0:r0 + 128, :], in_=ot[:]), deps=[s_b], dma_key=f"oo{ko}")
            oi.use(ko, s_st)
            fin.append(s_st)
        with nc.allow_non_contiguous_dma(reason="p6"):
            sc.run(final_waits=fin[-2:])
    return nc


def _finish_dummy(nc, out):
    with ExitStack() as st:
        z = st.enter_context(nc.sbuf_tensor("zz", [128, D], F32))
        s = Cnt(nc, st, "zz_s"); s2 = Cnt(nc, st, "zz_s2")
        with nc.Block() as blk:
            @blk.vector
            def _(e):
                e.memset(z[:], 0.0).then_inc(s.s, 1)

            @blk.gpsimd
            def _(e):
                e.wait_ge(s.s, 1)
                for i in range(T // 128):
                    e.dma_start(out=out[i * 128:(i + 1) * 128, :], in_=z[:]).then_inc(s2.s, 16)
                e.wait_ge(s2.s, 16 * (T // 128))
        clear_sems(nc)


def make_in_maps(inputs, cores, names=None):
    rot, identb, identf, esel = _consts()
    x = np.asarray(inputs["x"])[0]
    shapes = {"c": (1, D), "b_mod": (1, 6 * D), "norm1_g": (1, D), "q_norm_g": (1, 128), "k_norm_g": (1, 128),
              "pool_scale": (1, 2048), "norm2_g": (1, D), "b_router": (1, NE)}
    shared = {"rotm": rot, "identb": identb, "identf": identf, "onesb": np.ones((128, 128), ml_dtypes.bfloat16), "esel": esel.reshape(32, 32 * 128)}
    for k, v in inputs.items():
        if k == "x" or (names is not None and k not in names):
            continue
        a = np.asarray(v)
        shared[k] = a.reshape(shapes[k]) if k in shapes else np.ascontiguousarray(a[0])
        if k in ("w1", "w2") and os.environ.get("K_NEXP"):
            shared[k] = np.ascontiguousarray(shared[k][:int(os.environ["K_NEXP"])])
    maps = []
    for core in cores:
        C, Sn, pm, ic = _tables(core)
        m = dict(shared)
        m["x"] = np.ascontiguousarray(np.roll(x, -core * T, axis=0))
        m["ropeC"] = C; m["ropeS"] = Sn; m["pmask"] = pm; m["invcnt"] = ic
        if names is not None:
            m = {k: v for k, v in m.items() if k in names}
        maps.append(m)
    return maps


def kernel(**inputs):
    nc = build()
    maps = make_in_maps(inputs, list(range(NC)))
    res = run_bass_kernel_spmd(nc, maps, core_ids=list(range(NC)))
    outs = [res.results[i]["out"] for i in range(NC)]
    return np.concatenate(outs, axis=0).reshape(1, S, D).astype(np.float32)
```
